# Optimizing a Trainium2 kernel written in Bass

```python
import math
import functools
import jax
import jax.numpy as jnp
from jax import lax
import numpy as np

D_MODEL = 1024
BATCH = 4
SEQ = 8192
DEPTH = 2
DEC_BATCH = 32
DEC_SEQ = 16
PAST_LEN = 4096

CHUNK = 64
A_HEADS = 4
A_DIM = 64
A_WIDTH = A_HEADS * 2 * A_DIM
ROT_DIM = A_DIM // 4
ROPE_THETA = 500000.0
Q_BLOCK = 128
B_HEADS = 8
B_DIM = 64
B_WIDTH = B_HEADS * B_DIM
LEFT_CHUNKS = 8
B_REACH = LEFT_CHUNKS * CHUNK
BAND = B_REACH + CHUNK
REL_CLIP = 128
N_REL = 2 * REL_CLIP + 1
MIX_WIDTH = A_WIDTH + B_WIDTH
IN_SPLITS = [A_WIDTH, 2 * A_WIDTH, 3 * A_WIDTH, 3 * A_WIDTH + B_WIDTH, 3 * A_WIDTH + 2 * B_WIDTH]
IN_WIDTH = 3 * A_WIDTH + 3 * B_WIDTH
PK_HEADS = 8
N_KEYS = 128
N_EXPERTS = N_KEYS * N_KEYS
PK_TOPK = 16
PK_QDIM = 256
PK_HALF = PK_QDIM // 2
PEER_BLOCK = 128
EPS = 1e-6

kernel_name = 'hybrid_diffattn_chunkband_peer_stream_step'


def rms_norm(x, g):
    x32 = x.astype(jnp.float32)
    y = x32 * lax.rsqrt(jnp.mean(x32 * x32, axis=-1, keepdims=True) + EPS)
    return y.astype(x.dtype) * g


def rope_partial(x, pos):
    half = ROT_DIM // 2
    inv = ROPE_THETA ** (-jnp.arange(0, ROT_DIM, 2, dtype=jnp.float32) / ROT_DIM)
    ang = pos.astype(jnp.float32)[:, None] * inv[None, :]
    cos = jnp.cos(ang)[:, None, None, :].astype(x.dtype)
    sin = jnp.sin(ang)[:, None, None, :].astype(x.dtype)
    x1 = x[..., :half]
    x2 = x[..., half:ROT_DIM]
    return jnp.concatenate([x1 * cos - x2 * sin, x2 * cos + x1 * sin, x[..., ROT_DIM:]], axis=-1)


def diff_lambda(lq1, lk1, lq2, lk2, lam_init):
    f = lambda a: a.astype(jnp.float32)
    return jnp.exp(jnp.sum(f(lq1) * f(lk1))) - jnp.exp(jnp.sum(f(lq2) * f(lk2))) + lam_init


def diff_attend(q, k, v, lam, mask):
    s = jnp.einsum('bqhmd,bkhmd->bhmqk', q, k).astype(jnp.float32) * (A_DIM ** -0.5)
    if mask is not None:
        s = jnp.where(mask, s, -jnp.inf)
    p = jax.nn.softmax(s, axis=-1)
    a = p[:, :, 0] - lam * p[:, :, 1]
    return jnp.einsum('bhqk,bkhe->bqhe', a.astype(v.dtype), v)


def diff_attn_prompt(q, k, v, lam):
    bn, s = q.shape[0], q.shape[1]
    nb = s // Q_BLOCK
    qb = q.reshape(bn, nb, Q_BLOCK, A_HEADS, 2, A_DIM).swapaxes(0, 1)
    kchunk = jnp.arange(s) // CHUNK

    def one(args):
        qi, bi = args
        qchunk = (bi * Q_BLOCK + jnp.arange(Q_BLOCK)) // CHUNK
        mask = kchunk[None, :] <= qchunk[:, None]
        return diff_attend(qi, k, v, lam, mask)

    o = lax.map(one, (qb, jnp.arange(nb)))
    return o.swapaxes(0, 1).reshape(bn, s, A_HEADS, 2 * A_DIM)


def rel_bias_matrix(table, qpos, kpos):
    d = jnp.clip(qpos[:, None] - kpos[None, :], -REL_CLIP, REL_CLIP) + REL_CLIP
    return table[:, d].astype(jnp.float32)


def band_attend(q, k, v, bias, valid):
    s = jnp.einsum('...qhd,...khd->...hqk', q, k).astype(jnp.float32) * (B_DIM ** -0.5) + bias
    if valid is not None:
        s = jnp.where(valid, s, -jnp.inf)
    p = jax.nn.softmax(s, axis=-1)
    return jnp.einsum('...hqk,...khd->...qhd', p.astype(v.dtype), v)


def chunk_band_prompt(q, k, v, table):
    bn, s = q.shape[0], q.shape[1]
    nc = s // CHUNK
    pad = ((0, 0), (B_REACH, 0), (0, 0), (0, 0))
    kp = jnp.pad(k, pad).reshape(bn, nc + LEFT_CHUNKS, CHUNK, B_HEADS, B_DIM)
    vp = jnp.pad(v, pad).reshape(bn, nc + LEFT_CHUNKS, CHUNK, B_HEADS, B_DIM)
    qc = q.reshape(bn, nc, CHUNK, B_HEADS, B_DIM)
    idx = jnp.arange(nc)[:, None] + jnp.arange(LEFT_CHUNKS + 1)[None, :]
    bias = rel_bias_matrix(table, jnp.arange(CHUNK), jnp.arange(BAND) - B_REACH)
    kpos = jnp.arange(nc)[:, None] * CHUNK - B_REACH + jnp.arange(BAND)[None, :]
    valid = (kpos >= 0)[:, None, None, :]

    def one(args):
        qi, ki, vi = args
        kb = ki[idx].reshape(nc, BAND, B_HEADS, B_DIM)
        vb = vi[idx].reshape(nc, BAND, B_HEADS, B_DIM)
        return band_attend(qi, kb, vb, bias, valid)

    o = lax.map(one, (qc, kp, vp))
    return o.reshape(bn, s, B_HEADS, B_DIM)


def mixer_prompt(qa, ka, va, qb, kb, vb, lam, rel_table):
    bn, s = qa.shape[0], qa.shape[1]
    oa = diff_attn_prompt(qa, ka, va, lam)
    ob = chunk_band_prompt(qb, kb, vb, rel_table)
    keep = min(B_REACH, s)
    return oa, ob, (ka.reshape(bn, s, A_HEADS, 2 * A_DIM), va, kb[:, s - keep:], vb[:, s - keep:])


def mixer_sample(qa, ka, va, qb, kb, vb, lam, rel_table, cache_ak, cache_av, cache_bk, cache_bv):
    bn, t = qa.shape[0], qa.shape[1]
    p_len = cache_ak.shape[1]
    k_all = jnp.concatenate([cache_ak.reshape(bn, p_len, A_HEADS, 2, A_DIM), ka], axis=1)
    v_all = jnp.concatenate([cache_av, va], axis=1)
    oa = diff_attend(qa, k_all, v_all, lam, None)
    wb = cache_bk.shape[1]
    kb_all = jnp.concatenate([cache_bk, kb], axis=1)
    vb_all = jnp.concatenate([cache_bv, vb], axis=1)
    qpos = PAST_LEN + jnp.arange(t)
    kpos = PAST_LEN - wb + jnp.arange(wb + t)
    ob = band_attend(qb, kb_all, vb_all, rel_bias_matrix(rel_table, qpos, kpos), None)
    return oa, ob, (ka.reshape(bn, t, A_HEADS, 2 * A_DIM), va, kb_all[:, t:], vb_all[:, t:])


def peer_ffn(h, wq, sub_keys, u_tab, v_tab):
    n = h.shape[0]
    nb = -(-n // PEER_BLOCK)
    hp = jnp.pad(h, ((0, nb * PEER_BLOCK - n), (0, 0))).reshape(nb, PEER_BLOCK, D_MODEL)

    def one(hb):
        q = (hb @ wq).reshape(PEER_BLOCK, PK_HEADS, 2, PK_HALF)
        s = jnp.einsum('thmk,hmnk->thmn', q, sub_keys)
        sv, si = lax.top_k(s, PK_TOPK)
        cand = sv[..., 0, :, None] + sv[..., 1, None, :]
        cidx = si[..., 0, :, None] * N_KEYS + si[..., 1, None, :]
        cv, ci = lax.top_k(cand.reshape(PEER_BLOCK, PK_HEADS, PK_TOPK * PK_TOPK), PK_TOPK)
        eidx = jnp.take_along_axis(cidx.reshape(PEER_BLOCK, PK_HEADS, PK_TOPK * PK_TOPK), ci, axis=-1)
        g = jax.nn.softmax(cv.astype(jnp.float32), axis=-1)
        u = jnp.take(u_tab, eidx, axis=0)
        a = jax.nn.gelu(jnp.einsum('td,thkd->thk', hb, u))
        w = (g * a.astype(jnp.float32)).astype(hb.dtype)
        return jnp.einsum('thk,thkd->td', w, jnp.take(v_tab, eidx, axis=0))

    out = lax.map(one, hp).reshape(nb * PEER_BLOCK, D_MODEL)
    return out[:n]


def layer(x, c, pos, mixer, lam_init, w_ada, b_ada, g_attn, g_ffn, w_in, a_gain, b_gain, w_out,
          pk_wq, pk_keys, pk_u, pk_v):
    bn, L = x.shape[0], x.shape[1]
    mod = (jax.nn.silu(c) @ w_ada + b_ada)[:, None, :]
    sh1, sc1, gt1, sh2, sc2, gt2 = jnp.split(mod, 6, axis=-1)
    h = rms_norm(x, g_attn) * (1 + sc1) + sh1
    qa, ka, va, qb, kb, vb = jnp.split(h @ w_in, IN_SPLITS, axis=-1)
    qa = rope_partial(qa.reshape(bn, L, A_HEADS, 2, A_DIM), pos)
    ka = rope_partial(ka.reshape(bn, L, A_HEADS, 2, A_DIM), pos)
    va = va.reshape(bn, L, A_HEADS, 2 * A_DIM)
    qb = qb.reshape(bn, L, B_HEADS, B_DIM)
    kb = kb.reshape(bn, L, B_HEADS, B_DIM)
    vb = vb.reshape(bn, L, B_HEADS, B_DIM)
    oa, ob, state = mixer(qa, ka, va, qb, kb, vb)
    oa = (rms_norm(oa, a_gain) * (1.0 - lam_init)).reshape(bn, L, A_WIDTH)
    ob = rms_norm(ob.reshape(bn, L, B_WIDTH), b_gain)
    x = x + gt1 * (jnp.concatenate([oa, ob], axis=-1) @ w_out)
    h = rms_norm(x, g_ffn) * (1 + sc2) + sh2
    f = peer_ffn(h.reshape(bn * L, D_MODEL), pk_wq, pk_keys, pk_u, pk_v).reshape(bn, L, D_MODEL)
    x = x + gt2 * f
    return x, state


def setup_inputs(seed: int = 0) -> dict:
    key = jax.random.key(seed)
    ks = jax.random.split(key, 32)
    nrm = lambda k, shape, s: jax.random.normal(k, shape, jnp.float32) * s
    wb = min(B_REACH, PAST_LEN)
    return {
        'x_prompt': nrm(ks[0], (BATCH, SEQ, D_MODEL), 1.0),
        'x_sample': nrm(ks[1], (DEC_BATCH, DEC_SEQ, D_MODEL), 1.0),
        'c_prompt': nrm(ks[2], (BATCH, D_MODEL), 1.0),
        'c_sample': nrm(ks[3], (DEC_BATCH, D_MODEL), 1.0),
        'cache_a_k': nrm(ks[4], (DEPTH, DEC_BATCH, PAST_LEN, A_HEADS, 2 * A_DIM), 1.0),
        'cache_a_v': nrm(ks[5], (DEPTH, DEC_BATCH, PAST_LEN, A_HEADS, 2 * A_DIM), 1.0),
        'cache_b_k': nrm(ks[6], (DEPTH, DEC_BATCH, wb, B_HEADS, B_DIM), 1.0),
        'cache_b_v': nrm(ks[7], (DEPTH, DEC_BATCH, wb, B_HEADS, B_DIM), 1.0),
        'w_ada': nrm(ks[8], (DEPTH, D_MODEL, 6 * D_MODEL), 0.5 * D_MODEL ** -0.5),
        'b_ada': nrm(ks[9], (DEPTH, 6 * D_MODEL), 0.02),
        'g_attn': 1.0 + nrm(ks[10], (DEPTH, D_MODEL), 0.05),
        'g_ffn': 1.0 + nrm(ks[11], (DEPTH, D_MODEL), 0.05),
        'w_in': nrm(ks[12], (DEPTH, D_MODEL, IN_WIDTH), D_MODEL ** -0.5),
        'lam_q1': nrm(ks[13], (DEPTH, A_DIM), 0.1),
        'lam_k1': nrm(ks[14], (DEPTH, A_DIM), 0.1),
        'lam_q2': nrm(ks[15], (DEPTH, A_DIM), 0.1),
        'lam_k2': nrm(ks[16], (DEPTH, A_DIM), 0.1),
        'a_gain': 1.0 + nrm(ks[17], (DEPTH, 2 * A_DIM), 0.05),
        'rel_bias': nrm(ks[18], (DEPTH, B_HEADS, N_REL), 0.5),
        'b_gain': 1.0 + nrm(ks[19], (DEPTH, B_WIDTH), 0.05),
        'w_out': nrm(ks[20], (DEPTH, MIX_WIDTH, D_MODEL), MIX_WIDTH ** -0.5),
        'pk_wq': nrm(ks[21], (DEPTH, D_MODEL, PK_HEADS * PK_QDIM), D_MODEL ** -0.5),
        'pk_keys': nrm(ks[22], (DEPTH, PK_HEADS, 2, N_KEYS, PK_HALF), PK_HALF ** -0.5),
        'pk_u': nrm(ks[23], (DEPTH, N_EXPERTS, D_MODEL), D_MODEL ** -0.5),
        'pk_v': nrm(ks[24], (DEPTH, N_EXPERTS, D_MODEL), 0.3),
        'g_final': 1.0 + nrm(ks[25], (D_MODEL,), 0.05),
    }


def reference(x_prompt, x_sample, c_prompt, c_sample, cache_a_k, cache_a_v, cache_b_k, cache_b_v,
              w_ada, b_ada, g_attn, g_ffn, w_in, lam_q1, lam_k1, lam_q2, lam_k2, a_gain, rel_bias,
              b_gain, w_out, pk_wq, pk_keys, pk_u, pk_v, g_final):
    s_len = x_prompt.shape[1]
    t_len = x_sample.shape[1]
    pos_p = jnp.arange(s_len, dtype=jnp.int32)
    pos_s = PAST_LEN + jnp.arange(t_len, dtype=jnp.int32)
    xp, xs = x_prompt, x_sample
    st_p, st_s = [], []
    for l in range(DEPTH):
        lam_init = 0.8 - 0.6 * math.exp(-0.3 * l)
        lam = diff_lambda(lam_q1[l], lam_k1[l], lam_q2[l], lam_k2[l], lam_init)
        weights = (w_ada[l], b_ada[l], g_attn[l], g_ffn[l], w_in[l], a_gain[l], b_gain[l], w_out[l],
                   pk_wq[l], pk_keys[l], pk_u[l], pk_v[l])
        mix_p = functools.partial(mixer_prompt, lam=lam, rel_table=rel_bias[l])
        mix_s = functools.partial(mixer_sample, lam=lam, rel_table=rel_bias[l],
                                  cache_ak=cache_a_k[l], cache_av=cache_a_v[l],
                                  cache_bk=cache_b_k[l], cache_bv=cache_b_v[l])
        xp, sp = layer(xp, c_prompt, pos_p, mix_p, lam_init, *weights)
        xs, ss = layer(xs, c_sample, pos_s, mix_s, lam_init, *weights)
        st_p.append(sp)
        st_s.append(ss)
    y_prompt = rms_norm(xp, g_final)
    y_sample = rms_norm(xs, g_final)
    new_a_k_prompt = jnp.stack([s[0] for s in st_p])
    new_a_v_prompt = jnp.stack([s[1] for s in st_p])
    new_b_k_prompt = jnp.stack([s[2] for s in st_p])
    new_b_v_prompt = jnp.stack([s[3] for s in st_p])
    new_a_k_sample = jnp.stack([s[0] for s in st_s])
    new_a_v_sample = jnp.stack([s[1] for s in st_s])
    new_b_k_sample = jnp.stack([s[2] for s in st_s])
    new_b_v_sample = jnp.stack([s[3] for s in st_s])
    return (y_prompt, y_sample, new_a_k_prompt, new_a_v_prompt, new_b_k_prompt, new_b_v_prompt,
            new_a_k_sample, new_a_v_sample, new_b_k_sample, new_b_v_sample)
```

```python
import os
import math
import numpy as np
from contextlib import ExitStack
import concourse.bass as bass
import concourse.mybir as mybir
from concourse.bass_utils import run_bass_kernel_spmd

F32 = mybir.dt.float32
BF16 = mybir.dt.bfloat16
U32 = mybir.dt.uint32
AF = mybir.ActivationFunctionType
ALU = mybir.AluOpType
AX = mybir.AxisListType

D = 1024
SEQ = 8192
NS = 64
T = SEQ + NS
DEPTH = 2
PAST = 4096
EPS = 1e-6
NEXP = 16384
NEG = -30000.0


class Unit:
    __slots__ = ("name", "w", "r", "excl")

    def __init__(self, name):
        self.name = name
        self.w = None
        self.r = {}
        self.excl = False


class Op:
    __slots__ = ("eng", "fn", "deps", "inc", "cnt", "dma")


ENGS = ("pe", "act", "dve", "pool", "sp")


class Prog:
    def __init__(self, nc, es):
        self.nc = nc
        self.es = es
        self.ops = {e: [] for e in ENGS}
        self.eng_sem = {e: es.enter_context(nc.semaphore("s_" + e)) for e in ENGS}
        self.dma_sem = {}
        self.dma_cnt = {}
        self.nunits = 0

    def unit(self, name=None):
        self.nunits += 1
        return Unit(name or "u%d" % self.nunits)

    def _dep(self, op, d, kind):
        if d is op:
            return
        if d.dma is None and op.dma is None and d.eng == op.eng and kind != "raw":
            return
        op.deps.append(d)
        if d.dma is None:
            d.inc = True

    def add(self, eng, fn, reads=(), writes=(), dma=None):
        op = Op()
        op.eng = eng
        op.fn = fn
        op.deps = []
        op.inc = False
        op.cnt = 0
        op.dma = dma
        ex = [u for u in reads if u.excl]
        if ex:
            writes = list(writes) + [u for u in ex if u not in writes]
        for u in reads:
            if u.w is not None:
                self._dep(op, u.w, "raw")
        for u in writes:
            if u.w is not None:
                self._dep(op, u.w, "waw")
            for r in u.r.values():
                self._dep(op, r, "war")
        key = dma if dma is not None else eng
        for u in reads:
            u.r[key] = op
        for u in writes:
            u.w = op
            u.r = {}
        if dma is not None:
            if dma not in self.dma_sem:
                self.dma_sem[dma] = self.es.enter_context(self.nc.semaphore("d_" + dma))
                self.dma_cnt[dma] = 0
            self.dma_cnt[dma] += 16
            op.cnt = self.dma_cnt[dma]
        self.ops[eng].append(op)
        return op

    def _sem(self, d):
        return self.dma_sem[d.dma] if d.dma is not None else self.eng_sem[d.eng]

    def barrier(self):
        if os.environ.get("MK_NOBAR"):
            return
        deps = []
        for e in ENGS:
            for op in reversed(self.ops[e]):
                if op.dma is None and op.fn is not None:
                    deps.append(op)
                    break
        lastd = {}
        for e in ENGS:
            for op in self.ops[e]:
                if op.dma is not None:
                    lastd[op.dma] = op
        deps += list(lastd.values())
        for e in ENGS:
            op = Op()
            op.eng = e
            op.fn = None
            op.deps = []
            op.inc = False
            op.cnt = 0
            op.dma = None
            for d in deps:
                if d.dma is None and d.eng == e:
                    continue
                op.deps.append(d)
                if d.dma is None:
                    d.inc = True
            self.ops[e].append(op)

    def emit(self):
        nc = self.nc
        for e in ENGS:
            c = 0
            for op in self.ops[e]:
                if op.dma is None and op.inc:
                    c += 1
                    op.cnt = c
        final = [(self.dma_sem[k], v) for k, v in self.dma_cnt.items()]

        def run(ename, eng):
            waited = {}
            for op in self.ops[ename]:
                need = {}
                for d in op.deps:
                    s = self._sem(d)
                    k = id(s)
                    if k not in need or need[k][1] < d.cnt:
                        need[k] = (s, d.cnt)
                for k, (s, c) in need.items():
                    if waited.get(k, 0) < c:
                        eng.wait_ge(s, c)
                        waited[k] = c
                if op.fn is None:
                    continue
                inst = op.fn(eng)
                if op.dma is not None:
                    inst.then_inc(self.dma_sem[op.dma], 16)
                elif op.inc:
                    inst.then_inc(self.eng_sem[ename], 1)
            if ename == "sp":
                for s, c in final:
                    eng.wait_ge(s, c)

        with nc.Block() as block:
            @block.tensor
            def _(e):
                run("pe", e)

            @block.scalar
            def _(e):
                run("act", e)

            @block.vector
            def _(e):
                run("dve", e)

            @block.gpsimd
            def _(e):
                run("pool", e)

            @block.sync
            def _(e):
                run("sp", e)


class Tl:
    def __init__(self, pg, t, nunits=1):
        self.t = t
        self.u = [pg.unit() for _ in range(nunits)]

    def __getitem__(self, k):
        return self.t[k]


def build(stop=None):
    nc = bass.Bass("TRN2", target_bir_lowering=False)
    es = ExitStack()
    pg = Prog(nc, es)
    stop = stop or os.environ.get("MK_STOP", "")

    def din(name, shape, dt=F32):
        return nc.dram_tensor(name, list(shape), dt, kind="ExternalInput").ap()

    def dout(name, shape, dt=F32):
        return nc.dram_tensor(name, list(shape), dt, kind="ExternalOutput").ap()

    def dscr(name, shape, dt):
        return nc.dram_tensor(name, list(shape), dt).ap()

    IN_SHAPES = {
        "xin": [T, D], "cT": [128, 8, 5], "cak": [DEPTH, 4, PAST, 512], "cav": [DEPTH, 4, PAST, 512],
        "cbk": [DEPTH, 4, 512, 512], "cbv": [DEPTH, 4, 512, 512], "w_ada": [DEPTH, D, 6 * D],
        "b_adaT": [DEPTH, 128, 48], "g_attnT": [DEPTH, 128, 8], "g_ffnT": [DEPTH, 128, 8],
        "w_in": [DEPTH, D, 3072], "lamv": [DEPTH, 256], "a_gain": [DEPTH, 128],
        "biasT": [DEPTH, 8, 2, 128, 128], "bconst": [DEPTH, 8, 128], "b_gain": [DEPTH, 512],
        "w_out": [DEPTH, D, D], "pk_wq": [DEPTH, D, 2048], "pk_keys": [DEPTH, 16, 128, 128],
        "pk_u": [DEPTH, NEXP, D], "pk_v": [DEPTH, NEXP, D], "g_final": [1, D], "ident": [128, 128],
        "ropec": [T, 8], "ropes": [T, 8], "iota_in": [128, 128],
    }
    _ins = {}

    class _IN:
        def __getattr__(self, name):
            if name not in _ins:
                _ins[name] = din(name, IN_SHAPES[name])
            return _ins[name]

    I = _IN()
    build.used_inputs = _ins

    y_out = dout("y", [T, D])
    nak = dout("nak", [DEPTH, T, 512])
    nav = dout("nav", [DEPTH, T, 512])
    nbk_p = dout("nbk_p", [DEPTH, 512, 512])
    nbv_p = dout("nbv_p", [DEPTH, 512, 512])
    nbk_s = dout("nbk_s", [DEPTH, 4, 512, 512])
    nbv_s = dout("nbv_s", [DEPTH, 4, 512, 512])

    xT = dscr("xT", [8, 128, T], F32)
    QTA = dscr("QTA", [4, 128, T], BF16)
    KTA = dscr("KTA", [4, 128, T], BF16)
    VA = dscr("VA", [4, T, 136], BF16)
    QTB = dscr("QTB", [4, 128, T], BF16)
    KTB = dscr("KTB", [4, 128, T], BF16)
    VB = dscr("VB", [T, 8 * 72], BF16)
    UT = dscr("UT", [8, 128, NEXP], BF16)
    VBF = dscr("VBF", [NEXP, D], BF16)
    H2T = dscr("H2T", [8, 128, T], BF16)
    RT = dscr("RT", [3, 128, T], F32)
    u_xT = pg.unit("xT")
    u_qk = pg.unit("qkscr")
    u_uv = pg.unit("uvscr")
    u_h2 = pg.unit("h2scr")
    u_rt = pg.unit("rtscr")
    u_out = pg.unit("outs")

    sbn = [0]

    def sb(name, shape, dt=F32, nunits=1, st=None):
        sbn[0] += 1
        return Tl(pg, (st or es).enter_context(nc.sbuf_tensor("%s_%d" % (name, sbn[0]), list(shape), dt)), nunits)

    banks = [Tl(pg, es.enter_context(nc.psum_tensor("bank%d" % i, [128, 512], F32))) for i in range(8)]

    for b_ in banks:
        b_.u[0].excl = True

    def bk_bf(i):
        return banks[i].t[:].bitcast(BF16)

    PAR_KEYS = {"wbig", "wq", "wout", "biasb"}
    keyunit = {}

    def dma(eng, key, out, in_, reads, writes):
        if key not in PAR_KEYS:
            if key not in keyunit:
                keyunit[key] = pg.unit("k_" + key)
            writes = list(writes) + [keyunit[key]]
        pg.add(eng, lambda e: e.dma_start(out=out, in_=in_), reads, writes, dma=key)

    def mm(out, lhsT, rhs, start, stop, reads, writes):
        pg.add("pe", lambda e: e.matmul(out, lhsT, rhs, start=start, stop=stop), reads, writes)

    def tr(out, in_, idn, reads, writes):
        pg.add("pe", lambda e: e.transpose(out, in_, idn), reads, writes)

    def act(out, in_, func, reads, writes, bias=None, scale=None, accum=None):
        def f(e):
            kw = {}
            if bias is not None:
                kw["bias"] = bias
            if scale is not None:
                kw["scale"] = scale
            if accum is not None:
                kw["accum_out"] = accum
            return e.activation(out=out, in_=in_, func=func, **kw)
        pg.add("act", f, reads, writes)

    def V(eng, fn, reads, writes):
        pg.add(eng, fn, reads, writes)

    def tt(eng, out, a, b, op, reads, writes):
        pg.add(eng, lambda e: e.tensor_tensor(out, a, b, op), reads, writes)

    def ts(eng, out, a, s1, op0, reads, writes, s2=None, op1=None):
        if op1 is None:
            pg.add(eng, lambda e: e.tensor_scalar(out, a, s1, None, op0), reads, writes)
        else:
            pg.add(eng, lambda e: e.tensor_scalar(out, a, s1, s2, op0, op1), reads, writes)

    def stt(out, a, s, b, op0, op1, reads, writes):
        pg.add("dve", lambda e: e.scalar_tensor_tensor(out, a, s, b, op0, op1), reads, writes)

    def vmax(out, in_, reads, writes):
        pg.add("dve", lambda e: e.max(out=out, in_=in_), reads, writes)

    def vmaxidx(out, in_max, in_values, reads, writes):
        pg.add("dve", lambda e: e.max_index(out=out, in_max=in_max, in_values=in_values), reads, writes)

    def vmatch(out, rep, vals, reads, writes):
        pg.add("dve", lambda e: e.match_replace(out=out, in_to_replace=rep, in_values=vals, imm_value=-1e30),
               reads, writes)

    def vredsum(out, in_, reads, writes):
        pg.add("dve", lambda e: e.reduce_sum(out, in_, AX.X), reads, writes)

    def vrecip(out, in_, reads, writes):
        pg.add("dve", lambda e: e.reciprocal(out, in_), reads, writes)

    def cp(eng, out, in_, reads, writes):
        if eng == "act":
            pg.add("act", lambda e: e.copy(out, in_), reads, writes)
        else:
            pg.add(eng, lambda e: e.tensor_copy(out, in_), reads, writes)

    def memset(eng, ap, val, writes):
        pg.add(eng, lambda e: e.memset(ap, val), (), writes)

    ident_f = sb("ident_f", [128, 128])
    ident_b = sb("ident_b", [128, 128], BF16)
    ones_b = sb("ones_b", [128, 128], BF16)
    iota_b = sb("iota_b", [128, 128], BF16)
    iota16 = sb("iota16", [128, 16])
    mk = sb("mk", [128, 5, 128], BF16)
    epsc = sb("epsc", [128, 1])
    dma("sp", "c0", ident_f[:], I.ident[:, :], (), ident_f.u)
    dma("pool", "c1", ident_b[:], I.ident[:, :], (), ident_b.u)
    dma("pool", "c1", iota_b[:], I.iota_in[:, :], (), iota_b.u)
    dma("sp", "c0", iota16[:], I.iota_in[:, 0:16], (), iota16.u)
    memset("dve", ones_b[:], 1.0, ones_b.u)
    memset("dve", epsc[:], EPS, epsc.u)
    memset("dve", mk[:], 0.0, mk.u)
    memset("dve", mk[0:1, 0, 0:64], 0.0, mk.u)
    memset("dve", mk[0:1, 0, 64:128], 1.0, mk.u)
    memset("dve", mk[0:1, 1, 0:64], NEG, mk.u)
    memset("dve", mk[0:1, 1, 64:128], 0.0, mk.u)
    memset("dve", mk[0:1, 2, 0:64], 1.0, mk.u)
    memset("dve", mk[0:1, 2, 64:128], 0.0, mk.u)
    memset("dve", mk[0:1, 3, 0:64], 0.0, mk.u)
    memset("dve", mk[0:1, 3, 64:128], NEG, mk.u)
    memset("dve", mk[0:1, 4, :], 1.0, mk.u)

    NBLK = int(os.environ.get("MK_NB", "0"))
    DBG = bool(os.environ.get("MK_DBG"))
    dbg_xT = dout("dbg_xT", [8, 128, T]) if DBG else None
    dbg_RT = dout("dbg_RT", [3, 128, T]) if DBG else None

    def stage(name):
        return stop == name

    def finish():
        if DBG:
            pg.barrier()
            dma("sp", "dbg", dbg_xT[:, :, :], xT[:, :, :], (), ())
            dma("sp", "dbg", dbg_RT[:, :, :], RT[:, :, :], (), ())
        pg.emit()
        return nc, es

    modT = [sb("modT%d" % l, [128, 48, 5]) for l in range(DEPTH)]
    A1 = [sb("A1_%d" % l, [128, 8, 5]) for l in range(DEPTH)]
    A2 = [sb("A2_%d" % l, [128, 8, 5]) for l in range(DEPTH)]
    lam = sb("lam", [128, DEPTH, 4])
    gainA = sb("gainA", [128, DEPTH, 128])
    gainB = sb("gainB", [128, DEPTH, 512])
    with ExitStack() as ph:
        scT = sb("scT", [128, 8, 5], st=ph)
        dma("sp", "c0", scT[:], I.cT[:, :, :], (), scT.u)
        act(scT[:], scT[:], AF.Silu, scT.u, scT.u)
        badaT = sb("badaT", [128, DEPTH, 48], st=ph)
        gaT = sb("gaT", [128, DEPTH, 8], st=ph)
        gfT = sb("gfT", [128, DEPTH, 8], st=ph)
        for l in range(DEPTH):
            dma("sp", "c0", badaT[:, l, :], I.b_adaT[l], (), badaT.u)
            dma("sp", "c0", gaT[:, l, :], I.g_attnT[l], (), gaT.u)
            dma("sp", "c0", gfT[:, l, :], I.g_ffnT[l], (), gfT.u)
        wada = [sb("wada%d" % i, [128, 8, 512], st=ph) for i in range(2)]
        gi = 0
        for l in range(DEPTH):
            for g in range(12):
                wt = wada[gi % 2]
                gi += 1
                dma("sp", "wada%d" % (gi % 2), wt[:],
                    I.w_ada[l][:, g * 512:(g + 1) * 512].rearrange("(k p) f -> p k f", p=128), (), wt.u)
                for j in range(4):
                    ch = g * 4 + j
                    for k in range(8):
                        mm(banks[7].t[:, ch * 5:ch * 5 + 5], wt[:, k, j * 128:(j + 1) * 128], scT[:, k, :],
                           k == 0, k == 7, wt.u + scT.u, banks[7].u)
            tt("dve", modT[l][:], banks[7].t[:, 0:240].rearrange("p (c r) -> p c r", r=5),
               badaT[:, l, :].unsqueeze(2).to_broadcast([128, 48, 5]), ALU.add, banks[7].u + badaT.u, modT[l].u)
            stt(A1[l][:], modT[l][:, 8:16, :], 1.0, gaT[:, l, :].unsqueeze(2).to_broadcast([128, 8, 5]), ALU.add,
                ALU.mult, modT[l].u + gaT.u, A1[l].u)
            stt(A2[l][:], modT[l][:, 32:40, :], 1.0, gfT[:, l, :].unsqueeze(2).to_broadcast([128, 8, 5]), ALU.add,
                ALU.mult, modT[l].u + gfT.u, A2[l].u)
        lamsrc = sb("lamsrc", [128, DEPTH, 256], st=ph)
        lamt = sb("lamt", [128, 64], st=ph)
        for l in range(DEPTH):
            dma("sp", "c0", lamsrc[:, l, :], I.lamv[l:l + 1, :].partition_broadcast(128), (), lamsrc.u)
            dma("sp", "c0", gainA[:, l, :], I.a_gain[l:l + 1, :].partition_broadcast(128), (), gainA.u)
            dma("sp", "c0", gainB[:, l, :], I.b_gain[l:l + 1, :].partition_broadcast(128), (), gainB.u)
        for l in range(DEPTH):
            lam_init = 0.8 - 0.6 * math.exp(-0.3 * l)
            for i in range(2):
                tt("dve", lamt[:], lamsrc[:, l, i * 128:i * 128 + 64], lamsrc[:, l, i * 128 + 64:i * 128 + 128],
                   ALU.mult, lamsrc.u, lamt.u)
                V("dve", lambda e, l=l, i=i: e.reduce_sum(lam[:, l, i:i + 1], lamt[:], AX.X), lamt.u, lam.u)
            act(lam[:, l, 0:2], lam[:, l, 0:2], AF.Exp, lam.u, lam.u)
            tt("dve", lam[:, l, 2:3], lam[:, l, 1:2], lam[:, l, 0:1], ALU.subtract, lam.u, lam.u)
            ts("dve", lam[:, l, 2:3], lam[:, l, 2:3], -lam_init, ALU.add, lam.u, lam.u)
            ts("dve", gainA[:, l, :], gainA[:, l, :], 1.0 - lam_init, ALU.mult, gainA.u, gainA.u)
        pg.barrier()

    def segs(c0, n):
        out = []
        if c0 < SEQ:
            out.append((0, min(n, SEQ - c0), 0))
        for s in range(4):
            lo, hi = SEQ + 16 * s, SEQ + 16 * s + 16
            a, b = max(lo, c0), min(hi, c0 + n)
            if a < b:
                out.append((a - c0, b - c0, 1 + s))
        return out

    with ExitStack() as ph:
        xld = [sb("xld%d" % i, [128, D], st=ph) for i in range(2)]
        xtt = [sb("xtt%d" % i, [128, 8, 128], st=ph) for i in range(2)]
        ntile = (T + 127) // 128
        for ti in range(ntile):
            r0 = ti * 128
            m = min(128, T - r0)
            xl = xld[ti % 2]
            xo = xtt[ti % 2]
            dma("sp", "xld%d" % (ti % 2), xl[0:m, :], I.xin[r0:r0 + m, :], (), xl.u)
            for hf in range(2):
                b = banks[(ti * 2 + hf) % 4]
                for kk in range(4):
                    k = hf * 4 + kk
                    tr(b.t[:, kk * 128:kk * 128 + m], xl[0:m, k * 128:(k + 1) * 128], ident_f[0:m, 0:m],
                       xl.u + ident_f.u, b.u)
                cp("act" if hf == 0 else "dve", xo[:, hf * 4:(hf + 1) * 4, 0:m],
                   b.t[:, :].rearrange("p (k t) -> p k t", t=128)[:, :, 0:m], b.u, xo.u)
            dma("sp", "xtt%d" % (ti % 2), xT[:, :, r0:r0 + m].rearrange("k p t -> p k t"), xo[:, :, 0:m], xo.u, ())
        pg.barrier()
    if stage("p0"):
        return finish()

    xb = [sb("xb%d" % i, [128, 8, 512]) for i in range(2)]
    xbi = [0]

    def load_xb(c0, n):
        x = xb[xbi[0] % 2]
        xbi[0] += 1
        x.key = "xb%d" % (xbi[0] % 2)
        dma("sp", x.key, x[:, :, 0:n], xT[:, :, c0:c0 + n].rearrange("k p t -> p k t"), (), x.u)
        return x

    def store_xb(x, c0, n):
        dma("sp", x.key, xT[:, :, c0:c0 + n].rearrange("k p t -> p k t"), x[:, :, 0:n], x.u, ())

    class NB:
        def __init__(self, ph, name):
            self.sqb = sb(name + "sqb", [128, 8, 512], BF16, st=ph)
            self.rstd = sb(name + "rstd", [128, 512], st=ph)
            self.xn = sb(name + "xn", [128, 8, 512], st=ph)
            self.h = sb(name + "h", [128, 8, 512], BF16, st=ph)

    def norm_mod(nb, x, c0, n, Aq, Bq, boff, bank):
        sqb, rstd, xn, dst = nb.sqb, nb.rstd, nb.xn, nb.h
        act(sqb[:, :, 0:n], x[:, :, 0:n], AF.Square, x.u, sqb.u)
        for k in range(8):
            mm(bank.t[:, 0:n], ones_b[:], sqb[:, k, 0:n], k == 0, k == 7, ones_b.u + sqb.u, bank.u)
        act(rstd[:, 0:n], bank.t[:, 0:n], AF.Sqrt, bank.u + epsc.u, rstd.u, bias=epsc[:, 0:1], scale=1.0 / D)
        V("dve", lambda e: e.reciprocal(rstd[:, 0:n], rstd[:, 0:n]), rstd.u, rstd.u)
        tt("dve", xn[:, :, 0:n], x[:, :, 0:n], rstd[:, 0:n].unsqueeze(1).to_broadcast([128, 8, n]), ALU.mult,
           x.u + rstd.u, xn.u)
        for (lo, hi, r) in segs(c0, n):
            for k in range(8):
                act(dst[:, k, lo:hi], xn[:, k, lo:hi], AF.Identity, xn.u + Aq.u + Bq.u, dst.u,
                    bias=Bq[:, boff + k, r:r + 1], scale=Aq[:, k, r:r + 1])

    pblocks = [(c, 512) for c in range(0, SEQ, 512)]
    if NBLK:
        pblocks = pblocks[:NBLK] + pblocks[-1:]
    blocks = pblocks + [(SEQ, NS)]

    def p1(l):
        with ExitStack() as ph:
            nb = NB(ph, "p1")
            hT = nb.h
            wbig = sb("wbig", [128, 8, 3072], BF16, nunits=24, st=ph)
            qkf = [sb("qkf%d" % i, [128, 512], st=ph) for i in range(2)]
            qkb = [sb("qkb%d" % i, [128, 512], BF16, st=ph) for i in range(2)]
            vf = [sb("vf%d" % i, [128, 512], st=ph) for i in range(3)]
            vab = [sb("vab%d" % i, [128, 4, 136], BF16, st=ph) for i in range(2)]
            vbb = [sb("vbb%d" % i, [128, 8, 72], BF16, st=ph) for i in range(2)]
            for i in range(2):
                memset("pool", vab[i][:, :, 128:136], 1.0, vab[i].u)
                memset("pool", vbb[i][:, :, 64:72], 1.0, vbb[i].u)
            qkT = [sb("qkT%d" % i, [128, 4, 128], BF16, st=ph) for i in range(2)]
            qkbT = [sb("qkbT%d" % i, [128, 512], BF16, st=ph) for i in range(2)]
            rc = [sb("rc%d" % i, [128, 2, 8], st=ph) for i in range(2)]
            rtmp = sb("rtmp", [128, 4, 8, 8], st=ph)
            for k in range(8):
                for c3 in range(3):
                    dma("pool", "wbig", wbig[:, k, c3 * 1024:(c3 + 1) * 1024],
                        I.w_in[l][k * 128:(k + 1) * 128, c3 * 1024:(c3 + 1) * 1024], (), [wbig.u[k * 3 + c3]])
            cnt = 0
            for (c0, n) in blocks:
                x = load_xb(c0, n)
                norm_mod(nb, x, c0, n, A1[l], modT[l], 0, banks[7])
                outblk = (c0 == SEQ - 512) or (c0 == SEQ)
                for which, col0, scr, scl in ((0, 1536, QTB, 0.125), (1, 2048, KTB, 1.0)):
                    for cc in range(4):
                        b = banks[cnt % 2]
                        o = qkbT[cnt % 2]
                        cnt += 1
                        for k in range(8):
                            mm(b.t[:, 0:n], wbig[:, k, col0 + cc * 128:col0 + (cc + 1) * 128], hT[:, k, 0:n],
                               k == 0, k == 7, wbig.u + hT.u, b.u)
                        act(o[:, 0:n], b.t[:, 0:n], AF.Copy, b.u, o.u, scale=scl)
                        dma("sp", "qkbT%d" % ((cnt - 1) % 2), scr[cc, :, c0:c0 + n], o[:, 0:n], o.u, ())
                for st in range((n + 127) // 128):
                    m = min(128, n - st * 128)
                    r0 = c0 + st * 128
                    rcs = rc[st % 2]
                    dma("sp", "rc%d" % (st % 2), rcs[0:m, 0, :], I.ropec[r0:r0 + m, :], (), rcs.u)
                    dma("sp", "rc%d" % (st % 2), rcs[0:m, 1, :], I.ropes[r0:r0 + m, :], (), rcs.u)
                    for which, col0, scr, scl in ((0, 0, QTA, 0.125), (1, 512, KTA, 1.0)):
                        b = banks[2 + which]
                        f = qkf[which]
                        bb = qkb[which]
                        for k in range(8):
                            mm(b.t[0:m, :], hT[:, k, st * 128:st * 128 + m], wbig[:, k, col0:col0 + 512],
                               k == 0, k == 7, wbig.u + hT.u, b.u)
                        act(f[0:m, :], b.t[0:m, :], AF.Copy, b.u, f.u, scale=scl)
                        fv = f[0:m, :].rearrange("p (g e) -> p g e", e=64)
                        cosb = rcs[0:m, 0, :].unsqueeze(1).to_broadcast([m, 8, 8])
                        sinb = rcs[0:m, 1, :].unsqueeze(1).to_broadcast([m, 8, 8])
                        x1 = fv[:, :, 0:8]
                        x2 = fv[:, :, 8:16]
                        ru = rtmp.u + f.u + rcs.u
                        tt("pool", rtmp[0:m, 0], x1, cosb, ALU.mult, f.u + rcs.u, rtmp.u)
                        tt("pool", rtmp[0:m, 1], x2, sinb, ALU.mult, f.u + rcs.u, rtmp.u)
                        tt("pool", rtmp[0:m, 2], x2, cosb, ALU.mult, f.u + rcs.u, rtmp.u)
                        tt("pool", rtmp[0:m, 3], x1, sinb, ALU.mult, f.u + rcs.u, rtmp.u)
                        tt("pool", x1, rtmp[0:m, 0], rtmp[0:m, 1], ALU.subtract, ru, f.u)
                        tt("pool", x2, rtmp[0:m, 2], rtmp[0:m, 3], ALU.add, ru, f.u)
                        if which == 1:
                            dma("sp", "qkf1", nak[l, r0:r0 + m, :], f[0:m, :], f.u, ())
                        cp("dve", bb[0:m, :], f[0:m, :], f.u, bb.u)
                        tb = banks[4 + which]
                        o = qkT[which]
                        for h in range(4):
                            tr(tb.t[:, :].bitcast(BF16)[:, h * 128:h * 128 + m], bb[0:m, h * 128:(h + 1) * 128],
                               ident_b[0:m, 0:m], bb.u + ident_b.u, tb.u)
                        cp("dve", o[:, :, 0:m],
                           tb.t[:, :].bitcast(BF16)[:, 0:512].rearrange("p (h t) -> p h t", t=128)[:, :, 0:m], tb.u, o.u)
                        dma("sp", "qkT%d" % which, scr[:, :, r0:r0 + m].rearrange("h p t -> p h t"), o[:, :, 0:m], o.u, ())
                    b = banks[6]
                    for k in range(8):
                        mm(b.t[0:m, :], hT[:, k, st * 128:st * 128 + m], wbig[:, k, 1024:1536], k == 0, k == 7,
                           wbig.u + hT.u, b.u)
                    f = vf[0]
                    act(f[0:m, :], b.t[0:m, :], AF.Copy, b.u, f.u)
                    dma("sp", "vf0", nav[l, r0:r0 + m, :], f[0:m, :], f.u, ())
                    vb_ = vab[st % 2]
                    cp("dve", vb_[0:m, :, 0:128], b.t[0:m, :].rearrange("p (h e) -> p h e", e=128), b.u, vb_.u)
                    dma("sp", "vab%d" % (st % 2), VA[:, r0:r0 + m, :].rearrange("h t e -> t h e"), vb_[0:m, :, :], vb_.u, ())
                    b = banks[7]
                    for k in range(8):
                        mm(b.t[0:m, :], hT[:, k, st * 128:st * 128 + m], wbig[:, k, 2560:3072], k == 0, k == 7,
                           wbig.u + hT.u, b.u)
                    vb_ = vbb[st % 2]
                    cp("dve", vb_[0:m, :, 0:64], b.t[0:m, :].rearrange("p (h e) -> p h e", e=64), b.u, vb_.u)
                    dma("sp", "vbb%d" % (st % 2), VB[r0:r0 + m, :], vb_[0:m, :, :].rearrange("p h e -> p (h e)"), vb_.u, ())
                    if outblk:
                        f = vf[1]
                        act(f[0:m, :], b.t[0:m, :], AF.Copy, b.u, f.u)
                        if c0 == SEQ:
                            for s in range(4):
                                dma("sp", "vf1", nbv_s[l, s, 496:512, :], f[16 * s:16 * s + 16, :], f.u, ())
                        else:
                            dma("sp", "vf1", nbv_p[l, st * 128:st * 128 + m, :], f[0:m, :], f.u, ())
                        b = banks[6]
                        for k in range(8):
                            mm(b.t[0:m, :], hT[:, k, st * 128:st * 128 + m], wbig[:, k, 2048:2560], k == 0, k == 7,
                               wbig.u + hT.u, b.u)
                        f = vf[2]
                        act(f[0:m, :], b.t[0:m, :], AF.Copy, b.u, f.u)
                        if c0 == SEQ:
                            for s in range(4):
                                dma("sp", "vf2", nbk_s[l, s, 496:512, :], f[16 * s:16 * s + 16, :], f.u, ())
                        else:
                            dma("sp", "vf2", nbk_p[l, st * 128:st * 128 + m, :], f[0:m, :], f.u, ())
            for s in range(4):
                dma("sp", "roll", nbk_s[l, s, 0:496, :], I.cbk[l, s, 16:512, :], (), ())
                dma("sp", "roll", nbv_s[l, s, 0:496, :], I.cbv[l, s, 16:512, :], (), ())
            pg.barrier()

    def mmg(out, lhsT, rhs, start, stop, reads, writes):
        pg.add("pe", lambda e: e.matmul(out, lhsT, rhs, start=start, stop=stop, skip_group_check=True), reads, writes)

    def p2(l):
        P2F = int(os.environ.get('MK_P2F', '15'))
        with ExitStack() as ph:
            wout = sb("wout", [128, 8, 1024], BF16, nunits=8, st=ph)
            for k in range(8):
                dma("pool", "wout", wout[:, k, :], I.w_out[l][k * 128:(k + 1) * 128, :], (), [wout.u[k]])
            bias_b = sb("bias_b", [128, 8, 2, 128], BF16, nunits=16, st=ph)
            for hb in range(8):
                for j2 in range(2):
                    dma("pool", "biasb", bias_b[:, hb, j2, :], I.biasT[l, hb, j2], (), [bias_b.u[hb * 2 + j2]])
            cbrow = sb("cbrow", [128, 8, 128], BF16, st=ph)
            memset("pool", cbrow[:], 0.0, cbrow.u)
            dma("pool", "biasb", cbrow[0:1, :, :], I.bconst[l:l + 1, :, :], (), cbrow.u)
            qta = [sb("qta%d" % i, [128, 4, 512], BF16, st=ph) for i in range(2)]
            qtb = [sb("qtb%d" % i, [128, 4, 512], BF16, st=ph) for i in range(2)]
            kta = sb("kta", [128, SEQ], BF16, nunits=8, st=ph)
            vat = sb("vat", [128, 64, 136], BF16, nunits=8, st=ph)
            ktb = [sb("ktb%d" % i, [128, 4, 1024], BF16, st=ph) for i in range(2)]
            vbt = [sb("vbt%d" % i, [128, 8, 576], BF16, st=ph) for i in range(2)]
            pT = [sb("pT%d" % i, [128, 512], BF16, st=ph) for i in range(4)]
            oblk = sb("oblk", [128, 4, 1024], BF16, st=ph)
            oT = sb("oT", [128, 8, 512], BF16, st=ph)
            rz = sb("rz", [128, 8], st=ph)
            t1 = sb("t1", [128, 128], st=ph)
            oa = sb("oa", [128, 512], st=ph)
            junk = sb("junk", [128, 512], st=ph)
            ss = sb("ss", [128, 1], st=ph)

            def accA(m_, rr):
                idx = m_ * 4 + rr
                bk = banks[4 + idx // 3]
                c = (idx % 3) * 129
                return bk, bk.t[:, c:c + 129], (idx % 3 == 0)

            def epiA(h, rr, np_, dst):
                b0, a0, _ = accA(0, rr)
                b1, a1, _ = accA(1, rr)
                V("dve", lambda e: e.reciprocal(rz[0:np_, 0:1], a0[0:np_, 128:129]), b0.u, rz.u)
                V("dve", lambda e: e.reciprocal(rz[0:np_, 1:2], a1[0:np_, 128:129]), b1.u, rz.u)
                ts("dve", rz[0:np_, 1:2], rz[0:np_, 1:2], lam[0:np_, l, 2:3], ALU.mult, rz.u + lam.u, rz.u)
                ts("dve", t1[0:np_, :], a1[0:np_, 0:128], rz[0:np_, 1:2], ALU.mult, b1.u + rz.u, t1.u)
                stt(oa[0:np_, 0:128], a0[0:np_, 0:128], rz[0:np_, 0:1], t1[0:np_, :], ALU.mult, ALU.add,
                    b0.u + rz.u + t1.u, oa.u)
                act(junk[0:np_, 0:128], oa[0:np_, 0:128], AF.Square, oa.u, junk.u + ss.u, accum=ss[0:np_, 0:1])
                act(ss[0:np_, :], ss[0:np_, :], AF.Sqrt, ss.u + epsc.u, ss.u, bias=epsc[0:np_, 0:1], scale=1.0 / 128)
                V("dve", lambda e: e.reciprocal(ss[0:np_, :], ss[0:np_, :]), ss.u, ss.u)
                stt(dst, oa[0:np_, 0:128], ss[0:np_, 0:1], gainA[0:np_, l, :], ALU.mult, ALU.mult,
                    oa.u + ss.u + gainA.u, oblk.u)

            def epiB(np_, dst, w):
                for hf in range(2):
                    bk = banks[4 + hf]
                    v = bk.t[0:np_, 0:4 * w].rearrange("p (h e) -> p h e", e=w)
                    V("dve", lambda e, v=v, hf=hf: e.reciprocal(rz[0:np_, hf * 4:hf * 4 + 4], v[:, :, 64]), bk.u, rz.u)
                    tt("dve", oa[0:np_, hf * 256:(hf + 1) * 256].rearrange("p (h e) -> p h e", e=64), v[:, :, 0:64],
                       rz[0:np_, hf * 4:hf * 4 + 4].unsqueeze(2).to_broadcast([np_, 4, 64]), ALU.mult,
                       bk.u + rz.u, oa.u)
                act(junk[0:np_, :], oa[0:np_, :], AF.Square, oa.u, junk.u + ss.u, accum=ss[0:np_, 0:1])
                act(ss[0:np_, :], ss[0:np_, :], AF.Sqrt, ss.u + epsc.u, ss.u, bias=epsc[0:np_, 0:1], scale=1.0 / 512)
                V("dve", lambda e: e.reciprocal(ss[0:np_, :], ss[0:np_, :]), ss.u, ss.u)
                stt(dst, oa[0:np_, :], ss[0:np_, 0:1], gainB[0:np_, l, :], ALU.mult, ALU.mult,
                    oa.u + ss.u + gainB.u, oblk.u)

            def outproj(x, c0, n):
                for dc in range(8):
                    yb = banks[2 + dc % 2]
                    for kc in range(8):
                        mm(yb.t[:, 0:n], wout[:, kc, dc * 128:(dc + 1) * 128], oT[:, kc, 0:n], kc == 0, kc == 7,
                           wout.u + oT.u, yb.u)
                    for (lo, hi, r) in segs(c0, n):
                        stt(x[:, dc, lo:hi], yb.t[:, lo:hi], modT[l][:, 16 + dc, r:r + 1], x[:, dc, lo:hi],
                            ALU.mult, ALU.add, yb.u + modT[l].u + x.u, x.u)
                store_xb(x, c0, n)

            pcnt = 0
            for bi, (c0, n) in enumerate(pblocks):
                j = c0 // 512
                x = load_xb(c0, n)
                qa = qta[bi % 2]
                qb = qtb[bi % 2]
                dma("sp", "qta%d" % (bi % 2), qa[:], QTA[:, :, c0:c0 + 512].rearrange("h p t -> p h t"), (), qa.u)
                dma("sp", "qtb%d" % (bi % 2), qb[:], QTB[:, :, c0:c0 + 512].rearrange("h p t -> p h t"), (), qb.u)
                nkt = 4 * (j + 1)
                for h in range(4 if P2F & 1 else 0):
                    for u8 in range((nkt * 128 + 1023) // 1024):
                        lo = u8 * 1024
                        hi = min(lo + 1024, nkt * 128)
                        dma("sp", "kta%d" % u8, kta[:, lo:hi], KTA[h, :, lo:hi], (), [kta.u[u8]])
                        dma("sp", "vat%d" % u8, vat[:, lo // 128:hi // 128, :],
                            VA[h, lo:hi, :].rearrange("(kt p) e -> p kt e", p=128), (), [vat.u[u8]])
                    for kt in range(nkt):
                        r = kt - 4 * j
                        cs = max(r, 0) * 128
                        u8 = kt // 8
                        for m_ in range(2):
                            Sb = banks[pcnt % 4]
                            pt = pT[pcnt % 4]
                            pcnt += 1
                            mm(Sb.t[:, cs:512], kta[64 * m_:64 * m_ + 64, kt * 128:(kt + 1) * 128],
                               qa[64 * m_:64 * m_ + 64, h, cs:512], True, r < 0, [kta.u[u8]] + qa.u, Sb.u)
                            if r >= 0:
                                mm(Sb.t[:, cs:cs + 128], mk[:, 0, :], mk[:, 1, :], False, True, mk.u, Sb.u)
                            act(pt[:, cs:512], Sb.t[:, cs:512], AF.Exp, Sb.u, pt.u)
                            for rr in range(max(r, 0), 4):
                                bk, ap_, first = accA(m_, rr)
                                mmg(ap_, pt[:, rr * 128:(rr + 1) * 128], vat[:, kt, 0:129],
                                    (kt == 0 and first), kt == 4 * j + rr, pt.u + [vat.u[u8]], bk.u)
                    for rr in range(4):
                        epiA(h, rr, 128, oblk[:, rr, h * 128:(h + 1) * 128])
                lo_t = max(0, 4 * j - 4)
                hi_t = 4 * j + 4
                kb_ = ktb[bi % 2]
                vb_ = vbt[bi % 2]
                nkl = (hi_t - lo_t) * 128
                dma("sp", "ktb%d" % (bi % 2), kb_[:, :, 0:nkl],
                    KTB[:, :, lo_t * 128:hi_t * 128].rearrange("c p t -> p c t"), (), kb_.u)
                dma("sp", "vbt%d" % (bi % 2), vb_[:, 0:hi_t - lo_t, :],
                    VB[lo_t * 128:hi_t * 128, :].rearrange("(kt p) e -> p kt e", p=128), (), vb_.u)
                for rr in range(4 if P2F & 2 else 0):
                    i_ = 4 * j + rr
                    jjs = [jj for jj in range(5) if i_ - 4 + jj >= 0]
                    for jj in jjs:
                        ktl = i_ - 4 + jj - lo_t
                        for hf in range(2):
                            Sb = banks[pcnt % 4]
                            pt = pT[pcnt % 4]
                            pcnt += 1
                            for hh in range(4):
                                hb = hf * 4 + hh
                                cc = hb // 2
                                prt = (hb % 2) * 64
                                cols = Sb.t[:, hh * 128:(hh + 1) * 128]
                                mm(cols, kb_[prt:prt + 64, cc, ktl * 128:(ktl + 1) * 128],
                                   qb[prt:prt + 64, cc, rr * 128:(rr + 1) * 128], True, False, kb_.u + qb.u, Sb.u)
                                if jj <= 2:
                                    mm(cols, mk[:, 4, :], cbrow[:, hb, :], False, jj != 0, mk.u + cbrow.u, Sb.u)
                                    if jj == 0:
                                        mm(cols, mk[:, 2, :], mk[:, 3, :], False, True, mk.u, Sb.u)
                                elif jj == 3:
                                    mm(cols, ident_b[:], bias_b[:, hb, 0, :], False, True, ident_b.u + bias_b.u, Sb.u)
                                else:
                                    mm(cols, ident_b[:], bias_b[:, hb, 1, :], False, False, ident_b.u + bias_b.u, Sb.u)
                                    mm(cols, mk[:, 0, :], mk[:, 1, :], False, True, mk.u, Sb.u)
                            act(pt[:, :], Sb.t[:, :], AF.Exp, Sb.u, pt.u)
                            for hh in range(4):
                                hb = hf * 4 + hh
                                bk = banks[4 + hf]
                                mmg(bk.t[:, hh * 65:hh * 65 + 65], pt[:, hh * 128:(hh + 1) * 128],
                                    vb_[:, ktl, hb * 72:hb * 72 + 65], (jj == jjs[0] and hh == 0), jj == 4,
                                    pt.u + vb_.u, bk.u)
                    epiB(128, oblk[:, rr, 512:1024], 65)
                if not P2F & 4:
                    continue
                for rr in range(4):
                    tb = banks[rr % 2]
                    tbv = tb.t[:, :].bitcast(BF16)
                    for kc in range(8):
                        tr(tbv[:, kc * 128:(kc + 1) * 128], oblk[:, rr, kc * 128:(kc + 1) * 128], ident_b[:],
                           oblk.u + ident_b.u, tb.u)
                    cp("act" if rr % 2 == 0 else "dve", oT[:, :, rr * 128:(rr + 1) * 128],
                       tbv.rearrange("p (k t) -> p k t", t=128), tb.u, oT.u)
                outproj(x, c0, 512)

            x = load_xb(SEQ, NS)
            qas = sb("qas", [128, 4, NS], BF16, st=ph)
            qbs = sb("qbs", [128, 4, NS], BF16, st=ph)
            kan = sb("kan", [128, 4, NS], BF16, st=ph)
            kbn = sb("kbn", [128, 4, NS], BF16, st=ph)
            van = [sb("van%d" % s_, [16, 4, 136], BF16, st=ph) for s_ in range(4)]
            vbn = [sb("vbn%d" % s_, [16, 576], BF16, st=ph) for s_ in range(4)]
            dma("sp", "smp", qas[:], QTA[:, :, SEQ:T].rearrange("h p t -> p h t"), (), qas.u)
            dma("sp", "smp", qbs[:], QTB[:, :, SEQ:T].rearrange("h p t -> p h t"), (), qbs.u)
            dma("sp", "smp", kan[:], KTA[:, :, SEQ:T].rearrange("h p t -> p h t"), (), kan.u)
            dma("sp", "smp", kbn[:], KTB[:, :, SEQ:T].rearrange("h p t -> p h t"), (), kbn.u)
            for s_ in range(4):
                dma("sp", "smp", van[s_][:], VA[:, SEQ + 16 * s_:SEQ + 16 * s_ + 16, :].rearrange("h t e -> t h e"),
                    (), van[s_].u)
                dma("sp", "smp", vbn[s_][:], VB[SEQ + 16 * s_:SEQ + 16 * s_ + 16, :], (), vbn[s_].u)
            kcb = [sb("kcb%d" % i, [128, 512], BF16, st=ph) for i in range(2)]
            vca = [sb("vca%d" % i, [128, 4, 136], BF16, st=ph) for i in range(2)]
            vcb = [sb("vcb%d" % i, [128, 8, 72], BF16, st=ph) for i in range(2)]
            for i in range(2):
                memset("pool", vca[i][:, :, 128:136], 1.0, vca[i].u)
                memset("pool", vcb[i][:, :, 64:72], 1.0, vcb[i].u)
            kTs = [sb("kTs%d" % i, [128, 4, 128], BF16, st=ph) for i in range(2)]
            pts = [sb("pts%d" % i, [128, 128], BF16, st=ph) for i in range(2)]
            os_ = sb("os_", [16, 1024], BF16, st=ph)
            scnt = 0
            for s_ in range(4 if P2F & 8 else 0):
                q0 = 16 * s_
                SF = int(os.environ.get('MK_SF', '31'))
                for kt in ([int(v) for v in os.environ['MK_SK'].split(',')] if os.environ.get('MK_SK') else range(33 if SF & 1 else 0)):
                    kk = 128 if kt < 32 else 16
                    if kt < 32:
                        kc = kcb[scnt % 2]
                        vc = vca[scnt % 2]
                        kT = kTs[scnt % 2]
                        dma("pool", "kcb%d" % (scnt % 2), kc[:], I.cak[l, s_, kt * 128:(kt + 1) * 128, :], (), kc.u)
                        dma("pool", "vca%d" % (scnt % 2), vc[:, :, 0:128],
                            I.cav[l, s_, kt * 128:(kt + 1) * 128, :].rearrange("t (h e) -> t h e", e=128), (), vc.u)
                        tb = banks[0]
                        tbv = tb.t[:, :].bitcast(BF16)
                        for h in range(4):
                            tr(tbv[:, h * 128:(h + 1) * 128], kc[:, h * 128:(h + 1) * 128], ident_b[:],
                               kc.u + ident_b.u, tb.u)
                        cp("dve", kT[:], tbv[:, 0:512].rearrange("p (h t) -> p h t", t=128), tb.u, kT.u)
                        kTv = lambda h, m_, kT=kT: kT[64 * m_:64 * m_ + 64, h, :]
                        kTu = kT.u
                        vv = lambda h, vc=vc: vc[:, h, 0:129]
                        vu = vc.u
                    else:
                        if os.environ.get('MK_SWAP'):
                            kTv = lambda h, m_: kbn[64 * m_:64 * m_ + 64, h, q0:q0 + 16]
                        else:
                            kTv = lambda h, m_: kan[64 * m_:64 * m_ + 64, h, q0:q0 + 16]
                        kTu = kan.u + kbn.u
                        vv = lambda h: van[s_][0:16, h, 0:129]
                        vu = van[s_].u
                    Sbs = [(banks[1], banks[2]), (banks[3], banks[7])][scnt % 2]
                    pt = pts[scnt % 2]
                    scnt += 1
                    SX = int(os.environ.get('MK_SX', '7'))
                    VV_ = os.environ.get('MK_V', '')
                    for m_ in range((1 if VV_ == 'm0' else 2) if SX & 1 else 0):
                        for h in range(4):
                            g_ = h * 2 + m_
                            qq = qbs if os.environ.get('MK_SWAP') else qas
                            mm(Sbs[m_].t[0:kk, h * 16:h * 16 + 16], kTv(h, m_), qq[64 * m_:64 * m_ + 64, h, q0:q0 + 16],
                               True, True, kTu + qas.u + qbs.u, Sbs[m_].u)
                    for m_ in range(2):
                        act(pt[0:kk, m_ * 64:(m_ + 1) * 64], Sbs[m_].t[0:kk, 0:64], AF.Exp, Sbs[m_].u, pt.u)
                    for m_ in range(2 if SX & 4 else 0):
                        for h in range(4):
                            g_ = m_ * 4 + h
                            bk, ap_, first = accA(m_, h)
                            mmg(ap_[0:16, :], pt[0:kk, g_ * 16:g_ * 16 + 16], vv(h), (kt == 0 and first), kt == 32,
                                pt.u + vu, bk.u)
                for h in range(4 if SF & 2 else 0):
                    epiA(h, h, 16, os_[0:16, h * 128:(h + 1) * 128])
                for kt in range(5 if SF & 4 else 0):
                    kk = 128 if kt < 4 else 16
                    if kt < 4:
                        kc = kcb[scnt % 2]
                        vc = vcb[scnt % 2]
                        kT = kTs[scnt % 2]
                        dma("pool", "kcb%d" % (scnt % 2), kc[:], I.cbk[l, s_, kt * 128:(kt + 1) * 128, :], (), kc.u)
                        dma("pool", "vcb%d" % (scnt % 2), vc[:, :, 0:64],
                            I.cbv[l, s_, kt * 128:(kt + 1) * 128, :].rearrange("t (h e) -> t h e", e=64), (), vc.u)
                        tb = banks[scnt % 2]
                        tbv = tb.t[:, :].bitcast(BF16)
                        for cc in range(4):
                            tr(tbv[:, cc * 128:(cc + 1) * 128], kc[:, cc * 128:(cc + 1) * 128], ident_b[:],
                               kc.u + ident_b.u, tb.u)
                        cp("dve", kT[:], tbv[:, 0:512].rearrange("p (h t) -> p h t", t=128), tb.u, kT.u)
                        kTv = lambda cc, prt, kT=kT: kT[prt:prt + 64, cc, :]
                        kTu = kT.u
                        vv = lambda hb, vc=vc: vc[:, hb, 0:65]
                        vu = vc.u
                    else:
                        kTv = lambda cc, prt: kbn[prt:prt + 64, cc, q0:q0 + 16]
                        kTu = kbn.u
                        vv = lambda hb: vbn[s_][0:16, hb * 72:hb * 72 + 65]
                        vu = vbn[s_].u
                    Sb = banks[2 + scnt % 2]
                    pt = pts[scnt % 2]
                    scnt += 1
                    for hb in range(8):
                        cc = hb // 2
                        prt = (hb % 2) * 64
                        cols = Sb.t[0:kk, hb * 16:hb * 16 + 16]
                        mm(cols, kTv(cc, prt), qbs[prt:prt + 64, cc, q0:q0 + 16], True, False, kTu + qbs.u, Sb.u)
                        if kt <= 2:
                            mm(cols, mk[:, 4, 0:kk], cbrow[:, hb, 0:16], False, True, mk.u + cbrow.u, Sb.u)
                        elif kt == 3:
                            mm(cols, ident_b[:], bias_b[:, hb, 0, 0:16], False, True, ident_b.u + bias_b.u, Sb.u)
                        else:
                            mm(cols, ident_b[:, 0:16], bias_b[:, hb, 1, 0:16], False, True,
                               ident_b.u + bias_b.u, Sb.u)
                    act(pt[0:kk, :], Sb.t[0:kk, 0:128], AF.Exp, Sb.u, pt.u)
                    for hb in range(8):
                        bk = banks[4 + hb // 4]
                        hh = hb % 4
                        mmg(bk.t[0:16, hh * 65:hh * 65 + 65], pt[0:kk, hb * 16:hb * 16 + 16], vv(hb),
                            (kt == 0 and hh == 0), kt == 4, pt.u + vu, bk.u)
                if SF & 8:
                    epiB(16, os_[0:16, 512:1024], 65)
                if not SF & 16:
                    continue
                tb = banks[scnt % 2]
                tbv = tb.t[:, :].bitcast(BF16)
                for kc_ in range(8):
                    tr(tbv[:, kc_ * 16:kc_ * 16 + 16], os_[0:16, kc_ * 128:(kc_ + 1) * 128], ident_b[0:16, 0:16],
                       oblk.u + ident_b.u, tb.u)
                cp("dve", oT[:, :, q0:q0 + 16], tbv[:, 0:128].rearrange("p (k t) -> p k t", t=16), tb.u, oT.u)
            if P2F & 8:
                outproj(x, SEQ, NS)
            pg.barrier()

    def pu(l):
        with ExitStack() as ph:
            ubuf = [sb("ubuf%d" % i, [128, D], BF16, st=ph) for i in range(2)]
            vbuf = [sb("vbuf%d" % i, [128, D], BF16, st=ph) for i in range(2)]
            utb = [sb("utb%d" % i, [128, 8, 128], BF16, st=ph) for i in range(2)]
            for ec in range(128):
                ub = ubuf[ec % 2]
                vb_ = vbuf[ec % 2]
                ut = utb[ec % 2]
                dma("pool", "ubuf%d" % (ec % 2), ub[:], I.pk_u[l, ec * 128:(ec + 1) * 128, :], (), ub.u)
                dma("pool", "vbuf%d" % (ec % 2), vb_[:], I.pk_v[l, ec * 128:(ec + 1) * 128, :], (), vb_.u)
                tb = banks[ec % 2]
                tbv = tb.t[:, :].bitcast(BF16)
                for k in range(8):
                    tr(tbv[:, k * 128:(k + 1) * 128], ub[:, k * 128:(k + 1) * 128], ident_b[:], ub.u + ident_b.u, tb.u)
                cp("act" if ec % 2 == 0 else "dve", ut[:], tbv.rearrange("p (k e) -> p k e", e=128), tb.u, ut.u)
                dma("sp", "utb%d" % (ec % 2), UT[:, :, ec * 128:(ec + 1) * 128].rearrange("k p e -> p k e"), ut[:], ut.u, ())
                dma("sp", "vbufo%d" % (ec % 2), VBF[ec * 128:(ec + 1) * 128, :], vb_[:], vb_.u, ())
            pg.barrier()

    def p3(l):
        with ExitStack() as ph:
            nb = NB(ph, "p3")
            h2 = nb.h
            wq = sb("wq", [128, 8, 2048], BF16, nunits=16, st=ph)
            for k in range(8):
                for c2 in range(2):
                    dma("pool", "wq", wq[:, k, c2 * 1024:(c2 + 1) * 1024],
                        I.pk_wq[l][k * 128:(k + 1) * 128, c2 * 1024:(c2 + 1) * 1024], (), [wq.u[k * 2 + c2]])
            kraw = sb("kraw", [128, 16, 128], BF16, st=ph)
            keysT = sb("keysT", [128, 16, 128], BF16, st=ph)
            for g in range(16):
                dma("pool", "kraw", kraw[:, g, :], I.pk_keys[l, g], (), kraw.u)
            for g in range(16):
                tb = banks[g % 2]
                tbv = tb.t[:, :].bitcast(BF16)
                tr(tbv[:, 0:128], kraw[:, g, :], ident_b[:], kraw.u + ident_b.u, tb.u)
                cp("dve", keysT[:, g, :], tbv[:, 0:128], tb.u, keysT.u)
            qT = sb("qT", [128, 16, 512], BF16, st=ph)
            sc = sb("sc", [128, 16, 128], st=ph)
            sc2 = sb("sc2", [128, 16, 128], st=ph)
            sv = sb("sv", [128, 16, 16], st=ph)
            si = sb("si", [128, 16, 16], U32, st=ph)
            sif = sb("sif", [128, 16, 16], st=ph)
            cand = sb("cand", [128, 8, 256], st=ph)
            cand2 = sb("cand2", [128, 8, 256], st=ph)
            cv = sb("cv", [128, 8, 16], st=ph)
            ci = sb("ci", [128, 8, 16], U32, st=ph)
            cab = sb("cab", [128, 2, 8, 16], U32, st=ph)
            cabf = sb("cabf", [128, 2, 8, 16], st=ph)
            eq = sb("eq", [128, 8, 16, 16], st=ph)
            res = sb("res", [128, 3, 128], st=ph)
            gs = sb("gs", [128, 8], st=ph)
            rts = [sb("rts%d" % i, [128, 3, 128], st=ph) for i in range(2)]
            bcnt = 0
            for (c0, n) in blocks:
                x = load_xb(c0, n)
                norm_mod(nb, x, c0, n, A2[l], modT[l], 24, banks[7])
                dma("sp", "h2st", H2T[:, :, c0:c0 + n].rearrange("k p t -> p k t"), h2[:, :, 0:n], h2.u, ())
                for qc in range(16):
                    b = banks[qc % 2]
                    for k in range(8):
                        mm(b.t[:, 0:n], wq[:, k, qc * 128:(qc + 1) * 128], h2[:, k, 0:n], k == 0, k == 7,
                           wq.u + h2.u, b.u)
                    cp("act" if qc % 2 == 0 else "dve", qT[:, qc, 0:n], b.t[:, 0:n], b.u, qT.u)
                for st in range((n + 127) // 128):
                    m = min(128, n - st * 128)
                    r0 = c0 + st * 128
                    for qc in range(16):
                        b = banks[2 + qc // 4]
                        mm(b.t[0:m, (qc % 4) * 128:(qc % 4 + 1) * 128], qT[:, qc, st * 128:st * 128 + m],
                           keysT[:, qc, :], True, True, qT.u + keysT.u, b.u)
                    for q4 in range(4):
                        b = banks[2 + q4]
                        cp("act", sc[0:m, q4 * 4:(q4 + 1) * 4, :], b.t[0:m, :].rearrange("p (g n) -> p g n", n=128),
                           b.u, sc.u)
                    for g in range(16):
                        vmax(sv[0:m, g, 0:8], sc[0:m, g, :], sc.u, sv.u)
                    for g in range(16):
                        vmaxidx(si[0:m, g, 0:8], sv[0:m, g, 0:8], sc[0:m, g, :], sc.u + sv.u, si.u)
                    for g in range(16):
                        vmatch(sc2[0:m, g, :], sv[0:m, g, 0:8], sc[0:m, g, :], sc.u + sv.u, sc2.u)
                    for g in range(16):
                        vmax(sv[0:m, g, 8:16], sc2[0:m, g, :], sc2.u, sv.u)
                    for g in range(16):
                        vmaxidx(si[0:m, g, 8:16], sv[0:m, g, 8:16], sc2[0:m, g, :], sc2.u + sv.u, si.u)
                    svv = sv[0:m].rearrange("p (h two) a -> p h two a", two=2)
                    tt("dve", cand[0:m].rearrange("p h (a b) -> p h a b", b=16),
                       svv[:, :, 0, :].unsqueeze(3).to_broadcast([m, 8, 16, 16]),
                       svv[:, :, 1, :].unsqueeze(2).to_broadcast([m, 8, 16, 16]), ALU.add, sv.u, cand.u)
                    for h in range(8):
                        vmax(cv[0:m, h, 0:8], cand[0:m, h, :], cand.u, cv.u)
                    for h in range(8):
                        vmaxidx(ci[0:m, h, 0:8], cv[0:m, h, 0:8], cand[0:m, h, :], cand.u + cv.u, ci.u)
                    for h in range(8):
                        vmatch(cand2[0:m, h, :], cv[0:m, h, 0:8], cand[0:m, h, :], cand.u + cv.u, cand2.u)
                    for h in range(8):
                        vmax(cv[0:m, h, 8:16], cand2[0:m, h, :], cand2.u, cv.u)
                    for h in range(8):
                        vmaxidx(ci[0:m, h, 8:16], cv[0:m, h, 8:16], cand2[0:m, h, :], cand2.u + cv.u, ci.u)
                    ts("dve", cab[0:m, 0], ci[0:m], 4, ALU.logical_shift_right, ci.u, cab.u)
                    ts("dve", cab[0:m, 1], ci[0:m], 15, ALU.bitwise_and, ci.u, cab.u)
                    cp("dve", cabf[0:m], cab[0:m], cab.u, cabf.u)
                    cp("dve", sif[0:m], si[0:m], si.u, sif.u)
                    sifv = sif[0:m].rearrange("p (h two) a -> p h two a", two=2)
                    for w2 in range(2):
                        tt("dve", eq[0:m], cabf[0:m, w2].unsqueeze(3).to_broadcast([m, 8, 16, 16]),
                           iota16[0:m, :].unsqueeze(1).unsqueeze(1).to_broadcast([m, 8, 16, 16]), ALU.is_equal,
                           cabf.u + iota16.u, eq.u)
                        tt("dve", eq[0:m], eq[0:m], sifv[:, :, w2, :].unsqueeze(2).to_broadcast([m, 8, 16, 16]),
                           ALU.mult, eq.u + sif.u, eq.u)
                        vredsum(res[0:m, w2, :].rearrange("p (h k) -> p h k", k=16), eq[0:m], eq.u, res.u)
                    resg = res[0:m, 2, :].rearrange("p (h k) -> p h k", k=16)
                    tt("dve", resg, cv[0:m], cv[0:m, :, 0:1].to_broadcast([m, 8, 16]), ALU.subtract, cv.u, res.u)
                    act(resg, resg, AF.Exp, res.u, res.u)
                    vredsum(gs[0:m, :], res[0:m, 2, :].rearrange("p (h k) -> p h k", k=16), res.u, gs.u)
                    vrecip(gs[0:m, :], gs[0:m, :], gs.u, gs.u)
                    tt("dve", resg, resg, gs[0:m, :].unsqueeze(2).to_broadcast([m, 8, 16]), ALU.mult, res.u + gs.u, res.u)
                    tb = banks[6]
                    for q3 in range(3):
                        tr(tb.t[:, q3 * 128:q3 * 128 + m], res[0:m, q3, :], ident_f[0:m, 0:m], res.u + ident_f.u, tb.u)
                    ro = rts[bcnt % 2]
                    cp("act", ro[:, :, 0:m], tb.t[:, 0:384].rearrange("p (q t) -> p q t", t=128)[:, :, 0:m], tb.u, ro.u)
                    dma("sp", "rts%d" % (bcnt % 2), RT[:, :, r0:r0 + m].rearrange("q p t -> p q t"), ro[:, :, 0:m], ro.u, ())
                    bcnt += 1
            pg.barrier()

    def p4(l):
        TG = 256
        with ExitStack() as ph:
            Wall = sb("Wall", [128, TG, 128], BF16, st=ph)
            ohj = [sb("ohj%d" % i, [128, 32, 128], BF16, st=ph) for i in range(2)]
            ohi = [sb("ohi%d" % i, [128, 32, 128], BF16, st=ph) for i in range(2)]
            ust = [sb("ust%d" % i, [128, 8, 512], BF16, st=ph) for i in range(2)]
            vst = [sb("vst%d" % i, [128, 4, D], BF16, st=ph) for i in range(2)]
            h2g = [sb("h2g%d" % i, [128, 8, TG], BF16, st=ph) for i in range(2)]
            rtg = [sb("rtg%d" % i, [128, 3, TG], st=ph) for i in range(2)]
            gl = [sb("gl%d" % i, [128, TG], BF16, st=ph) for i in range(3)]
            pTt = [sb("pTt%d" % i, [128, TG], BF16, st=ph) for i in range(3)]
            pgroups = [(c, TG) for c in range(0, SEQ, TG)]
            if NBLK:
                pgroups = pgroups[:NBLK]
            groups = pgroups + [(SEQ, NS)]
            pc = 0
            wc = 0
            sc_ = 0
            for gi_, (c0, n) in enumerate(groups):
                h2 = h2g[gi_ % 2]
                rt = rtg[gi_ % 2]
                dma("sp", "h2g%d" % (gi_ % 2), h2[:, :, 0:n], H2T[:, :, c0:c0 + n].rearrange("k p t -> p k t"), (), h2.u)
                dma("sp", "rtg%d" % (gi_ % 2), rt[:, :, 0:n], RT[:, :, c0:c0 + n].rearrange("q p t -> p q t"), (), rt.u)
                x = load_xb(c0, n)
                for t0 in range(0, n, 32):
                    oj = ohj[pc % 2]
                    oi = ohi[pc % 2]
                    pc += 1
                    for t in range(32):
                        tk = t0 + t
                        ts("dve", oj[:, t, :], iota_b[:], rt[:, 1, tk:tk + 1], ALU.is_equal, iota_b.u + rt.u, oj.u)
                        ts("pool", oi[:, t, :], iota_b[:], rt[:, 0, tk:tk + 1], ALU.is_equal, iota_b.u + rt.u, oi.u,
                           s2=rt[:, 2, tk:tk + 1], op1=ALU.mult)
                    for t4 in range(0, 32, 4):
                        wb = banks[6 + wc % 2]
                        wc += 1
                        for t in range(4):
                            mm(wb.t[:, t * 128:(t + 1) * 128], oj[:, t4 + t, :], oi[:, t4 + t, :], True, True,
                               oj.u + oi.u, wb.u)
                        cp("act" if wc % 2 == 0 else "dve", Wall[:, t0 + t4:t0 + t4 + 4, :],
                           wb.t[:, :].rearrange("p (t i) -> p t i", i=128), wb.u, Wall.u)
                for eb in range(32):
                    us = ust[sc_ % 2]
                    vs = vst[sc_ % 2]
                    dma("sp", "ust%d" % (sc_ % 2), us[:], UT[:, :, eb * 512:(eb + 1) * 512].rearrange("k p e -> p k e"),
                        (), us.u)
                    dma("sp", "vst%d" % (sc_ % 2), vs[:], VBF[eb * 512:(eb + 1) * 512, :].rearrange("(c p) d -> p c d", p=128),
                        (), vs.u)
                    sc_ += 1
                    for c4 in range(4):
                        ec = eb * 4 + c4
                        ab = banks[4 + ec % 2]
                        for k in range(8):
                            mm(ab.t[:, 0:n], us[:, k, c4 * 128:(c4 + 1) * 128], h2[:, k, 0:n], k == 0, k == 7,
                               us.u + h2.u, ab.u)
                        g_ = gl[ec % 3]
                        p_ = pTt[ec % 3]
                        act(g_[:, 0:n], ab.t[:, 0:n], AF.Gelu_apprx_tanh, ab.u, g_.u)
                        tt("dve", p_[:, 0:n], g_[:, 0:n], Wall[:, 0:n, ec], ALU.mult, g_.u + Wall.u, p_.u)
                        for dc in range(8):
                            bk = banks[dc // 2]
                            mmg(bk.t[:, (dc % 2) * 256:(dc % 2) * 256 + n], vs[:, c4, dc * 128:(dc + 1) * 128],
                                p_[:, 0:n], (ec == 0 and dc % 2 == 0), ec == 127, vs.u + p_.u, bk.u)
                for dc in range(8):
                    bk = banks[dc // 2]
                    for (lo, hi, r) in segs(c0, n):
                        o_ = (dc % 2) * 256
                        stt(x[:, dc, lo:hi], bk.t[:, o_ + lo:o_ + hi], modT[l][:, 40 + dc, r:r + 1], x[:, dc, lo:hi],
                            ALU.mult, ALU.add, bk.u + modT[l].u + x.u, x.u)
                store_xb(x, c0, n)
            pg.barrier()

    def pfinal():
        with ExitStack() as ph:
            gfin = sb("gfin", [128, D], st=ph)
            dma("sp", "c0", gfin[:], I.g_final[0:1, :].partition_broadcast(128), (), gfin.u)
            xo = [sb("xo%d" % i, [128, D], st=ph) for i in range(2)]
            yo = [sb("yo%d" % i, [128, D], st=ph) for i in range(2)]
            xs_ = [sb("xs%d" % i, [128, 8, 128], st=ph) for i in range(2)]
            junk = sb("fjunk", [128, D], st=ph)
            ss = sb("fss", [128, 1], st=ph)
            tiles = list(range((T + 127) // 128))
            if NBLK:
                tiles = tiles[:4 * NBLK] + tiles[-5:]
            for ti in tiles:
                r0 = ti * 128
                m = min(128, T - r0)
                xs = xs_[ti % 2]
                dma("sp", "xs%d" % (ti % 2), xs[:, :, 0:m], xT[:, :, r0:r0 + m].rearrange("k p t -> p k t"), (), xs.u)
                o = xo[ti % 2]
                for hf in range(2):
                    b = banks[(ti * 2 + hf) % 4]
                    for kk in range(4):
                        k = hf * 4 + kk
                        tr(b.t[0:m, kk * 128:(kk + 1) * 128], xs[:, k, 0:m], ident_f[:], xs.u + ident_f.u, b.u)
                    cp("act" if hf == 0 else "dve", o[0:m, hf * 512:(hf + 1) * 512], b.t[0:m, :], b.u, o.u)
                act(junk[0:m, :], o[0:m, :], AF.Square, o.u, junk.u + ss.u, accum=ss[0:m, 0:1])
                act(ss[0:m, :], ss[0:m, :], AF.Sqrt, ss.u + epsc.u, ss.u, bias=epsc[0:m, 0:1], scale=1.0 / D)
                V("dve", lambda e, m=m: e.reciprocal(ss[0:m, :], ss[0:m, :]), ss.u, ss.u)
                y = yo[ti % 2]
                stt(y[0:m, :], o[0:m, :], ss[0:m, 0:1], gfin[0:m, :], ALU.mult, ALU.mult, o.u + ss.u + gfin.u, y.u)
                dma("sp", "yo%d" % (ti % 2), y_out[r0:r0 + m, :], y[0:m, :], y.u, ())

    for l in range(DEPTH):
        p1(l)
        if stage("p1_%d" % l):
            return finish()
        p2(l)
        if stage("p2_%d" % l):
            return finish()
        pu(l)
        p3(l)
        if stage("p3_%d" % l):
            return finish()
        p4(l)
        if stage("p4_%d" % l):
            return finish()
    pfinal()
    return finish()


_CACHE = {}


def _rope_tables():
    half = 8
    inv = (np.float32(500000.0) ** (-(np.arange(0, 16, 2, dtype=np.float32)) / np.float32(16))).astype(np.float32)
    pos = np.concatenate([np.arange(SEQ), np.tile(PAST + np.arange(16), 4)]).astype(np.float32)
    ang = (pos[:, None] * inv[None, :]).astype(np.float32)
    return np.cos(ang).astype(np.float32), np.sin(ang).astype(np.float32)


def make_in_maps(inp):
    f = lambda a: np.ascontiguousarray(np.asarray(a, dtype=np.float32))
    rel = f(inp["rel_bias"])
    q = np.arange(128)[None, :]
    k = np.arange(128)[:, None]
    d3 = np.clip(128 + q - k, -128, 128) + 128
    d4 = np.clip(q - k, -128, 128) + 128
    biasT = np.stack([rel[:, :, d3], rel[:, :, d4]], axis=2)
    bconst = np.ascontiguousarray(np.broadcast_to(rel[:, :, 256][:, :, None], (DEPTH, 8, 128)))
    rc, rs = _rope_tables()
    shared = {
        "w_ada": f(inp["w_ada"]),
        "b_adaT": f(np.asarray(inp["b_ada"]).reshape(DEPTH, 48, 128).transpose(0, 2, 1)),
        "g_attnT": f(np.asarray(inp["g_attn"]).reshape(DEPTH, 8, 128).transpose(0, 2, 1)),
        "g_ffnT": f(np.asarray(inp["g_ffn"]).reshape(DEPTH, 8, 128).transpose(0, 2, 1)),
        "w_in": f(inp["w_in"]),
        "lamv": f(np.concatenate([np.asarray(inp["lam_q1"]), np.asarray(inp["lam_k1"]),
                                  np.asarray(inp["lam_q2"]), np.asarray(inp["lam_k2"])], axis=1)),
        "a_gain": f(inp["a_gain"]),
        "biasT": f(biasT),
        "bconst": f(bconst),
        "b_gain": f(inp["b_gain"]),
        "w_out": f(inp["w_out"]),
        "pk_wq": f(inp["pk_wq"]),
        "pk_keys": f(np.asarray(inp["pk_keys"]).reshape(DEPTH, 16, 128, 128)),
        "pk_u": f(inp["pk_u"]),
        "pk_v": f(inp["pk_v"]),
        "g_final": f(np.asarray(inp["g_final"]).reshape(1, D)),
        "ident": np.eye(128, dtype=np.float32),
        "ropec": rc,
        "ropes": rs,
        "iota_in": np.ascontiguousarray(np.broadcast_to(np.arange(128, dtype=np.float32)[None, :], (128, 128))),
    }
    xp = np.asarray(inp["x_prompt"])
    xs = np.asarray(inp["x_sample"])
    cp_ = np.asarray(inp["c_prompt"])
    cs = np.asarray(inp["c_sample"])
    maps = []
    for c in range(8):
        b = c % 4
        sl = slice(4 * c, 4 * c + 4)
        c5 = np.concatenate([cp_[b:b + 1], cs[sl]], axis=0)
        m = dict(shared)
        m["xin"] = f(np.concatenate([xp[b], xs[sl].reshape(NS, D)], axis=0))
        m["cT"] = f(c5.reshape(5, 8, 128).transpose(2, 1, 0))
        m["cak"] = f(np.asarray(inp["cache_a_k"])[:, sl].reshape(DEPTH, 4, PAST, 512))
        m["cav"] = f(np.asarray(inp["cache_a_v"])[:, sl].reshape(DEPTH, 4, PAST, 512))
        m["cbk"] = f(np.asarray(inp["cache_b_k"])[:, sl].reshape(DEPTH, 4, 512, 512))
        m["cbv"] = f(np.asarray(inp["cache_b_v"])[:, sl].reshape(DEPTH, 4, 512, 512))
        maps.append(m)
    return maps


def assemble(results):
    y_p = np.stack([results[b]["y"][:SEQ] for b in range(4)])
    y_s = np.concatenate([results[c]["y"][SEQ:].reshape(4, 16, D) for c in range(8)])
    nakp = np.stack([results[b]["nak"][:, :SEQ] for b in range(4)], axis=1).reshape(DEPTH, 4, SEQ, 4, 128)
    navp = np.stack([results[b]["nav"][:, :SEQ] for b in range(4)], axis=1).reshape(DEPTH, 4, SEQ, 4, 128)
    nbkp = np.stack([results[b]["nbk_p"] for b in range(4)], axis=1).reshape(DEPTH, 4, 512, 8, 64)
    nbvp = np.stack([results[b]["nbv_p"] for b in range(4)], axis=1).reshape(DEPTH, 4, 512, 8, 64)
    naks = np.concatenate([results[c]["nak"][:, SEQ:].reshape(DEPTH, 4, 16, 4, 128) for c in range(8)], axis=1)
    navs = np.concatenate([results[c]["nav"][:, SEQ:].reshape(DEPTH, 4, 16, 4, 128) for c in range(8)], axis=1)
    nbks = np.concatenate([results[c]["nbk_s"].reshape(DEPTH, 4, 512, 8, 64) for c in range(8)], axis=1)
    nbvs = np.concatenate([results[c]["nbv_s"].reshape(DEPTH, 4, 512, 8, 64) for c in range(8)], axis=1)
    return tuple(np.ascontiguousarray(a, dtype=np.float32) for a in
                 (y_p, y_s, nakp, navp, nbkp, nbvp, naks, navs, nbks, nbvs))


def kernel(**inputs):
    nc, es = build()
    maps = make_in_maps(inputs)
    used = set(build.used_inputs)
    maps = [{k: v for k, v in m.items() if k in used} for m in maps]
    res = run_bass_kernel_spmd(nc, maps, core_ids=list(range(8)))
    return assemble(res.results)
```

```python
import os
import math
import numpy as np
from contextlib import ExitStack
import concourse.bass as bass
import concourse.mybir as mybir
from concourse.bass_utils import run_bass_kernel_spmd

F32 = mybir.dt.float32
BF16 = mybir.dt.bfloat16
U32 = mybir.dt.uint32
AF = mybir.ActivationFunctionType
ALU = mybir.AluOpType
AX = mybir.AxisListType

D = 1024
SEQ = 8192
NS = 64
T = SEQ + NS
DEPTH = 2
PAST = 4096
EPS = 1e-6
NEXP = 16384
NEG = -30000.0


class Unit:
    __slots__ = ("name", "w", "r", "excl")

    def __init__(self, name):
        self.name = name
        self.w = None
        self.r = {}
        self.excl = False


class Op:
    __slots__ = ("eng", "fn", "deps", "inc", "cnt", "dma")


ENGS = ("pe", "act", "dve", "pool", "sp")


class Prog:
    def __init__(self, nc, es):
        self.nc = nc
        self.es = es
        self.ops = {e: [] for e in ENGS}
        self.eng_sem = {e: es.enter_context(nc.semaphore("s_" + e)) for e in ENGS}
        self.dma_sem = {}
        self.dma_cnt = {}
        self.nunits = 0

    def unit(self, name=None):
        self.nunits += 1
        return Unit(name or "u%d" % self.nunits)

    def _dep(self, op, d, kind):
        if d is op:
            return
        if d.dma is None and op.dma is None and d.eng == op.eng and kind != "raw":
            return
        op.deps.append(d)
        if d.dma is None:
            d.inc = True

    def add(self, eng, fn, reads=(), writes=(), dma=None):
        op = Op()
        op.eng = eng
        op.fn = fn
        op.deps = []
        op.inc = False
        op.cnt = 0
        op.dma = dma
        ex = [u for u in reads if u.excl]
        if ex:
            writes = list(writes) + [u for u in ex if u not in writes]
        for u in reads:
            if u.w is not None:
                self._dep(op, u.w, "raw")
        for u in writes:
            if u.w is not None:
                self._dep(op, u.w, "waw")
            for r in u.r.values():
                self._dep(op, r, "war")
        key = dma if dma is not None else eng
        for u in reads:
            u.r[key] = op
        for u in writes:
            u.w = op
            u.r = {}
        if dma is not None:
            if dma not in self.dma_sem:
                self.dma_sem[dma] = self.es.enter_context(self.nc.semaphore("d_" + dma))
                self.dma_cnt[dma] = 0
            self.dma_cnt[dma] += 16
            op.cnt = self.dma_cnt[dma]
        self.ops[eng].append(op)
        return op

    def _sem(self, d):
        return self.dma_sem[d.dma] if d.dma is not None else self.eng_sem[d.eng]

    def barrier(self):
        if os.environ.get("MK_NOBAR"):
            return
        deps = []
        for e in ENGS:
            for op in reversed(self.ops[e]):
                if op.dma is None and op.fn is not None:
                    deps.append(op)
                    break
        lastd = {}
        for e in ENGS:
            for op in self.ops[e]:
                if op.dma is not None:
                    lastd[op.dma] = op
        deps += list(lastd.values())
        for e in ENGS:
            op = Op()
            op.eng = e
            op.fn = None
            op.deps = []
            op.inc = False
            op.cnt = 0
            op.dma = None
            for d in deps:
                if d.dma is None and d.eng == e:
                    continue
                op.deps.append(d)
                if d.dma is None:
                    d.inc = True
            self.ops[e].append(op)

    def emit(self):
        nc = self.nc
        for e in ENGS:
            c = 0
            for op in self.ops[e]:
                if op.dma is None and op.inc:
                    c += 1
                    op.cnt = c
        final = [(self.dma_sem[k], v) for k, v in self.dma_cnt.items()]

        def run(ename, eng):
            waited = {}
            for op in self.ops[ename]:
                need = {}
                for d in op.deps:
                    s = self._sem(d)
                    k = id(s)
                    if k not in need or need[k][1] < d.cnt:
                        need[k] = (s, d.cnt)
                for k, (s, c) in need.items():
                    if waited.get(k, 0) < c:
                        eng.wait_ge(s, c)
                        waited[k] = c
                if op.fn is None:
                    continue
                inst = op.fn(eng)
                if op.dma is not None:
                    inst.then_inc(self.dma_sem[op.dma], 16)
                elif op.inc:
                    inst.then_inc(self.eng_sem[ename], 1)
            if ename == "sp":
                for s, c in final:
                    eng.wait_ge(s, c)

        with nc.Block() as block:
            @block.tensor
            def _(e):
                run("pe", e)

            @block.scalar
            def _(e):
                run("act", e)

            @block.vector
            def _(e):
                run("dve", e)

            @block.gpsimd
            def _(e):
                run("pool", e)

            @block.sync
            def _(e):
                run("sp", e)


class Tl:
    def __init__(self, pg, t, nunits=1):
        self.t = t
        self.u = [pg.unit() for _ in range(nunits)]

    def __getitem__(self, k):
        return self.t[k]


def build(stop=None):
    nc = bass.Bass("TRN2", target_bir_lowering=False)
    es = ExitStack()
    pg = Prog(nc, es)
    stop = stop or os.environ.get("MK_STOP", "")

    def din(name, shape, dt=F32):
        return nc.dram_tensor(name, list(shape), dt, kind="ExternalInput").ap()

    def dout(name, shape, dt=F32):
        return nc.dram_tensor(name, list(shape), dt, kind="ExternalOutput").ap()

    def dscr(name, shape, dt):
        return nc.dram_tensor(name, list(shape), dt).ap()

    IN_SHAPES = {
        "xin": [T, D], "cT": [128, 8, 5], "cak": [DEPTH, 4, PAST, 512], "cav": [DEPTH, 4, PAST, 512],
        "cbk": [DEPTH, 4, 512, 512], "cbv": [DEPTH, 4, 512, 512], "w_ada": [DEPTH, D, 6 * D],
        "b_adaT": [DEPTH, 128, 48], "g_attnT": [DEPTH, 128, 8], "g_ffnT": [DEPTH, 128, 8],
        "w_in": [DEPTH, D, 3072], "lamv": [DEPTH, 256], "a_gain": [DEPTH, 128],
        "biasT": [DEPTH, 8, 2, 128, 128], "bconst": [DEPTH, 8, 128], "b_gain": [DEPTH, 512],
        "w_out": [DEPTH, D, D], "pk_wq": [DEPTH, D, 2048], "pk_keys": [DEPTH, 16, 128, 128],
        "pk_u": [DEPTH, NEXP, D], "pk_v": [DEPTH, NEXP, D], "g_final": [1, D], "ident": [128, 128],
        "ropec": [T, 8], "ropes": [T, 8], "iota_in": [128, 128],
    }
    _ins = {}

    class _IN:
        def __getattr__(self, name):
            if name not in _ins:
                _ins[name] = din(name, IN_SHAPES[name])
            return _ins[name]

    I = _IN()
    build.used_inputs = _ins

    y_out = dout("y", [T, D])
    nak = dout("nak", [DEPTH, T, 512])
    nav = dout("nav", [DEPTH, T, 512])
    nbk_p = dout("nbk_p", [DEPTH, 512, 512])
    nbv_p = dout("nbv_p", [DEPTH, 512, 512])
    nbk_s = dout("nbk_s", [DEPTH, 4, 512, 512])
    nbv_s = dout("nbv_s", [DEPTH, 4, 512, 512])

    xT = dscr("xT", [8, 128, T], F32)
    QTA = dscr("QTA", [4, 128, T], BF16)
    KTA = dscr("KTA", [4, 128, T], BF16)
    VA = dscr("VA", [4, T, 136], BF16)
    QTB = dscr("QTB", [4, 128, T], BF16)
    KTB = dscr("KTB", [4, 128, T], BF16)
    VB = dscr("VB", [T, 8 * 72], BF16)
    UT = dscr("UT", [8, 128, NEXP], BF16)
    VBF = dscr("VBF", [NEXP, D], BF16)
    H2T = dscr("H2T", [8, 128, T], BF16)
    RT = dscr("RT", [3, 128, T], F32)
    u_xT = pg.unit("xT")
    u_qk = pg.unit("qkscr")
    u_uv = pg.unit("uvscr")
    u_h2 = pg.unit("h2scr")
    u_rt = pg.unit("rtscr")
    u_out = pg.unit("outs")

    sbn = [0]

    def sb(name, shape, dt=F32, nunits=1, st=None):
        sbn[0] += 1
        return Tl(pg, (st or es).enter_context(nc.sbuf_tensor("%s_%d" % (name, sbn[0]), list(shape), dt)), nunits)

    banks = [Tl(pg, es.enter_context(nc.psum_tensor("bank%d" % i, [128, 512], F32))) for i in range(8)]

    for b_ in banks:
        b_.u[0].excl = True

    def bk_bf(i):
        return banks[i].t[:].bitcast(BF16)

    PAR_KEYS = {"wbig", "wq", "wout", "biasb"}
    keyunit = {}

    def dma(eng, key, out, in_, reads, writes):
        if key not in PAR_KEYS:
            if key not in keyunit:
                keyunit[key] = pg.unit("k_" + key)
            writes = list(writes) + [keyunit[key]]
        pg.add(eng, lambda e: e.dma_start(out=out, in_=in_), reads, writes, dma=key)

    def mm(out, lhsT, rhs, start, stop, reads, writes):
        pg.add("pe", lambda e: e.matmul(out, lhsT, rhs, start=start, stop=stop), reads, writes)

    def tr(out, in_, idn, reads, writes):
        pg.add("pe", lambda e: e.transpose(out, in_, idn), reads, writes)

    def act(out, in_, func, reads, writes, bias=None, scale=None, accum=None):
        def f(e):
            kw = {}
            if bias is not None:
                kw["bias"] = bias
            if scale is not None:
                kw["scale"] = scale
            if accum is not None:
                kw["accum_out"] = accum
            return e.activation(out=out, in_=in_, func=func, **kw)
        pg.add("act", f, reads, writes)

    def V(eng, fn, reads, writes):
        pg.add(eng, fn, reads, writes)

    def tt(eng, out, a, b, op, reads, writes):
        pg.add(eng, lambda e: e.tensor_tensor(out, a, b, op), reads, writes)

    def ts(eng, out, a, s1, op0, reads, writes, s2=None, op1=None):
        if op1 is None:
            pg.add(eng, lambda e: e.tensor_scalar(out, a, s1, None, op0), reads, writes)
        else:
            pg.add(eng, lambda e: e.tensor_scalar(out, a, s1, s2, op0, op1), reads, writes)

    def stt(out, a, s, b, op0, op1, reads, writes):
        pg.add("dve", lambda e: e.scalar_tensor_tensor(out, a, s, b, op0, op1), reads, writes)

    def vmax(out, in_, reads, writes):
        pg.add("dve", lambda e: e.max(out=out, in_=in_), reads, writes)

    def vmaxidx(out, in_max, in_values, reads, writes):
        pg.add("dve", lambda e: e.max_index(out=out, in_max=in_max, in_values=in_values), reads, writes)

    def vmatch(out, rep, vals, reads, writes):
        pg.add("dve", lambda e: e.match_replace(out=out, in_to_replace=rep, in_values=vals, imm_value=-1e30),
               reads, writes)

    def vredsum(out, in_, reads, writes):
        pg.add("dve", lambda e: e.reduce_sum(out, in_, AX.X), reads, writes)

    def vrecip(out, in_, reads, writes):
        pg.add("dve", lambda e: e.reciprocal(out, in_), reads, writes)

    def cp(eng, out, in_, reads, writes):
        if eng == "act":
            pg.add("act", lambda e: e.copy(out, in_), reads, writes)
        else:
            pg.add(eng, lambda e: e.tensor_copy(out, in_), reads, writes)

    def memset(eng, ap, val, writes):
        pg.add(eng, lambda e: e.memset(ap, val), (), writes)

    ident_f = sb("ident_f", [128, 128])
    ident_b = sb("ident_b", [128, 128], BF16)
    ones_b = sb("ones_b", [128, 128], BF16)
    iota_b = sb("iota_b", [128, 128], BF16)
    iota16 = sb("iota16", [128, 16])
    mk = sb("mk", [128, 5, 128], BF16)
    epsc = sb("epsc", [128, 1])
    dma("sp", "c0", ident_f[:], I.ident[:, :], (), ident_f.u)
    dma("pool", "c1", ident_b[:], I.ident[:, :], (), ident_b.u)
    dma("pool", "c1", iota_b[:], I.iota_in[:, :], (), iota_b.u)
    dma("sp", "c0", iota16[:], I.iota_in[:, 0:16], (), iota16.u)
    memset("dve", ones_b[:], 1.0, ones_b.u)
    memset("dve", epsc[:], EPS, epsc.u)
    memset("dve", mk[:], 0.0, mk.u)
    memset("dve", mk[0:1, 0, 0:64], 0.0, mk.u)
    memset("dve", mk[0:1, 0, 64:128], 1.0, mk.u)
    memset("dve", mk[0:1, 1, 0:64], NEG, mk.u)
    memset("dve", mk[0:1, 1, 64:128], 0.0, mk.u)
    memset("dve", mk[0:1, 2, 0:64], 1.0, mk.u)
    memset("dve", mk[0:1, 2, 64:128], 0.0, mk.u)
    memset("dve", mk[0:1, 3, 0:64], 0.0, mk.u)
    memset("dve", mk[0:1, 3, 64:128], NEG, mk.u)
    memset("dve", mk[0:1, 4, :], 1.0, mk.u)

    NBLK = int(os.environ.get("MK_NB", "0"))
    DBG = bool(os.environ.get("MK_DBG"))
    dbg_xT = dout("dbg_xT", [8, 128, T]) if DBG else None
    dbg_RT = dout("dbg_RT", [3, 128, T]) if DBG else None

    def stage(name):
        return stop == name

    def finish():
        if DBG:
            pg.barrier()
            dma("sp", "dbg", dbg_xT[:, :, :], xT[:, :, :], (), ())
            dma("sp", "dbg", dbg_RT[:, :, :], RT[:, :, :], (), ())
        pg.emit()
        return nc, es

    modT = [sb("modT%d" % l, [128, 48, 5]) for l in range(DEPTH)]
    A1 = [sb("A1_%d" % l, [128, 8, 5]) for l in range(DEPTH)]
    A2 = [sb("A2_%d" % l, [128, 8, 5]) for l in range(DEPTH)]
    lam = sb("lam", [128, DEPTH, 4])
    gainA = sb("gainA", [128, DEPTH, 128])
    gainB = sb("gainB", [128, DEPTH, 512])
    with ExitStack() as ph:
        scT = sb("scT", [128, 8, 5], st=ph)
        dma("sp", "c0", scT[:], I.cT[:, :, :], (), scT.u)
        act(scT[:], scT[:], AF.Silu, scT.u, scT.u)
        badaT = sb("badaT", [128, DEPTH, 48], st=ph)
        gaT = sb("gaT", [128, DEPTH, 8], st=ph)
        gfT = sb("gfT", [128, DEPTH, 8], st=ph)
        for l in range(DEPTH):
            dma("sp", "c0", badaT[:, l, :], I.b_adaT[l], (), badaT.u)
            dma("sp", "c0", gaT[:, l, :], I.g_attnT[l], (), gaT.u)
            dma("sp", "c0", gfT[:, l, :], I.g_ffnT[l], (), gfT.u)
        wada = [sb("wada%d" % i, [128, 8, 512], st=ph) for i in range(2)]
        gi = 0
        for l in range(DEPTH):
            for g in range(12):
                wt = wada[gi % 2]
                gi += 1
                dma("sp", "wada%d" % (gi % 2), wt[:],
                    I.w_ada[l][:, g * 512:(g + 1) * 512].rearrange("(k p) f -> p k f", p=128), (), wt.u)
                for j in range(4):
                    ch = g * 4 + j
                    for k in range(8):
                        mm(banks[7].t[:, ch * 5:ch * 5 + 5], wt[:, k, j * 128:(j + 1) * 128], scT[:, k, :],
                           k == 0, k == 7, wt.u + scT.u, banks[7].u)
            tt("dve", modT[l][:], banks[7].t[:, 0:240].rearrange("p (c r) -> p c r", r=5),
               badaT[:, l, :].unsqueeze(2).to_broadcast([128, 48, 5]), ALU.add, banks[7].u + badaT.u, modT[l].u)
            stt(A1[l][:], modT[l][:, 8:16, :], 1.0, gaT[:, l, :].unsqueeze(2).to_broadcast([128, 8, 5]), ALU.add,
                ALU.mult, modT[l].u + gaT.u, A1[l].u)
            stt(A2[l][:], modT[l][:, 32:40, :], 1.0, gfT[:, l, :].unsqueeze(2).to_broadcast([128, 8, 5]), ALU.add,
                ALU.mult, modT[l].u + gfT.u, A2[l].u)
        lamsrc = sb("lamsrc", [128, DEPTH, 256], st=ph)
        lamt = sb("lamt", [128, 64], st=ph)
        for l in range(DEPTH):
            dma("sp", "c0", lamsrc[:, l, :], I.lamv[l:l + 1, :].partition_broadcast(128), (), lamsrc.u)
            dma("sp", "c0", gainA[:, l, :], I.a_gain[l:l + 1, :].partition_broadcast(128), (), gainA.u)
            dma("sp", "c0", gainB[:, l, :], I.b_gain[l:l + 1, :].partition_broadcast(128), (), gainB.u)
        for l in range(DEPTH):
            lam_init = 0.8 - 0.6 * math.exp(-0.3 * l)
            for i in range(2):
                tt("dve", lamt[:], lamsrc[:, l, i * 128:i * 128 + 64], lamsrc[:, l, i * 128 + 64:i * 128 + 128],
                   ALU.mult, lamsrc.u, lamt.u)
                V("dve", lambda e, l=l, i=i: e.reduce_sum(lam[:, l, i:i + 1], lamt[:], AX.X), lamt.u, lam.u)
            act(lam[:, l, 0:2], lam[:, l, 0:2], AF.Exp, lam.u, lam.u)
            tt("dve", lam[:, l, 2:3], lam[:, l, 1:2], lam[:, l, 0:1], ALU.subtract, lam.u, lam.u)
            ts("dve", lam[:, l, 2:3], lam[:, l, 2:3], -lam_init, ALU.add, lam.u, lam.u)
            ts("dve", gainA[:, l, :], gainA[:, l, :], 1.0 - lam_init, ALU.mult, gainA.u, gainA.u)
        pg.barrier()

    def segs(c0, n):
        out = []
        if c0 < SEQ:
            out.append((0, min(n, SEQ - c0), 0))
        for s in range(4):
            lo, hi = SEQ + 16 * s, SEQ + 16 * s + 16
            a, b = max(lo, c0), min(hi, c0 + n)
            if a < b:
                out.append((a - c0, b - c0, 1 + s))
        return out

    with ExitStack() as ph:
        xld = [sb("xld%d" % i, [128, D], st=ph) for i in range(2)]
        xtt = [sb("xtt%d" % i, [128, 8, 128], st=ph) for i in range(2)]
        ntile = (T + 127) // 128
        for ti in range(ntile):
            r0 = ti * 128
            m = min(128, T - r0)
            xl = xld[ti % 2]
            xo = xtt[ti % 2]
            dma("sp", "xld%d" % (ti % 2), xl[0:m, :], I.xin[r0:r0 + m, :], (), xl.u)
            for hf in range(2):
                b = banks[(ti * 2 + hf) % 4]
                for kk in range(4):
                    k = hf * 4 + kk
                    tr(b.t[:, kk * 128:kk * 128 + m], xl[0:m, k * 128:(k + 1) * 128], ident_f[0:m, 0:m],
                       xl.u + ident_f.u, b.u)
                cp("act" if hf == 0 else "dve", xo[:, hf * 4:(hf + 1) * 4, 0:m],
                   b.t[:, :].rearrange("p (k t) -> p k t", t=128)[:, :, 0:m], b.u, xo.u)
            dma("sp", "xtt%d" % (ti % 2), xT[:, :, r0:r0 + m].rearrange("k p t -> p k t"), xo[:, :, 0:m], xo.u, ())
        pg.barrier()
    if stage("p0"):
        return finish()

    xb = [sb("xb%d" % i, [128, 8, 512]) for i in range(2)]
    xbi = [0]

    def load_xb(c0, n):
        x = xb[xbi[0] % 2]
        xbi[0] += 1
        x.key = "xb%d" % (xbi[0] % 2)
        dma("sp", x.key, x[:, :, 0:n], xT[:, :, c0:c0 + n].rearrange("k p t -> p k t"), (), x.u)
        return x

    def store_xb(x, c0, n):
        dma("sp", x.key, xT[:, :, c0:c0 + n].rearrange("k p t -> p k t"), x[:, :, 0:n], x.u, ())

    class NB:
        def __init__(self, ph, name):
            self.sqb = sb(name + "sqb", [128, 8, 512], BF16, st=ph)
            self.rstd = sb(name + "rstd", [128, 512], st=ph)
            self.xn = sb(name + "xn", [128, 8, 512], st=ph)
            self.h = sb(name + "h", [128, 8, 512], BF16, st=ph)

    def norm_mod(nb, x, c0, n, Aq, Bq, boff, bank):
        sqb, rstd, xn, dst = nb.sqb, nb.rstd, nb.xn, nb.h
        act(sqb[:, :, 0:n], x[:, :, 0:n], AF.Square, x.u, sqb.u)
        for k in range(8):
            mm(bank.t[:, 0:n], ones_b[:], sqb[:, k, 0:n], k == 0, k == 7, ones_b.u + sqb.u, bank.u)
        act(rstd[:, 0:n], bank.t[:, 0:n], AF.Sqrt, bank.u + epsc.u, rstd.u, bias=epsc[:, 0:1], scale=1.0 / D)
        V("dve", lambda e: e.reciprocal(rstd[:, 0:n], rstd[:, 0:n]), rstd.u, rstd.u)
        tt("dve", xn[:, :, 0:n], x[:, :, 0:n], rstd[:, 0:n].unsqueeze(1).to_broadcast([128, 8, n]), ALU.mult,
           x.u + rstd.u, xn.u)
        for (lo, hi, r) in segs(c0, n):
            for k in range(8):
                act(dst[:, k, lo:hi], xn[:, k, lo:hi], AF.Identity, xn.u + Aq.u + Bq.u, dst.u,
                    bias=Bq[:, boff + k, r:r + 1], scale=Aq[:, k, r:r + 1])

    pblocks = [(c, 512) for c in range(0, SEQ, 512)]
    if NBLK:
        pblocks = pblocks[:NBLK] + pblocks[-1:]
    blocks = pblocks + [(SEQ, NS)]

    def p1(l):
        with ExitStack() as ph:
            nb = NB(ph, "p1")
            hT = nb.h
            wbig = sb("wbig", [128, 8, 3072], BF16, nunits=24, st=ph)
            qkf = [sb("qkf%d" % i, [128, 512], st=ph) for i in range(2)]
            qkb = [sb("qkb%d" % i, [128, 512], BF16, st=ph) for i in range(2)]
            vf = [sb("vf%d" % i, [128, 512], st=ph) for i in range(3)]
            vab = [sb("vab%d" % i, [128, 4, 136], BF16, st=ph) for i in range(2)]
            vbb = [sb("vbb%d" % i, [128, 8, 72], BF16, st=ph) for i in range(2)]
            for i in range(2):
                memset("pool", vab[i][:, :, 128:136], 1.0, vab[i].u)
                memset("pool", vbb[i][:, :, 64:72], 1.0, vbb[i].u)
            qkT = [sb("qkT%d" % i, [128, 4, 128], BF16, st=ph) for i in range(2)]
            qkbT = [sb("qkbT%d" % i, [128, 512], BF16, st=ph) for i in range(2)]
            rc = [sb("rc%d" % i, [128, 2, 8], st=ph) for i in range(2)]
            rtmp = sb("rtmp", [128, 4, 8, 8], st=ph)
            for k in range(8):
                for c3 in range(3):
                    dma("pool", "wbig", wbig[:, k, c3 * 1024:(c3 + 1) * 1024],
                        I.w_in[l][k * 128:(k + 1) * 128, c3 * 1024:(c3 + 1) * 1024], (), [wbig.u[k * 3 + c3]])
            cnt = 0
            for (c0, n) in blocks:
                x = load_xb(c0, n)
                norm_mod(nb, x, c0, n, A1[l], modT[l], 0, banks[7])
                outblk = (c0 == SEQ - 512) or (c0 == SEQ)
                for which, col0, scr, scl in ((0, 1536, QTB, 0.125), (1, 2048, KTB, 1.0)):
                    for cc in range(4):
                        b = banks[cnt % 2]
                        o = qkbT[cnt % 2]
                        cnt += 1
                        for k in range(8):
                            mm(b.t[:, 0:n], wbig[:, k, col0 + cc * 128:col0 + (cc + 1) * 128], hT[:, k, 0:n],
                               k == 0, k == 7, wbig.u + hT.u, b.u)
                        act(o[:, 0:n], b.t[:, 0:n], AF.Copy, b.u, o.u, scale=scl)
                        dma("sp", "qkbT%d" % ((cnt - 1) % 2), scr[cc, :, c0:c0 + n], o[:, 0:n], o.u, ())
                for st in range((n + 127) // 128):
                    m = min(128, n - st * 128)
                    r0 = c0 + st * 128
                    rcs = rc[st % 2]
                    dma("sp", "rc%d" % (st % 2), rcs[0:m, 0, :], I.ropec[r0:r0 + m, :], (), rcs.u)
                    dma("sp", "rc%d" % (st % 2), rcs[0:m, 1, :], I.ropes[r0:r0 + m, :], (), rcs.u)
                    for which, col0, scr, scl in ((0, 0, QTA, 0.125), (1, 512, KTA, 1.0)):
                        b = banks[2 + which]
                        f = qkf[which]
                        bb = qkb[which]
                        for k in range(8):
                            mm(b.t[0:m, :], hT[:, k, st * 128:st * 128 + m], wbig[:, k, col0:col0 + 512],
                               k == 0, k == 7, wbig.u + hT.u, b.u)
                        act(f[0:m, :], b.t[0:m, :], AF.Copy, b.u, f.u, scale=scl)
                        fv = f[0:m, :].rearrange("p (g e) -> p g e", e=64)
                        cosb = rcs[0:m, 0, :].unsqueeze(1).to_broadcast([m, 8, 8])
                        sinb = rcs[0:m, 1, :].unsqueeze(1).to_broadcast([m, 8, 8])
                        x1 = fv[:, :, 0:8]
                        x2 = fv[:, :, 8:16]
                        ru = rtmp.u + f.u + rcs.u
                        tt("pool", rtmp[0:m, 0], x1, cosb, ALU.mult, f.u + rcs.u, rtmp.u)
                        tt("pool", rtmp[0:m, 1], x2, sinb, ALU.mult, f.u + rcs.u, rtmp.u)
                        tt("pool", rtmp[0:m, 2], x2, cosb, ALU.mult, f.u + rcs.u, rtmp.u)
                        tt("pool", rtmp[0:m, 3], x1, sinb, ALU.mult, f.u + rcs.u, rtmp.u)
                        tt("pool", x1, rtmp[0:m, 0], rtmp[0:m, 1], ALU.subtract, ru, f.u)
                        tt("pool", x2, rtmp[0:m, 2], rtmp[0:m, 3], ALU.add, ru, f.u)
                        if which == 1:
                            dma("sp", "qkf1", nak[l, r0:r0 + m, :], f[0:m, :], f.u, ())
                        cp("dve", bb[0:m, :], f[0:m, :], f.u, bb.u)
                        tb = banks[4 + which]
                        o = qkT[which]
                        for h in range(4):
                            tr(tb.t[:, :].bitcast(BF16)[:, h * 128:h * 128 + m], bb[0:m, h * 128:(h + 1) * 128],
                               ident_b[0:m, 0:m], bb.u + ident_b.u, tb.u)
                        cp("dve", o[:, :, 0:m],
                           tb.t[:, :].bitcast(BF16)[:, 0:512].rearrange("p (h t) -> p h t", t=128)[:, :, 0:m], tb.u, o.u)
                        dma("sp", "qkT%d" % which, scr[:, :, r0:r0 + m].rearrange("h p t -> p h t"), o[:, :, 0:m], o.u, ())
                    b = banks[6]
                    for k in range(8):
                        mm(b.t[0:m, :], hT[:, k, st * 128:st * 128 + m], wbig[:, k, 1024:1536], k == 0, k == 7,
                           wbig.u + hT.u, b.u)
                    f = vf[0]
                    act(f[0:m, :], b.t[0:m, :], AF.Copy, b.u, f.u)
                    dma("sp", "vf0", nav[l, r0:r0 + m, :], f[0:m, :], f.u, ())
                    vb_ = vab[st % 2]
                    cp("dve", vb_[0:m, :, 0:128], b.t[0:m, :].rearrange("p (h e) -> p h e", e=128), b.u, vb_.u)
                    dma("sp", "vab%d" % (st % 2), VA[:, r0:r0 + m, :].rearrange("h t e -> t h e"), vb_[0:m, :, :], vb_.u, ())
                    b = banks[7]
                    for k in range(8):
                        mm(b.t[0:m, :], hT[:, k, st * 128:st * 128 + m], wbig[:, k, 2560:3072], k == 0, k == 7,
                           wbig.u + hT.u, b.u)
                    vb_ = vbb[st % 2]
                    cp("dve", vb_[0:m, :, 0:64], b.t[0:m, :].rearrange("p (h e) -> p h e", e=64), b.u, vb_.u)
                    dma("sp", "vbb%d" % (st % 2), VB[r0:r0 + m, :], vb_[0:m, :, :].rearrange("p h e -> p (h e)"), vb_.u, ())
                    if outblk:
                        f = vf[1]
                        act(f[0:m, :], b.t[0:m, :], AF.Copy, b.u, f.u)
                        if c0 == SEQ:
                            for s in range(4):
                                dma("sp", "vf1", nbv_s[l, s, 496:512, :], f[16 * s:16 * s + 16, :], f.u, ())
                        else:
                            dma("sp", "vf1", nbv_p[l, st * 128:st * 128 + m, :], f[0:m, :], f.u, ())
                        b = banks[6]
                        for k in range(8):
                            mm(b.t[0:m, :], hT[:, k, st * 128:st * 128 + m], wbig[:, k, 2048:2560], k == 0, k == 7,
                               wbig.u + hT.u, b.u)
                        f = vf[2]
                        act(f[0:m, :], b.t[0:m, :], AF.Copy, b.u, f.u)
                        if c0 == SEQ:
                            for s in range(4):
                                dma("sp", "vf2", nbk_s[l, s, 496:512, :], f[16 * s:16 * s + 16, :], f.u, ())
                        else:
                            dma("sp", "vf2", nbk_p[l, st * 128:st * 128 + m, :], f[0:m, :], f.u, ())
            for s in range(4):
                dma("sp", "roll", nbk_s[l, s, 0:496, :], I.cbk[l, s, 16:512, :], (), ())
                dma("sp", "roll", nbv_s[l, s, 0:496, :], I.cbv[l, s, 16:512, :], (), ())
            pg.barrier()

    def mmg(out, lhsT, rhs, start, stop, reads, writes):
        pg.add("pe", lambda e: e.matmul(out, lhsT, rhs, start=start, stop=stop, skip_group_check=True), reads, writes)

    def p2(l):
        P2F = int(os.environ.get('MK_P2F', '15'))
        with ExitStack() as ph:
            wout = sb("wout", [128, 8, 1024], BF16, nunits=8, st=ph)
            for k in range(8):
                dma("pool", "wout", wout[:, k, :], I.w_out[l][k * 128:(k + 1) * 128, :], (), [wout.u[k]])
            bias_b = sb("bias_b", [128, 8, 2, 128], BF16, nunits=16, st=ph)
            for hb in range(8):
                for j2 in range(2):
                    dma("pool", "biasb", bias_b[:, hb, j2, :], I.biasT[l, hb, j2], (), [bias_b.u[hb * 2 + j2]])
            cbrow = sb("cbrow", [128, 8, 128], BF16, st=ph)
            memset("pool", cbrow[:], 0.0, cbrow.u)
            dma("pool", "biasb", cbrow[0:1, :, :], I.bconst[l:l + 1, :, :], (), cbrow.u)
            qta = [sb("qta%d" % i, [128, 4, 512], BF16, st=ph) for i in range(2)]
            qtb = [sb("qtb%d" % i, [128, 4, 512], BF16, st=ph) for i in range(2)]
            kta = sb("kta", [128, SEQ], BF16, nunits=8, st=ph)
            vat = sb("vat", [128, 64, 136], BF16, nunits=8, st=ph)
            ktb = [sb("ktb%d" % i, [128, 4, 1024], BF16, st=ph) for i in range(2)]
            vbt = [sb("vbt%d" % i, [128, 8, 576], BF16, st=ph) for i in range(2)]
            pT = [sb("pT%d" % i, [128, 512], BF16, st=ph) for i in range(6)]
            oblk = sb("oblk", [128, 4, 1024], BF16, st=ph)
            oT = sb("oT", [128, 8, 512], BF16, st=ph)
            rz = sb("rz", [128, 8], st=ph)
            t1 = sb("t1", [128, 128], st=ph)
            oa = sb("oa", [128, 512], st=ph)
            junk = sb("junk", [128, 512], st=ph)
            ss = sb("ss", [128, 1], st=ph)

            def accA(m_, rr):
                idx = m_ * 4 + rr
                bk = banks[4 + idx // 3]
                c = (idx % 3) * 129
                return bk, bk.t[:, c:c + 129], (idx % 3 == 0)

            def epiA(h, rr, np_, dst):
                b0, a0, _ = accA(0, rr)
                b1, a1, _ = accA(1, rr)
                V("dve", lambda e: e.reciprocal(rz[0:np_, 0:1], a0[0:np_, 128:129]), b0.u, rz.u)
                V("dve", lambda e: e.reciprocal(rz[0:np_, 1:2], a1[0:np_, 128:129]), b1.u, rz.u)
                ts("dve", rz[0:np_, 1:2], rz[0:np_, 1:2], lam[0:np_, l, 2:3], ALU.mult, rz.u + lam.u, rz.u)
                ts("dve", t1[0:np_, :], a1[0:np_, 0:128], rz[0:np_, 1:2], ALU.mult, b1.u + rz.u, t1.u)
                stt(oa[0:np_, 0:128], a0[0:np_, 0:128], rz[0:np_, 0:1], t1[0:np_, :], ALU.mult, ALU.add,
                    b0.u + rz.u + t1.u, oa.u)
                act(junk[0:np_, 0:128], oa[0:np_, 0:128], AF.Square, oa.u, junk.u + ss.u, accum=ss[0:np_, 0:1])
                act(ss[0:np_, :], ss[0:np_, :], AF.Sqrt, ss.u + epsc.u, ss.u, bias=epsc[0:np_, 0:1], scale=1.0 / 128)
                V("dve", lambda e: e.reciprocal(ss[0:np_, :], ss[0:np_, :]), ss.u, ss.u)
                stt(dst, oa[0:np_, 0:128], ss[0:np_, 0:1], gainA[0:np_, l, :], ALU.mult, ALU.mult,
                    oa.u + ss.u + gainA.u, oblk.u)

            def epiB(np_, dst, w):
                for hf in range(2):
                    bk = banks[4 + hf]
                    v = bk.t[0:np_, 0:4 * w].rearrange("p (h e) -> p h e", e=w)
                    V("dve", lambda e, v=v, hf=hf: e.reciprocal(rz[0:np_, hf * 4:hf * 4 + 4], v[:, :, 64]), bk.u, rz.u)
                    tt("dve", oa[0:np_, hf * 256:(hf + 1) * 256].rearrange("p (h e) -> p h e", e=64), v[:, :, 0:64],
                       rz[0:np_, hf * 4:hf * 4 + 4].unsqueeze(2).to_broadcast([np_, 4, 64]), ALU.mult,
                       bk.u + rz.u, oa.u)
                act(junk[0:np_, :], oa[0:np_, :], AF.Square, oa.u, junk.u + ss.u, accum=ss[0:np_, 0:1])
                act(ss[0:np_, :], ss[0:np_, :], AF.Sqrt, ss.u + epsc.u, ss.u, bias=epsc[0:np_, 0:1], scale=1.0 / 512)
                V("dve", lambda e: e.reciprocal(ss[0:np_, :], ss[0:np_, :]), ss.u, ss.u)
                stt(dst, oa[0:np_, :], ss[0:np_, 0:1], gainB[0:np_, l, :], ALU.mult, ALU.mult,
                    oa.u + ss.u + gainB.u, oblk.u)

            def outproj(x, c0, n):
                for dc in range(8):
                    yb = banks[2 + dc % 2]
                    for kc in range(8):
                        mm(yb.t[:, 0:n], wout[:, kc, dc * 128:(dc + 1) * 128], oT[:, kc, 0:n], kc == 0, kc == 7,
                           wout.u + oT.u, yb.u)
                    for (lo, hi, r) in segs(c0, n):
                        stt(x[:, dc, lo:hi], yb.t[:, lo:hi], modT[l][:, 16 + dc, r:r + 1], x[:, dc, lo:hi],
                            ALU.mult, ALU.add, yb.u + modT[l].u + x.u, x.u)
                store_xb(x, c0, n)

            pcnt = 0
            for bi, (c0, n) in enumerate(pblocks):
                j = c0 // 512
                x = load_xb(c0, n)
                qa = qta[bi % 2]
                qb = qtb[bi % 2]
                dma("sp", "qta%d" % (bi % 2), qa[:], QTA[:, :, c0:c0 + 512].rearrange("h p t -> p h t"), (), qa.u)
                dma("sp", "qtb%d" % (bi % 2), qb[:], QTB[:, :, c0:c0 + 512].rearrange("h p t -> p h t"), (), qb.u)
                nkt = 4 * (j + 1)
                for h in range(4 if P2F & 1 else 0):
                    for u8 in range((nkt * 128 + 1023) // 1024):
                        lo = u8 * 1024
                        hi = min(lo + 1024, nkt * 128)
                        dma("sp", "kta%d" % u8, kta[:, lo:hi], KTA[h, :, lo:hi], (), [kta.u[u8]])
                        dma("sp", "vat%d" % u8, vat[:, lo // 128:hi // 128, :],
                            VA[h, lo:hi, :].rearrange("(kt p) e -> p kt e", p=128), (), [vat.u[u8]])
                    def emit_pv(kt, pts_):
                        r = kt - 4 * j
                        u8 = kt // 8
                        for m_ in range(2):
                            pt = pts_[m_]
                            for rr in range(max(r, 0), 4):
                                bk, ap_, first = accA(m_, rr)
                                mmg(ap_, pt[:, rr * 128:(rr + 1) * 128], vat[:, kt, 0:129],
                                    (kt == 0 and first), kt == 4 * j + rr, pt.u + [vat.u[u8]], bk.u)

                    pend = None
                    for kt in range(nkt):
                        r = kt - 4 * j
                        cs = max(r, 0) * 128
                        u8 = kt // 8
                        cur = []
                        for m_ in range(2):
                            Sb = banks[pcnt % 4]
                            pt = pT[pcnt % 6]
                            pcnt += 1
                            mm(Sb.t[:, cs:512], kta[64 * m_:64 * m_ + 64, kt * 128:(kt + 1) * 128],
                               qa[64 * m_:64 * m_ + 64, h, cs:512], True, r < 0, [kta.u[u8]] + qa.u, Sb.u)
                            if r >= 0:
                                mm(Sb.t[:, cs:cs + 128], mk[:, 0, :], mk[:, 1, :], False, True, mk.u, Sb.u)
                            act(pt[:, cs:512], Sb.t[:, cs:512], AF.Exp, Sb.u, pt.u)
                            cur.append(pt)
                        if pend is not None:
                            emit_pv(*pend)
                        pend = (kt, cur)
                    emit_pv(*pend)
                    for rr in range(4):
                        epiA(h, rr, 128, oblk[:, rr, h * 128:(h + 1) * 128])
                lo_t = max(0, 4 * j - 4)
                hi_t = 4 * j + 4
                kb_ = ktb[bi % 2]
                vb_ = vbt[bi % 2]
                nkl = (hi_t - lo_t) * 128
                dma("sp", "ktb%d" % (bi % 2), kb_[:, :, 0:nkl],
                    KTB[:, :, lo_t * 128:hi_t * 128].rearrange("c p t -> p c t"), (), kb_.u)
                dma("sp", "vbt%d" % (bi % 2), vb_[:, 0:hi_t - lo_t, :],
                    VB[lo_t * 128:hi_t * 128, :].rearrange("(kt p) e -> p kt e", p=128), (), vb_.u)
                for rr in range(4 if P2F & 2 else 0):
                    i_ = 4 * j + rr
                    jjs = [jj for jj in range(5) if i_ - 4 + jj >= 0]
                    for jj in jjs:
                        ktl = i_ - 4 + jj - lo_t
                        for hf in range(2):
                            Sb = banks[pcnt % 4]
                            pt = pT[pcnt % 6]
                            pcnt += 1
                            for hh in range(4):
                                hb = hf * 4 + hh
                                cc = hb // 2
                                prt = (hb % 2) * 64
                                cols = Sb.t[:, hh * 128:(hh + 1) * 128]
                                mm(cols, kb_[prt:prt + 64, cc, ktl * 128:(ktl + 1) * 128],
                                   qb[prt:prt + 64, cc, rr * 128:(rr + 1) * 128], True, False, kb_.u + qb.u, Sb.u)
                                if jj <= 2:
                                    mm(cols, mk[:, 4, :], cbrow[:, hb, :], False, jj != 0, mk.u + cbrow.u, Sb.u)
                                    if jj == 0:
                                        mm(cols, mk[:, 2, :], mk[:, 3, :], False, True, mk.u, Sb.u)
                                elif jj == 3:
                                    mm(cols, ident_b[:], bias_b[:, hb, 0, :], False, True, ident_b.u + bias_b.u, Sb.u)
                                else:
                                    mm(cols, ident_b[:], bias_b[:, hb, 1, :], False, False, ident_b.u + bias_b.u, Sb.u)
                                    mm(cols, mk[:, 0, :], mk[:, 1, :], False, True, mk.u, Sb.u)
                            act(pt[:, :], Sb.t[:, :], AF.Exp, Sb.u, pt.u)
                            for hh in range(4):
                                hb = hf * 4 + hh
                                bk = banks[4 + hf]
                                mmg(bk.t[:, hh * 65:hh * 65 + 65], pt[:, hh * 128:(hh + 1) * 128],
                                    vb_[:, ktl, hb * 72:hb * 72 + 65], (jj == jjs[0] and hh == 0), jj == 4,
                                    pt.u + vb_.u, bk.u)
                    epiB(128, oblk[:, rr, 512:1024], 65)
                if not P2F & 4:
                    continue
                for rr in range(4):
                    tb = banks[rr % 2]
                    tbv = tb.t[:, :].bitcast(BF16)
                    for kc in range(8):
                        tr(tbv[:, kc * 128:(kc + 1) * 128], oblk[:, rr, kc * 128:(kc + 1) * 128], ident_b[:],
                           oblk.u + ident_b.u, tb.u)
                    cp("act" if rr % 2 == 0 else "dve", oT[:, :, rr * 128:(rr + 1) * 128],
                       tbv.rearrange("p (k t) -> p k t", t=128), tb.u, oT.u)
                outproj(x, c0, 512)

            x = load_xb(SEQ, NS)
            qas = sb("qas", [128, 4, NS], BF16, st=ph)
            qbs = sb("qbs", [128, 4, NS], BF16, st=ph)
            kan = sb("kan", [128, 4, NS], BF16, st=ph)
            kbn = sb("kbn", [128, 4, NS], BF16, st=ph)
            van = [sb("van%d" % s_, [16, 4, 136], BF16, st=ph) for s_ in range(4)]
            vbn = [sb("vbn%d" % s_, [16, 576], BF16, st=ph) for s_ in range(4)]
            dma("sp", "smp", qas[:], QTA[:, :, SEQ:T].rearrange("h p t -> p h t"), (), qas.u)
            dma("sp", "smp", qbs[:], QTB[:, :, SEQ:T].rearrange("h p t -> p h t"), (), qbs.u)
            dma("sp", "smp", kan[:], KTA[:, :, SEQ:T].rearrange("h p t -> p h t"), (), kan.u)
            dma("sp", "smp", kbn[:], KTB[:, :, SEQ:T].rearrange("h p t -> p h t"), (), kbn.u)
            for s_ in range(4):
                dma("sp", "smp", van[s_][:], VA[:, SEQ + 16 * s_:SEQ + 16 * s_ + 16, :].rearrange("h t e -> t h e"),
                    (), van[s_].u)
                dma("sp", "smp", vbn[s_][:], VB[SEQ + 16 * s_:SEQ + 16 * s_ + 16, :], (), vbn[s_].u)
            kcb = [sb("kcb%d" % i, [128, 512], BF16, st=ph) for i in range(2)]
            vca = [sb("vca%d" % i, [128, 4, 136], BF16, st=ph) for i in range(2)]
            vcb = [sb("vcb%d" % i, [128, 8, 72], BF16, st=ph) for i in range(2)]
            for i in range(2):
                memset("pool", vca[i][:, :, 128:136], 1.0, vca[i].u)
                memset("pool", vcb[i][:, :, 64:72], 1.0, vcb[i].u)
            kTs = [sb("kTs%d" % i, [128, 4, 128], BF16, st=ph) for i in range(2)]
            pts = [sb("pts%d" % i, [128, 128], BF16, st=ph) for i in range(2)]
            os_ = sb("os_", [16, 1024], BF16, st=ph)
            scnt = 0
            for s_ in range(4 if P2F & 8 else 0):
                q0 = 16 * s_
                SF = int(os.environ.get('MK_SF', '31'))
                for kt in ([int(v) for v in os.environ['MK_SK'].split(',')] if os.environ.get('MK_SK') else range(33 if SF & 1 else 0)):
                    kk = 128 if kt < 32 else 16
                    if kt < 32:
                        kc = kcb[scnt % 2]
                        vc = vca[scnt % 2]
                        kT = kTs[scnt % 2]
                        dma("pool", "kcb%d" % (scnt % 2), kc[:], I.cak[l, s_, kt * 128:(kt + 1) * 128, :], (), kc.u)
                        dma("pool", "vca%d" % (scnt % 2), vc[:, :, 0:128],
                            I.cav[l, s_, kt * 128:(kt + 1) * 128, :].rearrange("t (h e) -> t h e", e=128), (), vc.u)
                        tb = banks[0]
                        tbv = tb.t[:, :].bitcast(BF16)
                        for h in range(4):
                            tr(tbv[:, h * 128:(h + 1) * 128], kc[:, h * 128:(h + 1) * 128], ident_b[:],
                               kc.u + ident_b.u, tb.u)
                        cp("dve", kT[:], tbv[:, 0:512].rearrange("p (h t) -> p h t", t=128), tb.u, kT.u)
                        kTv = lambda h, m_, kT=kT: kT[64 * m_:64 * m_ + 64, h, :]
                        kTu = kT.u
                        vv = lambda h, vc=vc: vc[:, h, 0:129]
                        vu = vc.u
                    else:
                        if os.environ.get('MK_SWAP'):
                            kTv = lambda h, m_: kbn[64 * m_:64 * m_ + 64, h, q0:q0 + 16]
                        else:
                            kTv = lambda h, m_: kan[64 * m_:64 * m_ + 64, h, q0:q0 + 16]
                        kTu = kan.u + kbn.u
                        vv = lambda h: van[s_][0:16, h, 0:129]
                        vu = van[s_].u
                    Sbs = [(banks[1], banks[2]), (banks[3], banks[7])][scnt % 2]
                    pt = pts[scnt % 2]
                    scnt += 1
                    SX = int(os.environ.get('MK_SX', '7'))
                    VV_ = os.environ.get('MK_V', '')
                    for m_ in range((1 if VV_ == 'm0' else 2) if SX & 1 else 0):
                        for h in range(4):
                            g_ = h * 2 + m_
                            qq = qbs if os.environ.get('MK_SWAP') else qas
                            mm(Sbs[m_].t[0:kk, h * 16:h * 16 + 16], kTv(h, m_), qq[64 * m_:64 * m_ + 64, h, q0:q0 + 16],
                               True, True, kTu + qas.u + qbs.u, Sbs[m_].u)
                    for m_ in range(2):
                        act(pt[0:kk, m_ * 64:(m_ + 1) * 64], Sbs[m_].t[0:kk, 0:64], AF.Exp, Sbs[m_].u, pt.u)
                    for m_ in range(2 if SX & 4 else 0):
                        for h in range(4):
                            g_ = m_ * 4 + h
                            bk, ap_, first = accA(m_, h)
                            mmg(ap_[0:16, :], pt[0:kk, g_ * 16:g_ * 16 + 16], vv(h), (kt == 0 and first), kt == 32,
                                pt.u + vu, bk.u)
                for h in range(4 if SF & 2 else 0):
                    epiA(h, h, 16, os_[0:16, h * 128:(h + 1) * 128])
                for kt in range(5 if SF & 4 else 0):
                    kk = 128 if kt < 4 else 16
                    if kt < 4:
                        kc = kcb[scnt % 2]
                        vc = vcb[scnt % 2]
                        kT = kTs[scnt % 2]
                        dma("pool", "kcb%d" % (scnt % 2), kc[:], I.cbk[l, s_, kt * 128:(kt + 1) * 128, :], (), kc.u)
                        dma("pool", "vcb%d" % (scnt % 2), vc[:, :, 0:64],
                            I.cbv[l, s_, kt * 128:(kt + 1) * 128, :].rearrange("t (h e) -> t h e", e=64), (), vc.u)
                        tb = banks[scnt % 2]
                        tbv = tb.t[:, :].bitcast(BF16)
                        for cc in range(4):
                            tr(tbv[:, cc * 128:(cc + 1) * 128], kc[:, cc * 128:(cc + 1) * 128], ident_b[:],
                               kc.u + ident_b.u, tb.u)
                        cp("dve", kT[:], tbv[:, 0:512].rearrange("p (h t) -> p h t", t=128), tb.u, kT.u)
                        kTv = lambda cc, prt, kT=kT: kT[prt:prt + 64, cc, :]
                        kTu = kT.u
                        vv = lambda hb, vc=vc: vc[:, hb, 0:65]
                        vu = vc.u
                    else:
                        kTv = lambda cc, prt: kbn[prt:prt + 64, cc, q0:q0 + 16]
                        kTu = kbn.u
                        vv = lambda hb: vbn[s_][0:16, hb * 72:hb * 72 + 65]
                        vu = vbn[s_].u
                    Sb = banks[2 + scnt % 2]
                    pt = pts[scnt % 2]
                    scnt += 1
                    for hb in range(8):
                        cc = hb // 2
                        prt = (hb % 2) * 64
                        cols = Sb.t[0:kk, hb * 16:hb * 16 + 16]
                        mm(cols, kTv(cc, prt), qbs[prt:prt + 64, cc, q0:q0 + 16], True, False, kTu + qbs.u, Sb.u)
                        if kt <= 2:
                            mm(cols, mk[:, 4, 0:kk], cbrow[:, hb, 0:16], False, True, mk.u + cbrow.u, Sb.u)
                        elif kt == 3:
                            mm(cols, ident_b[:], bias_b[:, hb, 0, 0:16], False, True, ident_b.u + bias_b.u, Sb.u)
                        else:
                            mm(cols, ident_b[:, 0:16], bias_b[:, hb, 1, 0:16], False, True,
                               ident_b.u + bias_b.u, Sb.u)
                    act(pt[0:kk, :], Sb.t[0:kk, 0:128], AF.Exp, Sb.u, pt.u)
                    for hb in range(8):
                        bk = banks[4 + hb // 4]
                        hh = hb % 4
                        mmg(bk.t[0:16, hh * 65:hh * 65 + 65], pt[0:kk, hb * 16:hb * 16 + 16], vv(hb),
                            (kt == 0 and hh == 0), kt == 4, pt.u + vu, bk.u)
                if SF & 8:
                    epiB(16, os_[0:16, 512:1024], 65)
                if not SF & 16:
                    continue
                tb = banks[scnt % 2]
                tbv = tb.t[:, :].bitcast(BF16)
                for kc_ in range(8):
                    tr(tbv[:, kc_ * 16:kc_ * 16 + 16], os_[0:16, kc_ * 128:(kc_ + 1) * 128], ident_b[0:16, 0:16],
                       oblk.u + ident_b.u, tb.u)
                cp("dve", oT[:, :, q0:q0 + 16], tbv[:, 0:128].rearrange("p (k t) -> p k t", t=16), tb.u, oT.u)
            if P2F & 8:
                outproj(x, SEQ, NS)
            pg.barrier()

    def pu(l):
        with ExitStack() as ph:
            ubuf = [sb("ubuf%d" % i, [128, D], BF16, st=ph) for i in range(2)]
            vbuf = [sb("vbuf%d" % i, [128, D], BF16, st=ph) for i in range(2)]
            utb = [sb("utb%d" % i, [128, 8, 128], BF16, st=ph) for i in range(2)]
            for ec in range(128):
                ub = ubuf[ec % 2]
                vb_ = vbuf[ec % 2]
                ut = utb[ec % 2]
                dma("pool", "ubuf%d" % (ec % 2), ub[:], I.pk_u[l, ec * 128:(ec + 1) * 128, :], (), ub.u)
                dma("pool", "vbuf%d" % (ec % 2), vb_[:], I.pk_v[l, ec * 128:(ec + 1) * 128, :], (), vb_.u)
                tb = banks[ec % 2]
                tbv = tb.t[:, :].bitcast(BF16)
                for k in range(8):
                    tr(tbv[:, k * 128:(k + 1) * 128], ub[:, k * 128:(k + 1) * 128], ident_b[:], ub.u + ident_b.u, tb.u)
                cp("act" if ec % 2 == 0 else "dve", ut[:], tbv.rearrange("p (k e) -> p k e", e=128), tb.u, ut.u)
                dma("sp", "utb%d" % (ec % 2), UT[:, :, ec * 128:(ec + 1) * 128].rearrange("k p e -> p k e"), ut[:], ut.u, ())
                dma("sp", "vbufo%d" % (ec % 2), VBF[ec * 128:(ec + 1) * 128, :], vb_[:], vb_.u, ())
            pg.barrier()

    def p3(l):
        with ExitStack() as ph:
            nb = NB(ph, "p3")
            h2 = nb.h
            wq = sb("wq", [128, 8, 2048], BF16, nunits=16, st=ph)
            for k in range(8):
                for c2 in range(2):
                    dma("pool", "wq", wq[:, k, c2 * 1024:(c2 + 1) * 1024],
                        I.pk_wq[l][k * 128:(k + 1) * 128, c2 * 1024:(c2 + 1) * 1024], (), [wq.u[k * 2 + c2]])
            kraw = sb("kraw", [128, 16, 128], BF16, st=ph)
            keysT = sb("keysT", [128, 16, 128], BF16, st=ph)
            for g in range(16):
                dma("pool", "kraw", kraw[:, g, :], I.pk_keys[l, g], (), kraw.u)
            for g in range(16):
                tb = banks[g % 2]
                tbv = tb.t[:, :].bitcast(BF16)
                tr(tbv[:, 0:128], kraw[:, g, :], ident_b[:], kraw.u + ident_b.u, tb.u)
                cp("dve", keysT[:, g, :], tbv[:, 0:128], tb.u, keysT.u)
            qT = sb("qT", [128, 16, 512], BF16, st=ph)
            sc = sb("sc", [128, 16, 128], st=ph)
            sc2 = sb("sc2", [128, 16, 128], st=ph)
            sv = sb("sv", [128, 16, 16], st=ph)
            si = sb("si", [128, 16, 16], U32, st=ph)
            sif = sb("sif", [128, 16, 16], st=ph)
            cand = sb("cand", [128, 8, 256], st=ph)
            cand2 = sb("cand2", [128, 8, 256], st=ph)
            cv = sb("cv", [128, 8, 16], st=ph)
            ci = sb("ci", [128, 8, 16], U32, st=ph)
            cab = sb("cab", [128, 2, 8, 16], U32, st=ph)
            cabf = sb("cabf", [128, 2, 8, 16], st=ph)
            eq = sb("eq", [128, 8, 16, 16], st=ph)
            res = sb("res", [128, 3, 128], st=ph)
            gs = sb("gs", [128, 8], st=ph)
            rts = [sb("rts%d" % i, [128, 3, 128], st=ph) for i in range(2)]
            bcnt = 0
            for (c0, n) in blocks:
                x = load_xb(c0, n)
                norm_mod(nb, x, c0, n, A2[l], modT[l], 24, banks[7])
                dma("sp", "h2st", H2T[:, :, c0:c0 + n].rearrange("k p t -> p k t"), h2[:, :, 0:n], h2.u, ())
                for qc in range(16):
                    b = banks[qc % 2]
                    for k in range(8):
                        mm(b.t[:, 0:n], wq[:, k, qc * 128:(qc + 1) * 128], h2[:, k, 0:n], k == 0, k == 7,
                           wq.u + h2.u, b.u)
                    cp("act" if qc % 2 == 0 else "dve", qT[:, qc, 0:n], b.t[:, 0:n], b.u, qT.u)
                for st in range((n + 127) // 128):
                    m = min(128, n - st * 128)
                    r0 = c0 + st * 128
                    for qc in range(16):
                        b = banks[2 + qc // 4]
                        mm(b.t[0:m, (qc % 4) * 128:(qc % 4 + 1) * 128], qT[:, qc, st * 128:st * 128 + m],
                           keysT[:, qc, :], True, True, qT.u + keysT.u, b.u)
                    for q4 in range(4):
                        b = banks[2 + q4]
                        cp("act", sc[0:m, q4 * 4:(q4 + 1) * 4, :], b.t[0:m, :].rearrange("p (g n) -> p g n", n=128),
                           b.u, sc.u)
                    for g in range(16):
                        vmax(sv[0:m, g, 0:8], sc[0:m, g, :], sc.u, sv.u)
                    for g in range(16):
                        vmaxidx(si[0:m, g, 0:8], sv[0:m, g, 0:8], sc[0:m, g, :], sc.u + sv.u, si.u)
                    for g in range(16):
                        vmatch(sc2[0:m, g, :], sv[0:m, g, 0:8], sc[0:m, g, :], sc.u + sv.u, sc2.u)
                    for g in range(16):
                        vmax(sv[0:m, g, 8:16], sc2[0:m, g, :], sc2.u, sv.u)
                    for g in range(16):
                        vmaxidx(si[0:m, g, 8:16], sv[0:m, g, 8:16], sc2[0:m, g, :], sc2.u + sv.u, si.u)
                    svv = sv[0:m].rearrange("p (h two) a -> p h two a", two=2)
                    tt("dve", cand[0:m].rearrange("p h (a b) -> p h a b", b=16),
                       svv[:, :, 0, :].unsqueeze(3).to_broadcast([m, 8, 16, 16]),
                       svv[:, :, 1, :].unsqueeze(2).to_broadcast([m, 8, 16, 16]), ALU.add, sv.u, cand.u)
                    for h in range(8):
                        vmax(cv[0:m, h, 0:8], cand[0:m, h, :], cand.u, cv.u)
                    for h in range(8):
                        vmaxidx(ci[0:m, h, 0:8], cv[0:m, h, 0:8], cand[0:m, h, :], cand.u + cv.u, ci.u)
                    for h in range(8):
                        vmatch(cand2[0:m, h, :], cv[0:m, h, 0:8], cand[0:m, h, :], cand.u + cv.u, cand2.u)
                    for h in range(8):
                        vmax(cv[0:m, h, 8:16], cand2[0:m, h, :], cand2.u, cv.u)
                    for h in range(8):
                        vmaxidx(ci[0:m, h, 8:16], cv[0:m, h, 8:16], cand2[0:m, h, :], cand2.u + cv.u, ci.u)
                    ts("dve", cab[0:m, 0], ci[0:m], 4, ALU.logical_shift_right, ci.u, cab.u)
                    ts("dve", cab[0:m, 1], ci[0:m], 15, ALU.bitwise_and, ci.u, cab.u)
                    cp("dve", cabf[0:m], cab[0:m], cab.u, cabf.u)
                    cp("dve", sif[0:m], si[0:m], si.u, sif.u)
                    sifv = sif[0:m].rearrange("p (h two) a -> p h two a", two=2)
                    for w2 in range(2):
                        tt("dve", eq[0:m], cabf[0:m, w2].unsqueeze(3).to_broadcast([m, 8, 16, 16]),
                           iota16[0:m, :].unsqueeze(1).unsqueeze(1).to_broadcast([m, 8, 16, 16]), ALU.is_equal,
                           cabf.u + iota16.u, eq.u)
                        tt("dve", eq[0:m], eq[0:m], sifv[:, :, w2, :].unsqueeze(2).to_broadcast([m, 8, 16, 16]),
                           ALU.mult, eq.u + sif.u, eq.u)
                        vredsum(res[0:m, w2, :].rearrange("p (h k) -> p h k", k=16), eq[0:m], eq.u, res.u)
                    resg = res[0:m, 2, :].rearrange("p (h k) -> p h k", k=16)
                    tt("dve", resg, cv[0:m], cv[0:m, :, 0:1].to_broadcast([m, 8, 16]), ALU.subtract, cv.u, res.u)
                    act(resg, resg, AF.Exp, res.u, res.u)
                    vredsum(gs[0:m, :], res[0:m, 2, :].rearrange("p (h k) -> p h k", k=16), res.u, gs.u)
                    vrecip(gs[0:m, :], gs[0:m, :], gs.u, gs.u)
                    tt("dve", resg, resg, gs[0:m, :].unsqueeze(2).to_broadcast([m, 8, 16]), ALU.mult, res.u + gs.u, res.u)
                    tb = banks[6]
                    for q3 in range(3):
                        tr(tb.t[:, q3 * 128:q3 * 128 + m], res[0:m, q3, :], ident_f[0:m, 0:m], res.u + ident_f.u, tb.u)
                    ro = rts[bcnt % 2]
                    cp("act", ro[:, :, 0:m], tb.t[:, 0:384].rearrange("p (q t) -> p q t", t=128)[:, :, 0:m], tb.u, ro.u)
                    dma("sp", "rts%d" % (bcnt % 2), RT[:, :, r0:r0 + m].rearrange("q p t -> p q t"), ro[:, :, 0:m], ro.u, ())
                    bcnt += 1
            pg.barrier()

    def p4(l):
        TG = 256
        with ExitStack() as ph:
            Wall = sb("Wall", [128, TG, 128], BF16, st=ph)
            ohj = [sb("ohj%d" % i, [128, 32, 128], BF16, st=ph) for i in range(2)]
            ohi = [sb("ohi%d" % i, [128, 32, 128], BF16, st=ph) for i in range(2)]
            ust = [sb("ust%d" % i, [128, 8, 512], BF16, st=ph) for i in range(2)]
            vst = [sb("vst%d" % i, [128, 4, D], BF16, st=ph) for i in range(2)]
            h2g = [sb("h2g%d" % i, [128, 8, TG], BF16, st=ph) for i in range(2)]
            rtg = [sb("rtg%d" % i, [128, 3, TG], st=ph) for i in range(2)]
            gl = [sb("gl%d" % i, [128, TG], BF16, st=ph) for i in range(3)]
            pTt = [sb("pTt%d" % i, [128, TG], BF16, st=ph) for i in range(3)]
            pgroups = [(c, TG) for c in range(0, SEQ, TG)]
            if NBLK:
                pgroups = pgroups[:NBLK]
            groups = pgroups + [(SEQ, NS)]
            pc = 0
            wc = 0
            sc_ = 0
            for gi_, (c0, n) in enumerate(groups):
                h2 = h2g[gi_ % 2]
                rt = rtg[gi_ % 2]
                dma("sp", "h2g%d" % (gi_ % 2), h2[:, :, 0:n], H2T[:, :, c0:c0 + n].rearrange("k p t -> p k t"), (), h2.u)
                dma("sp", "rtg%d" % (gi_ % 2), rt[:, :, 0:n], RT[:, :, c0:c0 + n].rearrange("q p t -> p q t"), (), rt.u)
                x = load_xb(c0, n)
                for t0 in range(0, n, 32):
                    oj = ohj[pc % 2]
                    oi = ohi[pc % 2]
                    pc += 1
                    for t in range(32):
                        tk = t0 + t
                        ts("dve", oj[:, t, :], iota_b[:], rt[:, 1, tk:tk + 1], ALU.is_equal, iota_b.u + rt.u, oj.u)
                        ts("dve", oi[:, t, :], iota_b[:], rt[:, 0, tk:tk + 1], ALU.is_equal, iota_b.u + rt.u, oi.u,
                           s2=rt[:, 2, tk:tk + 1], op1=ALU.mult)
                    for t4 in range(0, 32, 4):
                        wb = banks[6 + wc % 2]
                        wc += 1
                        for t in range(4):
                            mm(wb.t[:, t * 128:(t + 1) * 128], oj[:, t4 + t, :], oi[:, t4 + t, :], True, True,
                               oj.u + oi.u, wb.u)
                        cp("act", Wall[:, t0 + t4:t0 + t4 + 4, :],
                           wb.t[:, :].rearrange("p (t i) -> p t i", i=128), wb.u, Wall.u)
                for eb in range(32):
                    us = ust[sc_ % 2]
                    vs = vst[sc_ % 2]
                    dma("sp", "ust%d" % (sc_ % 2), us[:], UT[:, :, eb * 512:(eb + 1) * 512].rearrange("k p e -> p k e"),
                        (), us.u)
                    dma("sp", "vst%d" % (sc_ % 2), vs[:], VBF[eb * 512:(eb + 1) * 512, :].rearrange("(c p) d -> p c d", p=128),
                        (), vs.u)
                    sc_ += 1
                    for c4 in range(4):
                        ec = eb * 4 + c4
                        ab = banks[4 + ec % 2]
                        for k in range(8):
                            mm(ab.t[:, 0:n], us[:, k, c4 * 128:(c4 + 1) * 128], h2[:, k, 0:n], k == 0, k == 7,
                               us.u + h2.u, ab.u)
                        g_ = gl[ec % 3]
                        p_ = pTt[ec % 3]
                        act(g_[:, 0:n], ab.t[:, 0:n], AF.Gelu_apprx_tanh, ab.u, g_.u)
                        tt("pool", p_[:, 0:n], g_[:, 0:n], Wall[:, 0:n, ec], ALU.mult, g_.u + Wall.u, p_.u)
                        for dc in range(8):
                            bk = banks[dc // 2]
                            mmg(bk.t[:, (dc % 2) * 256:(dc % 2) * 256 + n], vs[:, c4, dc * 128:(dc + 1) * 128],
                                p_[:, 0:n], (ec == 0 and dc % 2 == 0), ec == 127, vs.u + p_.u, bk.u)
                for dc in range(8):
                    bk = banks[dc // 2]
                    for (lo, hi, r) in segs(c0, n):
                        o_ = (dc % 2) * 256
                        stt(x[:, dc, lo:hi], bk.t[:, o_ + lo:o_ + hi], modT[l][:, 40 + dc, r:r + 1], x[:, dc, lo:hi],
                            ALU.mult, ALU.add, bk.u + modT[l].u + x.u, x.u)
                store_xb(x, c0, n)
            pg.barrier()

    def pfinal():
        with ExitStack() as ph:
            gfin = sb("gfin", [128, D], st=ph)
            dma("sp", "c0", gfin[:], I.g_final[0:1, :].partition_broadcast(128), (), gfin.u)
            xo = [sb("xo%d" % i, [128, D], st=ph) for i in range(2)]
            yo = [sb("yo%d" % i, [128, D], st=ph) for i in range(2)]
            xs_ = [sb("xs%d" % i, [128, 8, 128], st=ph) for i in range(2)]
            junk = sb("fjunk", [128, D], st=ph)
            ss = sb("fss", [128, 1], st=ph)
            tiles = list(range((T + 127) // 128))
            if NBLK:
                tiles = tiles[:4 * NBLK] + tiles[-5:]
            for ti in tiles:
                r0 = ti * 128
                m = min(128, T - r0)
                xs = xs_[ti % 2]
                dma("sp", "xs%d" % (ti % 2), xs[:, :, 0:m], xT[:, :, r0:r0 + m].rearrange("k p t -> p k t"), (), xs.u)
                o = xo[ti % 2]
                for hf in range(2):
                    b = banks[(ti * 2 + hf) % 4]
                    for kk in range(4):
                        k = hf * 4 + kk
                        tr(b.t[0:m, kk * 128:(kk + 1) * 128], xs[:, k, 0:m], ident_f[:], xs.u + ident_f.u, b.u)
                    cp("act" if hf == 0 else "dve", o[0:m, hf * 512:(hf + 1) * 512], b.t[0:m, :], b.u, o.u)
                act(junk[0:m, :], o[0:m, :], AF.Square, o.u, junk.u + ss.u, accum=ss[0:m, 0:1])
                act(ss[0:m, :], ss[0:m, :], AF.Sqrt, ss.u + epsc.u, ss.u, bias=epsc[0:m, 0:1], scale=1.0 / D)
                V("dve", lambda e, m=m: e.reciprocal(ss[0:m, :], ss[0:m, :]), ss.u, ss.u)
                y = yo[ti % 2]
                stt(y[0:m, :], o[0:m, :], ss[0:m, 0:1], gfin[0:m, :], ALU.mult, ALU.mult, o.u + ss.u + gfin.u, y.u)
                dma("sp", "yo%d" % (ti % 2), y_out[r0:r0 + m, :], y[0:m, :], y.u, ())

    for l in range(DEPTH):
        p1(l)
        if stage("p1_%d" % l):
            return finish()
        p2(l)
        if stage("p2_%d" % l):
            return finish()
        pu(l)
        p3(l)
        if stage("p3_%d" % l):
            return finish()
        p4(l)
        if stage("p4_%d" % l):
            return finish()
    pfinal()
    return finish()


_CACHE = {}


def _rope_tables():
    half = 8
    inv = (np.float32(500000.0) ** (-(np.arange(0, 16, 2, dtype=np.float32)) / np.float32(16))).astype(np.float32)
    pos = np.concatenate([np.arange(SEQ), np.tile(PAST + np.arange(16), 4)]).astype(np.float32)
    ang = (pos[:, None] * inv[None, :]).astype(np.float32)
    return np.cos(ang).astype(np.float32), np.sin(ang).astype(np.float32)


def make_in_maps(inp):
    f = lambda a: np.ascontiguousarray(np.asarray(a, dtype=np.float32))
    rel = f(inp["rel_bias"])
    q = np.arange(128)[None, :]
    k = np.arange(128)[:, None]
    d3 = np.clip(128 + q - k, -128, 128) + 128
    d4 = np.clip(q - k, -128, 128) + 128
    biasT = np.stack([rel[:, :, d3], rel[:, :, d4]], axis=2)
    bconst = np.ascontiguousarray(np.broadcast_to(rel[:, :, 256][:, :, None], (DEPTH, 8, 128)))
    rc, rs = _rope_tables()
    shared = {
        "w_ada": f(inp["w_ada"]),
        "b_adaT": f(np.asarray(inp["b_ada"]).reshape(DEPTH, 48, 128).transpose(0, 2, 1)),
        "g_attnT": f(np.asarray(inp["g_attn"]).reshape(DEPTH, 8, 128).transpose(0, 2, 1)),
        "g_ffnT": f(np.asarray(inp["g_ffn"]).reshape(DEPTH, 8, 128).transpose(0, 2, 1)),
        "w_in": f(inp["w_in"]),
        "lamv": f(np.concatenate([np.asarray(inp["lam_q1"]), np.asarray(inp["lam_k1"]),
                                  np.asarray(inp["lam_q2"]), np.asarray(inp["lam_k2"])], axis=1)),
        "a_gain": f(inp["a_gain"]),
        "biasT": f(biasT),
        "bconst": f(bconst),
        "b_gain": f(inp["b_gain"]),
        "w_out": f(inp["w_out"]),
        "pk_wq": f(inp["pk_wq"]),
        "pk_keys": f(np.asarray(inp["pk_keys"]).reshape(DEPTH, 16, 128, 128)),
        "pk_u": f(inp["pk_u"]),
        "pk_v": f(inp["pk_v"]),
        "g_final": f(np.asarray(inp["g_final"]).reshape(1, D)),
        "ident": np.eye(128, dtype=np.float32),
        "ropec": rc,
        "ropes": rs,
        "iota_in": np.ascontiguousarray(np.broadcast_to(np.arange(128, dtype=np.float32)[None, :], (128, 128))),
    }
    xp = np.asarray(inp["x_prompt"])
    xs = np.asarray(inp["x_sample"])
    cp_ = np.asarray(inp["c_prompt"])
    cs = np.asarray(inp["c_sample"])
    maps = []
    for c in range(8):
        b = c % 4
        sl = slice(4 * c, 4 * c + 4)
        c5 = np.concatenate([cp_[b:b + 1], cs[sl]], axis=0)
        m = dict(shared)
        m["xin"] = f(np.concatenate([xp[b], xs[sl].reshape(NS, D)], axis=0))
        m["cT"] = f(c5.reshape(5, 8, 128).transpose(2, 1, 0))
        m["cak"] = f(np.asarray(inp["cache_a_k"])[:, sl].reshape(DEPTH, 4, PAST, 512))
        m["cav"] = f(np.asarray(inp["cache_a_v"])[:, sl].reshape(DEPTH, 4, PAST, 512))
        m["cbk"] = f(np.asarray(inp["cache_b_k"])[:, sl].reshape(DEPTH, 4, 512, 512))
        m["cbv"] = f(np.asarray(inp["cache_b_v"])[:, sl].reshape(DEPTH, 4, 512, 512))
        maps.append(m)
    return maps


def assemble(results):
    y_p = np.stack([results[b]["y"][:SEQ] for b in range(4)])
    y_s = np.concatenate([results[c]["y"][SEQ:].reshape(4, 16, D) for c in range(8)])
    nakp = np.stack([results[b]["nak"][:, :SEQ] for b in range(4)], axis=1).reshape(DEPTH, 4, SEQ, 4, 128)
    navp = np.stack([results[b]["nav"][:, :SEQ] for b in range(4)], axis=1).reshape(DEPTH, 4, SEQ, 4, 128)
    nbkp = np.stack([results[b]["nbk_p"] for b in range(4)], axis=1).reshape(DEPTH, 4, 512, 8, 64)
    nbvp = np.stack([results[b]["nbv_p"] for b in range(4)], axis=1).reshape(DEPTH, 4, 512, 8, 64)
    naks = np.concatenate([results[c]["nak"][:, SEQ:].reshape(DEPTH, 4, 16, 4, 128) for c in range(8)], axis=1)
    navs = np.concatenate([results[c]["nav"][:, SEQ:].reshape(DEPTH, 4, 16, 4, 128) for c in range(8)], axis=1)
    nbks = np.concatenate([results[c]["nbk_s"].reshape(DEPTH, 4, 512, 8, 64) for c in range(8)], axis=1)
    nbvs = np.concatenate([results[c]["nbv_s"].reshape(DEPTH, 4, 512, 8, 64) for c in range(8)], axis=1)
    return tuple(np.ascontiguousarray(a, dtype=np.float32) for a in
                 (y_p, y_s, nakp, navp, nbkp, nbvp, naks, navs, nbks, nbvs))


def kernel(**inputs):
    nc, es = build()
    maps = make_in_maps(inputs)
    used = set(build.used_inputs)
    maps = [{k: v for k, v in m.items() if k in used} for m in maps]
    res = run_bass_kernel_spmd(nc, maps, core_ids=list(range(8)))
    return assemble(res.results)
```

```python
import os
import math
import numpy as np
from contextlib import ExitStack
import concourse.bass as bass
import concourse.mybir as mybir
from concourse.bass_utils import run_bass_kernel_spmd

F32 = mybir.dt.float32
BF16 = mybir.dt.bfloat16
U32 = mybir.dt.uint32
AF = mybir.ActivationFunctionType
ALU = mybir.AluOpType
AX = mybir.AxisListType

D = 1024
SEQ = 8192
NS = 64
T = SEQ + NS
DEPTH = 2
PAST = 4096
EPS = 1e-6
NEXP = 16384
NEG = -30000.0


class Unit:
    __slots__ = ("name", "w", "r", "excl")

    def __init__(self, name):
        self.name = name
        self.w = None
        self.r = {}
        self.excl = False


class Op:
    __slots__ = ("eng", "fn", "deps", "inc", "cnt", "dma")


ENGS = ("pe", "act", "dve", "pool", "sp")


class Prog:
    def __init__(self, nc, es):
        self.nc = nc
        self.es = es
        self.ops = {e: [] for e in ENGS}
        self.eng_sem = {e: es.enter_context(nc.semaphore("s_" + e)) for e in ENGS}
        self.dma_sem = {}
        self.dma_cnt = {}
        self.nunits = 0

    def unit(self, name=None):
        self.nunits += 1
        return Unit(name or "u%d" % self.nunits)

    def _dep(self, op, d, kind):
        if d is op:
            return
        if d.dma is None and op.dma is None and d.eng == op.eng and kind != "raw":
            return
        op.deps.append(d)
        if d.dma is None:
            d.inc = True

    def add(self, eng, fn, reads=(), writes=(), dma=None):
        op = Op()
        op.eng = eng
        op.fn = fn
        op.deps = []
        op.inc = False
        op.cnt = 0
        op.dma = dma
        ex = [u for u in reads if u.excl]
        if ex:
            writes = list(writes) + [u for u in ex if u not in writes]
        for u in reads:
            if u.w is not None:
                self._dep(op, u.w, "raw")
        for u in writes:
            if u.w is not None:
                self._dep(op, u.w, "waw")
            for r in u.r.values():
                self._dep(op, r, "war")
        key = dma if dma is not None else eng
        for u in reads:
            u.r[key] = op
        for u in writes:
            u.w = op
            u.r = {}
        if dma is not None:
            if dma not in self.dma_sem:
                self.dma_sem[dma] = self.es.enter_context(self.nc.semaphore("d_" + dma))
                self.dma_cnt[dma] = 0
            self.dma_cnt[dma] += 16
            op.cnt = self.dma_cnt[dma]
        self.ops[eng].append(op)
        return op

    def _sem(self, d):
        return self.dma_sem[d.dma] if d.dma is not None else self.eng_sem[d.eng]

    def barrier(self):
        if os.environ.get("MK_NOBAR"):
            return
        deps = []
        for e in ENGS:
            for op in reversed(self.ops[e]):
                if op.dma is None and op.fn is not None:
                    deps.append(op)
                    break
        lastd = {}
        for e in ENGS:
            for op in self.ops[e]:
                if op.dma is not None:
                    lastd[op.dma] = op
        deps += list(lastd.values())
        for e in ENGS:
            op = Op()
            op.eng = e
            op.fn = None
            op.deps = []
            op.inc = False
            op.cnt = 0
            op.dma = None
            for d in deps:
                if d.dma is None and d.eng == e:
                    continue
                op.deps.append(d)
                if d.dma is None:
                    d.inc = True
            self.ops[e].append(op)

    def emit(self):
        nc = self.nc
        for e in ENGS:
            c = 0
            for op in self.ops[e]:
                if op.dma is None and op.inc:
                    c += 1
                    op.cnt = c
        final = [(self.dma_sem[k], v) for k, v in self.dma_cnt.items()]

        def run(ename, eng):
            waited = {}
            for op in self.ops[ename]:
                need = {}
                for d in op.deps:
                    s = self._sem(d)
                    k = id(s)
                    if k not in need or need[k][1] < d.cnt:
                        need[k] = (s, d.cnt)
                for k, (s, c) in need.items():
                    if waited.get(k, 0) < c:
                        eng.wait_ge(s, c)
                        waited[k] = c
                if op.fn is None:
                    continue
                inst = op.fn(eng)
                if op.dma is not None:
                    inst.then_inc(self.dma_sem[op.dma], 16)
                elif op.inc:
                    inst.then_inc(self.eng_sem[ename], 1)
            if ename == "sp":
                for s, c in final:
                    eng.wait_ge(s, c)

        with nc.Block() as block:
            @block.tensor
            def _(e):
                run("pe", e)

            @block.scalar
            def _(e):
                run("act", e)

            @block.vector
            def _(e):
                run("dve", e)

            @block.gpsimd
            def _(e):
                run("pool", e)

            @block.sync
            def _(e):
                run("sp", e)


class Tl:
    def __init__(self, pg, t, nunits=1):
        self.t = t
        self.u = [pg.unit() for _ in range(nunits)]

    def __getitem__(self, k):
        return self.t[k]


def build(stop=None):
    nc = bass.Bass("TRN2", target_bir_lowering=False)
    es = ExitStack()
    pg = Prog(nc, es)
    stop = stop or os.environ.get("MK_STOP", "")

    def din(name, shape, dt=F32):
        return nc.dram_tensor(name, list(shape), dt, kind="ExternalInput").ap()

    def dout(name, shape, dt=F32):
        return nc.dram_tensor(name, list(shape), dt, kind="ExternalOutput").ap()

    def dscr(name, shape, dt):
        return nc.dram_tensor(name, list(shape), dt).ap()

    IN_SHAPES = {
        "xin": [T, D], "cT": [128, 8, 5], "cak": [DEPTH, 4, PAST, 512], "cav": [DEPTH, 4, PAST, 512],
        "cbk": [DEPTH, 4, 512, 512], "cbv": [DEPTH, 4, 512, 512], "w_ada": [DEPTH, D, 6 * D],
        "b_adaT": [DEPTH, 128, 48], "g_attnT": [DEPTH, 128, 8], "g_ffnT": [DEPTH, 128, 8],
        "w_in": [DEPTH, D, 3072], "lamv": [DEPTH, 256], "a_gain": [DEPTH, 128],
        "biasT": [DEPTH, 8, 2, 128, 128], "bconst": [DEPTH, 8, 128], "b_gain": [DEPTH, 512],
        "w_out": [DEPTH, D, D], "pk_wq": [DEPTH, D, 2048], "pk_keys": [DEPTH, 16, 128, 128],
        "pk_u": [DEPTH, NEXP, D], "pk_v": [DEPTH, NEXP, D], "g_final": [1, D], "ident": [128, 128],
        "ropec": [T, 8], "ropes": [T, 8], "iota_in": [128, 128],
    }
    _ins = {}

    class _IN:
        def __getattr__(self, name):
            if name not in _ins:
                _ins[name] = din(name, IN_SHAPES[name])
            return _ins[name]

    I = _IN()
    build.used_inputs = _ins

    y_out = dout("y", [T, D])
    nak = dout("nak", [DEPTH, T, 512])
    nav = dout("nav", [DEPTH, T, 512])
    nbk_p = dout("nbk_p", [DEPTH, 512, 512])
    nbv_p = dout("nbv_p", [DEPTH, 512, 512])
    nbk_s = dout("nbk_s", [DEPTH, 4, 512, 512])
    nbv_s = dout("nbv_s", [DEPTH, 4, 512, 512])

    xT = dscr("xT", [8, 128, T], F32)
    QTA = dscr("QTA", [4, 128, T], BF16)
    KTA = dscr("KTA", [4, 128, T], BF16)
    VA = dscr("VA", [4, T, 136], BF16)
    QTB = dscr("QTB", [4, 128, T], BF16)
    KTB = dscr("KTB", [4, 128, T], BF16)
    VB = dscr("VB", [T, 8 * 72], BF16)
    UT = dscr("UT", [8, 128, NEXP], BF16)
    VBF = dscr("VBF", [NEXP, D], BF16)
    H2T = dscr("H2T", [8, 128, T], BF16)
    RT = dscr("RT", [3, 128, T], F32)
    u_xT = pg.unit("xT")
    u_qk = pg.unit("qkscr")
    u_uv = pg.unit("uvscr")
    u_h2 = pg.unit("h2scr")
    u_rt = pg.unit("rtscr")
    u_out = pg.unit("outs")

    sbn = [0]

    def sb(name, shape, dt=F32, nunits=1, st=None):
        sbn[0] += 1
        return Tl(pg, (st or es).enter_context(nc.sbuf_tensor("%s_%d" % (name, sbn[0]), list(shape), dt)), nunits)

    banks = [Tl(pg, es.enter_context(nc.psum_tensor("bank%d" % i, [128, 512], F32))) for i in range(8)]

    for b_ in banks:
        b_.u[0].excl = True

    def bk_bf(i):
        return banks[i].t[:].bitcast(BF16)

    PAR_KEYS = {"wbig", "wq", "wout", "biasb"}
    keyunit = {}

    def dma(eng, key, out, in_, reads, writes):
        if key not in PAR_KEYS:
            if key not in keyunit:
                keyunit[key] = pg.unit("k_" + key)
            writes = list(writes) + [keyunit[key]]
        pg.add(eng, lambda e: e.dma_start(out=out, in_=in_), reads, writes, dma=key)

    def mm(out, lhsT, rhs, start, stop, reads, writes):
        pg.add("pe", lambda e: e.matmul(out, lhsT, rhs, start=start, stop=stop), reads, writes)

    def tr(out, in_, idn, reads, writes):
        pg.add("pe", lambda e: e.transpose(out, in_, idn), reads, writes)

    def act(out, in_, func, reads, writes, bias=None, scale=None, accum=None):
        def f(e):
            kw = {}
            if bias is not None:
                kw["bias"] = bias
            if scale is not None:
                kw["scale"] = scale
            if accum is not None:
                kw["accum_out"] = accum
            return e.activation(out=out, in_=in_, func=func, **kw)
        pg.add("act", f, reads, writes)

    def V(eng, fn, reads, writes):
        pg.add(eng, fn, reads, writes)

    def tt(eng, out, a, b, op, reads, writes):
        pg.add(eng, lambda e: e.tensor_tensor(out, a, b, op), reads, writes)

    def ts(eng, out, a, s1, op0, reads, writes, s2=None, op1=None):
        if op1 is None:
            pg.add(eng, lambda e: e.tensor_scalar(out, a, s1, None, op0), reads, writes)
        else:
            pg.add(eng, lambda e: e.tensor_scalar(out, a, s1, s2, op0, op1), reads, writes)

    def stt(out, a, s, b, op0, op1, reads, writes):
        pg.add("dve", lambda e: e.scalar_tensor_tensor(out, a, s, b, op0, op1), reads, writes)

    def vmax(out, in_, reads, writes):
        pg.add("dve", lambda e: e.max(out=out, in_=in_), reads, writes)

    def vmaxidx(out, in_max, in_values, reads, writes):
        pg.add("dve", lambda e: e.max_index(out=out, in_max=in_max, in_values=in_values), reads, writes)

    def vmatch(out, rep, vals, reads, writes):
        pg.add("dve", lambda e: e.match_replace(out=out, in_to_replace=rep, in_values=vals, imm_value=-1e30),
               reads, writes)

    def vredsum(out, in_, reads, writes):
        pg.add("dve", lambda e: e.reduce_sum(out, in_, AX.X), reads, writes)

    def vrecip(out, in_, reads, writes):
        pg.add("dve", lambda e: e.reciprocal(out, in_), reads, writes)

    def cp(eng, out, in_, reads, writes):
        if eng == "act":
            pg.add("act", lambda e: e.copy(out, in_), reads, writes)
        else:
            pg.add(eng, lambda e: e.tensor_copy(out, in_), reads, writes)

    def memset(eng, ap, val, writes):
        pg.add(eng, lambda e: e.memset(ap, val), (), writes)

    ident_f = sb("ident_f", [128, 128])
    ident_b = sb("ident_b", [128, 128], BF16)
    ones_b = sb("ones_b", [128, 128], BF16)
    iota_b = sb("iota_b", [128, 128], BF16)
    iota16 = sb("iota16", [128, 16])
    mk = sb("mk", [128, 5, 128], BF16)
    epsc = sb("epsc", [128, 1])
    dma("sp", "c0", ident_f[:], I.ident[:, :], (), ident_f.u)
    dma("pool", "c1", ident_b[:], I.ident[:, :], (), ident_b.u)
    dma("pool", "c1", iota_b[:], I.iota_in[:, :], (), iota_b.u)
    dma("sp", "c0", iota16[:], I.iota_in[:, 0:16], (), iota16.u)
    memset("dve", ones_b[:], 1.0, ones_b.u)
    memset("dve", epsc[:], EPS, epsc.u)
    memset("dve", mk[:], 0.0, mk.u)
    memset("dve", mk[0:1, 0, 0:64], 0.0, mk.u)
    memset("dve", mk[0:1, 0, 64:128], 1.0, mk.u)
    memset("dve", mk[0:1, 1, 0:64], NEG, mk.u)
    memset("dve", mk[0:1, 1, 64:128], 0.0, mk.u)
    memset("dve", mk[0:1, 2, 0:64], 1.0, mk.u)
    memset("dve", mk[0:1, 2, 64:128], 0.0, mk.u)
    memset("dve", mk[0:1, 3, 0:64], 0.0, mk.u)
    memset("dve", mk[0:1, 3, 64:128], NEG, mk.u)
    memset("dve", mk[0:1, 4, :], 1.0, mk.u)

    NBLK = int(os.environ.get("MK_NB", "0"))
    DBG = bool(os.environ.get("MK_DBG"))
    dbg_xT = dout("dbg_xT", [8, 128, T]) if DBG else None
    dbg_RT = dout("dbg_RT", [3, 128, T]) if DBG else None

    def stage(name):
        return stop == name

    def finish():
        if DBG:
            pg.barrier()
            dma("sp", "dbg", dbg_xT[:, :, :], xT[:, :, :], (), ())
            dma("sp", "dbg", dbg_RT[:, :, :], RT[:, :, :], (), ())
        pg.emit()
        return nc, es

    modT = [sb("modT%d" % l, [128, 48, 5]) for l in range(DEPTH)]
    A1 = [sb("A1_%d" % l, [128, 8, 5]) for l in range(DEPTH)]
    A2 = [sb("A2_%d" % l, [128, 8, 5]) for l in range(DEPTH)]
    lam = sb("lam", [128, DEPTH, 4])
    gainA = sb("gainA", [128, DEPTH, 128])
    gainB = sb("gainB", [128, DEPTH, 512])
    with ExitStack() as ph:
        scT = sb("scT", [128, 8, 5], st=ph)
        dma("sp", "c0", scT[:], I.cT[:, :, :], (), scT.u)
        act(scT[:], scT[:], AF.Silu, scT.u, scT.u)
        badaT = sb("badaT", [128, DEPTH, 48], st=ph)
        gaT = sb("gaT", [128, DEPTH, 8], st=ph)
        gfT = sb("gfT", [128, DEPTH, 8], st=ph)
        for l in range(DEPTH):
            dma("sp", "c0", badaT[:, l, :], I.b_adaT[l], (), badaT.u)
            dma("sp", "c0", gaT[:, l, :], I.g_attnT[l], (), gaT.u)
            dma("sp", "c0", gfT[:, l, :], I.g_ffnT[l], (), gfT.u)
        wada = [sb("wada%d" % i, [128, 8, 512], st=ph) for i in range(2)]
        gi = 0
        for l in range(DEPTH):
            for g in range(12):
                wt = wada[gi % 2]
                gi += 1
                dma("sp", "wada%d" % (gi % 2), wt[:],
                    I.w_ada[l][:, g * 512:(g + 1) * 512].rearrange("(k p) f -> p k f", p=128), (), wt.u)
                for j in range(4):
                    ch = g * 4 + j
                    for k in range(8):
                        mm(banks[7].t[:, ch * 5:ch * 5 + 5], wt[:, k, j * 128:(j + 1) * 128], scT[:, k, :],
                           k == 0, k == 7, wt.u + scT.u, banks[7].u)
            tt("dve", modT[l][:], banks[7].t[:, 0:240].rearrange("p (c r) -> p c r", r=5),
               badaT[:, l, :].unsqueeze(2).to_broadcast([128, 48, 5]), ALU.add, banks[7].u + badaT.u, modT[l].u)
            stt(A1[l][:], modT[l][:, 8:16, :], 1.0, gaT[:, l, :].unsqueeze(2).to_broadcast([128, 8, 5]), ALU.add,
                ALU.mult, modT[l].u + gaT.u, A1[l].u)
            stt(A2[l][:], modT[l][:, 32:40, :], 1.0, gfT[:, l, :].unsqueeze(2).to_broadcast([128, 8, 5]), ALU.add,
                ALU.mult, modT[l].u + gfT.u, A2[l].u)
        lamsrc = sb("lamsrc", [128, DEPTH, 256], st=ph)
        lamt = sb("lamt", [128, 64], st=ph)
        for l in range(DEPTH):
            dma("sp", "c0", lamsrc[:, l, :], I.lamv[l:l + 1, :].partition_broadcast(128), (), lamsrc.u)
            dma("sp", "c0", gainA[:, l, :], I.a_gain[l:l + 1, :].partition_broadcast(128), (), gainA.u)
            dma("sp", "c0", gainB[:, l, :], I.b_gain[l:l + 1, :].partition_broadcast(128), (), gainB.u)
        for l in range(DEPTH):
            lam_init = 0.8 - 0.6 * math.exp(-0.3 * l)
            for i in range(2):
                tt("dve", lamt[:], lamsrc[:, l, i * 128:i * 128 + 64], lamsrc[:, l, i * 128 + 64:i * 128 + 128],
                   ALU.mult, lamsrc.u, lamt.u)
                V("dve", lambda e, l=l, i=i: e.reduce_sum(lam[:, l, i:i + 1], lamt[:], AX.X), lamt.u, lam.u)
            act(lam[:, l, 0:2], lam[:, l, 0:2], AF.Exp, lam.u, lam.u)
            tt("dve", lam[:, l, 2:3], lam[:, l, 1:2], lam[:, l, 0:1], ALU.subtract, lam.u, lam.u)
            ts("dve", lam[:, l, 2:3], lam[:, l, 2:3], -lam_init, ALU.add, lam.u, lam.u)
            ts("dve", gainA[:, l, :], gainA[:, l, :], 1.0 - lam_init, ALU.mult, gainA.u, gainA.u)
        pg.barrier()

    def segs(c0, n):
        out = []
        if c0 < SEQ:
            out.append((0, min(n, SEQ - c0), 0))
        for s in range(4):
            lo, hi = SEQ + 16 * s, SEQ + 16 * s + 16
            a, b = max(lo, c0), min(hi, c0 + n)
            if a < b:
                out.append((a - c0, b - c0, 1 + s))
        return out

    with ExitStack() as ph:
        xld = [sb("xld%d" % i, [128, D], st=ph) for i in range(2)]
        xtt = [sb("xtt%d" % i, [128, 8, 128], st=ph) for i in range(2)]
        ntile = (T + 127) // 128
        for ti in range(ntile):
            r0 = ti * 128
            m = min(128, T - r0)
            xl = xld[ti % 2]
            xo = xtt[ti % 2]
            dma("sp", "xld%d" % (ti % 2), xl[0:m, :], I.xin[r0:r0 + m, :], (), xl.u)
            for hf in range(2):
                b = banks[(ti * 2 + hf) % 4]
                for kk in range(4):
                    k = hf * 4 + kk
                    tr(b.t[:, kk * 128:kk * 128 + m], xl[0:m, k * 128:(k + 1) * 128], ident_f[0:m, 0:m],
                       xl.u + ident_f.u, b.u)
                cp("act" if hf == 0 else "dve", xo[:, hf * 4:(hf + 1) * 4, 0:m],
                   b.t[:, :].rearrange("p (k t) -> p k t", t=128)[:, :, 0:m], b.u, xo.u)
            dma("sp", "xtt%d" % (ti % 2), xT[:, :, r0:r0 + m].rearrange("k p t -> p k t"), xo[:, :, 0:m], xo.u, ())
        pg.barrier()
    if stage("p0"):
        return finish()

    xb = [sb("xb%d" % i, [128, 8, 512]) for i in range(2)]
    xbi = [0]

    def load_xb(c0, n):
        x = xb[xbi[0] % 2]
        xbi[0] += 1
        x.key = "xb%d" % (xbi[0] % 2)
        dma("sp", x.key, x[:, :, 0:n], xT[:, :, c0:c0 + n].rearrange("k p t -> p k t"), (), x.u)
        return x

    def store_xb(x, c0, n):
        dma("sp", x.key, xT[:, :, c0:c0 + n].rearrange("k p t -> p k t"), x[:, :, 0:n], x.u, ())

    class NB:
        def __init__(self, ph, name):
            self.sqb = sb(name + "sqb", [128, 8, 512], BF16, st=ph)
            self.rstd = sb(name + "rstd", [128, 512], st=ph)
            self.xn = sb(name + "xn", [128, 8, 512], st=ph)
            self.h = sb(name + "h", [128, 8, 512], BF16, st=ph)

    def norm_mod(nb, x, c0, n, Aq, Bq, boff, bank):
        sqb, rstd, xn, dst = nb.sqb, nb.rstd, nb.xn, nb.h
        act(sqb[:, :, 0:n], x[:, :, 0:n], AF.Square, x.u, sqb.u)
        for k in range(8):
            mm(bank.t[:, 0:n], ones_b[:], sqb[:, k, 0:n], k == 0, k == 7, ones_b.u + sqb.u, bank.u)
        act(rstd[:, 0:n], bank.t[:, 0:n], AF.Sqrt, bank.u + epsc.u, rstd.u, bias=epsc[:, 0:1], scale=1.0 / D)
        V("dve", lambda e: e.reciprocal(rstd[:, 0:n], rstd[:, 0:n]), rstd.u, rstd.u)
        tt("dve", xn[:, :, 0:n], x[:, :, 0:n], rstd[:, 0:n].unsqueeze(1).to_broadcast([128, 8, n]), ALU.mult,
           x.u + rstd.u, xn.u)
        for (lo, hi, r) in segs(c0, n):
            for k in range(8):
                act(dst[:, k, lo:hi], xn[:, k, lo:hi], AF.Identity, xn.u + Aq.u + Bq.u, dst.u,
                    bias=Bq[:, boff + k, r:r + 1], scale=Aq[:, k, r:r + 1])

    pblocks = [(c, 512) for c in range(0, SEQ, 512)]
    if NBLK:
        pblocks = pblocks[:NBLK] + pblocks[-1:]
    blocks = pblocks + [(SEQ, NS)]

    def p1(l):
        with ExitStack() as ph:
            nb = NB(ph, "p1")
            hT = nb.h
            wbig = sb("wbig", [128, 8, 3072], BF16, nunits=24, st=ph)
            qkf = [sb("qkf%d" % i, [128, 512], st=ph) for i in range(2)]
            qkb = [sb("qkb%d" % i, [128, 512], BF16, st=ph) for i in range(2)]
            vf = [sb("vf%d" % i, [128, 512], st=ph) for i in range(3)]
            vab = [sb("vab%d" % i, [128, 4, 136], BF16, st=ph) for i in range(2)]
            vbb = [sb("vbb%d" % i, [128, 8, 72], BF16, st=ph) for i in range(2)]
            for i in range(2):
                memset("pool", vab[i][:, :, 128:136], 1.0, vab[i].u)
                memset("pool", vbb[i][:, :, 64:72], 1.0, vbb[i].u)
            qkT = [sb("qkT%d" % i, [128, 4, 128], BF16, st=ph) for i in range(2)]
            qkbT = [sb("qkbT%d" % i, [128, 512], BF16, st=ph) for i in range(2)]
            rc = [sb("rc%d" % i, [128, 2, 8], st=ph) for i in range(2)]
            rtmp = sb("rtmp", [128, 4, 8, 8], st=ph)
            for k in range(8):
                for c3 in range(3):
                    dma("pool", "wbig", wbig[:, k, c3 * 1024:(c3 + 1) * 1024],
                        I.w_in[l][k * 128:(k + 1) * 128, c3 * 1024:(c3 + 1) * 1024], (), [wbig.u[k * 3 + c3]])
            cnt = 0
            for (c0, n) in blocks:
                x = load_xb(c0, n)
                norm_mod(nb, x, c0, n, A1[l], modT[l], 0, banks[7])
                outblk = (c0 == SEQ - 512) or (c0 == SEQ)
                for which, col0, scr, scl in ((0, 1536, QTB, 0.125), (1, 2048, KTB, 1.0)):
                    for cc in range(4):
                        b = banks[cnt % 2]
                        o = qkbT[cnt % 2]
                        cnt += 1
                        for k in range(8):
                            mm(b.t[:, 0:n], wbig[:, k, col0 + cc * 128:col0 + (cc + 1) * 128], hT[:, k, 0:n],
                               k == 0, k == 7, wbig.u + hT.u, b.u)
                        act(o[:, 0:n], b.t[:, 0:n], AF.Copy, b.u, o.u, scale=scl)
                        dma("sp", "qkbT%d" % ((cnt - 1) % 2), scr[cc, :, c0:c0 + n], o[:, 0:n], o.u, ())
                for st in range((n + 127) // 128):
                    m = min(128, n - st * 128)
                    r0 = c0 + st * 128
                    rcs = rc[st % 2]
                    dma("sp", "rc%d" % (st % 2), rcs[0:m, 0, :], I.ropec[r0:r0 + m, :], (), rcs.u)
                    dma("sp", "rc%d" % (st % 2), rcs[0:m, 1, :], I.ropes[r0:r0 + m, :], (), rcs.u)
                    for which, col0, scr, scl in ((0, 0, QTA, 0.125), (1, 512, KTA, 1.0)):
                        b = banks[2 + which]
                        f = qkf[which]
                        bb = qkb[which]
                        for k in range(8):
                            mm(b.t[0:m, :], hT[:, k, st * 128:st * 128 + m], wbig[:, k, col0:col0 + 512],
                               k == 0, k == 7, wbig.u + hT.u, b.u)
                        act(f[0:m, :], b.t[0:m, :], AF.Copy, b.u, f.u, scale=scl)
                        fv = f[0:m, :].rearrange("p (g e) -> p g e", e=64)
                        cosb = rcs[0:m, 0, :].unsqueeze(1).to_broadcast([m, 8, 8])
                        sinb = rcs[0:m, 1, :].unsqueeze(1).to_broadcast([m, 8, 8])
                        x1 = fv[:, :, 0:8]
                        x2 = fv[:, :, 8:16]
                        ru = rtmp.u + f.u + rcs.u
                        tt("pool", rtmp[0:m, 0], x1, cosb, ALU.mult, f.u + rcs.u, rtmp.u)
                        tt("pool", rtmp[0:m, 1], x2, sinb, ALU.mult, f.u + rcs.u, rtmp.u)
                        tt("pool", rtmp[0:m, 2], x2, cosb, ALU.mult, f.u + rcs.u, rtmp.u)
                        tt("pool", rtmp[0:m, 3], x1, sinb, ALU.mult, f.u + rcs.u, rtmp.u)
                        tt("pool", x1, rtmp[0:m, 0], rtmp[0:m, 1], ALU.subtract, ru, f.u)
                        tt("pool", x2, rtmp[0:m, 2], rtmp[0:m, 3], ALU.add, ru, f.u)
                        if which == 1:
                            dma("sp", "qkf1", nak[l, r0:r0 + m, :], f[0:m, :], f.u, ())
                        cp("dve", bb[0:m, :], f[0:m, :], f.u, bb.u)
                        tb = banks[4 + which]
                        o = qkT[which]
                        for h in range(4):
                            tr(tb.t[:, :].bitcast(BF16)[:, h * 128:h * 128 + m], bb[0:m, h * 128:(h + 1) * 128],
                               ident_b[0:m, 0:m], bb.u + ident_b.u, tb.u)
                        cp("dve", o[:, :, 0:m],
                           tb.t[:, :].bitcast(BF16)[:, 0:512].rearrange("p (h t) -> p h t", t=128)[:, :, 0:m], tb.u, o.u)
                        dma("sp", "qkT%d" % which, scr[:, :, r0:r0 + m].rearrange("h p t -> p h t"), o[:, :, 0:m], o.u, ())
                    b = banks[6]
                    for k in range(8):
                        mm(b.t[0:m, :], hT[:, k, st * 128:st * 128 + m], wbig[:, k, 1024:1536], k == 0, k == 7,
                           wbig.u + hT.u, b.u)
                    f = vf[0]
                    act(f[0:m, :], b.t[0:m, :], AF.Copy, b.u, f.u)
                    dma("sp", "vf0", nav[l, r0:r0 + m, :], f[0:m, :], f.u, ())
                    vb_ = vab[st % 2]
                    cp("dve", vb_[0:m, :, 0:128], b.t[0:m, :].rearrange("p (h e) -> p h e", e=128), b.u, vb_.u)
                    dma("sp", "vab%d" % (st % 2), VA[:, r0:r0 + m, :].rearrange("h t e -> t h e"), vb_[0:m, :, :], vb_.u, ())
                    b = banks[7]
                    for k in range(8):
                        mm(b.t[0:m, :], hT[:, k, st * 128:st * 128 + m], wbig[:, k, 2560:3072], k == 0, k == 7,
                           wbig.u + hT.u, b.u)
                    vb_ = vbb[st % 2]
                    cp("dve", vb_[0:m, :, 0:64], b.t[0:m, :].rearrange("p (h e) -> p h e", e=64), b.u, vb_.u)
                    dma("sp", "vbb%d" % (st % 2), VB[r0:r0 + m, :], vb_[0:m, :, :].rearrange("p h e -> p (h e)"), vb_.u, ())
                    if outblk:
                        f = vf[1]
                        act(f[0:m, :], b.t[0:m, :], AF.Copy, b.u, f.u)
                        if c0 == SEQ:
                            for s in range(4):
                                dma("sp", "vf1", nbv_s[l, s, 496:512, :], f[16 * s:16 * s + 16, :], f.u, ())
                        else:
                            dma("sp", "vf1", nbv_p[l, st * 128:st * 128 + m, :], f[0:m, :], f.u, ())
                        b = banks[6]
                        for k in range(8):
                            mm(b.t[0:m, :], hT[:, k, st * 128:st * 128 + m], wbig[:, k, 2048:2560], k == 0, k == 7,
                               wbig.u + hT.u, b.u)
                        f = vf[2]
                        act(f[0:m, :], b.t[0:m, :], AF.Copy, b.u, f.u)
                        if c0 == SEQ:
                            for s in range(4):
                                dma("sp", "vf2", nbk_s[l, s, 496:512, :], f[16 * s:16 * s + 16, :], f.u, ())
                        else:
                            dma("sp", "vf2", nbk_p[l, st * 128:st * 128 + m, :], f[0:m, :], f.u, ())
            for s in range(4):
                dma("sp", "roll", nbk_s[l, s, 0:496, :], I.cbk[l, s, 16:512, :], (), ())
                dma("sp", "roll", nbv_s[l, s, 0:496, :], I.cbv[l, s, 16:512, :], (), ())
            pg.barrier()

    def mmg(out, lhsT, rhs, start, stop, reads, writes):
        pg.add("pe", lambda e: e.matmul(out, lhsT, rhs, start=start, stop=stop, skip_group_check=True), reads, writes)

    def p2(l):
        P2F = int(os.environ.get('MK_P2F', '15'))
        with ExitStack() as ph:
            wout = sb("wout", [128, 8, 1024], BF16, nunits=8, st=ph)
            for k in range(8):
                dma("pool", "wout", wout[:, k, :], I.w_out[l][k * 128:(k + 1) * 128, :], (), [wout.u[k]])
            bias_b = sb("bias_b", [128, 8, 2, 128], BF16, nunits=16, st=ph)
            for hb in range(8):
                for j2 in range(2):
                    dma("pool", "biasb", bias_b[:, hb, j2, :], I.biasT[l, hb, j2], (), [bias_b.u[hb * 2 + j2]])
            cbrow = sb("cbrow", [128, 8, 128], BF16, st=ph)
            memset("pool", cbrow[:], 0.0, cbrow.u)
            dma("pool", "biasb", cbrow[0:1, :, :], I.bconst[l:l + 1, :, :], (), cbrow.u)
            qta = [sb("qta%d" % i, [128, 4, 512], BF16, st=ph) for i in range(2)]
            qtb = [sb("qtb%d" % i, [128, 4, 512], BF16, st=ph) for i in range(2)]
            kta = sb("kta", [128, SEQ], BF16, nunits=8, st=ph)
            vat = sb("vat", [128, 64, 136], BF16, nunits=8, st=ph)
            ktb = [sb("ktb%d" % i, [128, 4, 1024], BF16, st=ph) for i in range(2)]
            vbt = [sb("vbt%d" % i, [128, 8, 576], BF16, st=ph) for i in range(2)]
            pT = [sb("pT%d" % i, [128, 512], BF16, st=ph) for i in range(6)]
            oblk = sb("oblk", [128, 4, 1024], BF16, st=ph)
            oT = sb("oT", [128, 8, 512], BF16, st=ph)
            rz = sb("rz", [128, 8], st=ph)
            t1 = sb("t1", [128, 128], st=ph)
            oa = sb("oa", [128, 512], st=ph)
            junk = sb("junk", [128, 512], st=ph)
            ss = sb("ss", [128, 1], st=ph)

            def accA(m_, rr):
                idx = m_ * 4 + rr
                bk = banks[4 + idx // 3]
                c = (idx % 3) * 129
                return bk, bk.t[:, c:c + 129], (idx % 3 == 0)

            def epiA(h, rr, np_, dst):
                b0, a0, _ = accA(0, rr)
                b1, a1, _ = accA(1, rr)
                V("dve", lambda e: e.reciprocal(rz[0:np_, 0:1], a0[0:np_, 128:129]), b0.u, rz.u)
                V("dve", lambda e: e.reciprocal(rz[0:np_, 1:2], a1[0:np_, 128:129]), b1.u, rz.u)
                ts("dve", rz[0:np_, 1:2], rz[0:np_, 1:2], lam[0:np_, l, 2:3], ALU.mult, rz.u + lam.u, rz.u)
                ts("dve", t1[0:np_, :], a1[0:np_, 0:128], rz[0:np_, 1:2], ALU.mult, b1.u + rz.u, t1.u)
                stt(oa[0:np_, 0:128], a0[0:np_, 0:128], rz[0:np_, 0:1], t1[0:np_, :], ALU.mult, ALU.add,
                    b0.u + rz.u + t1.u, oa.u)
                act(junk[0:np_, 0:128], oa[0:np_, 0:128], AF.Square, oa.u, junk.u + ss.u, accum=ss[0:np_, 0:1])
                act(ss[0:np_, :], ss[0:np_, :], AF.Sqrt, ss.u + epsc.u, ss.u, bias=epsc[0:np_, 0:1], scale=1.0 / 128)
                V("dve", lambda e: e.reciprocal(ss[0:np_, :], ss[0:np_, :]), ss.u, ss.u)
                stt(dst, oa[0:np_, 0:128], ss[0:np_, 0:1], gainA[0:np_, l, :], ALU.mult, ALU.mult,
                    oa.u + ss.u + gainA.u, oblk.u)

            def epiB(np_, dst, w):
                for hf in range(2):
                    bk = banks[4 + hf]
                    v = bk.t[0:np_, 0:4 * w].rearrange("p (h e) -> p h e", e=w)
                    V("dve", lambda e, v=v, hf=hf: e.reciprocal(rz[0:np_, hf * 4:hf * 4 + 4], v[:, :, 64]), bk.u, rz.u)
                    tt("dve", oa[0:np_, hf * 256:(hf + 1) * 256].rearrange("p (h e) -> p h e", e=64), v[:, :, 0:64],
                       rz[0:np_, hf * 4:hf * 4 + 4].unsqueeze(2).to_broadcast([np_, 4, 64]), ALU.mult,
                       bk.u + rz.u, oa.u)
                act(junk[0:np_, :], oa[0:np_, :], AF.Square, oa.u, junk.u + ss.u, accum=ss[0:np_, 0:1])
                act(ss[0:np_, :], ss[0:np_, :], AF.Sqrt, ss.u + epsc.u, ss.u, bias=epsc[0:np_, 0:1], scale=1.0 / 512)
                V("dve", lambda e: e.reciprocal(ss[0:np_, :], ss[0:np_, :]), ss.u, ss.u)
                stt(dst, oa[0:np_, :], ss[0:np_, 0:1], gainB[0:np_, l, :], ALU.mult, ALU.mult,
                    oa.u + ss.u + gainB.u, oblk.u)

            def outproj(x, c0, n):
                for dc in range(8):
                    yb = banks[2 + dc % 2]
                    for kc in range(8):
                        mm(yb.t[:, 0:n], wout[:, kc, dc * 128:(dc + 1) * 128], oT[:, kc, 0:n], kc == 0, kc == 7,
                           wout.u + oT.u, yb.u)
                    for (lo, hi, r) in segs(c0, n):
                        stt(x[:, dc, lo:hi], yb.t[:, lo:hi], modT[l][:, 16 + dc, r:r + 1], x[:, dc, lo:hi],
                            ALU.mult, ALU.add, yb.u + modT[l].u + x.u, x.u)
                store_xb(x, c0, n)

            pcnt = 0
            for bi, (c0, n) in enumerate(pblocks):
                j = c0 // 512
                x = load_xb(c0, n)
                qa = qta[bi % 2]
                qb = qtb[bi % 2]
                dma("sp", "qta%d" % (bi % 2), qa[:], QTA[:, :, c0:c0 + 512].rearrange("h p t -> p h t"), (), qa.u)
                dma("sp", "qtb%d" % (bi % 2), qb[:], QTB[:, :, c0:c0 + 512].rearrange("h p t -> p h t"), (), qb.u)
                nkt = 4 * (j + 1)
                for h in range(4 if P2F & 1 else 0):
                    for u8 in range((nkt * 128 + 1023) // 1024):
                        lo = u8 * 1024
                        hi = min(lo + 1024, nkt * 128)
                        dma("sp", "kta%d" % u8, kta[:, lo:hi], KTA[h, :, lo:hi], (), [kta.u[u8]])
                        dma("sp", "vat%d" % u8, vat[:, lo // 128:hi // 128, :],
                            VA[h, lo:hi, :].rearrange("(kt p) e -> p kt e", p=128), (), [vat.u[u8]])
                    def emit_pv(kt, pts_):
                        r = kt - 4 * j
                        u8 = kt // 8
                        for m_ in range(2):
                            pt = pts_[m_]
                            for rr in range(max(r, 0), 4):
                                bk, ap_, first = accA(m_, rr)
                                mmg(ap_, pt[:, rr * 128:(rr + 1) * 128], vat[:, kt, 0:129],
                                    (kt == 0 and first), kt == 4 * j + rr, pt.u + [vat.u[u8]], bk.u)

                    pend = None
                    for kt in range(nkt):
                        r = kt - 4 * j
                        cs = max(r, 0) * 128
                        u8 = kt // 8
                        cur = []
                        for m_ in range(2):
                            Sb = banks[pcnt % 4]
                            pt = pT[pcnt % 6]
                            pcnt += 1
                            mm(Sb.t[:, cs:512], kta[64 * m_:64 * m_ + 64, kt * 128:(kt + 1) * 128],
                               qa[64 * m_:64 * m_ + 64, h, cs:512], True, r < 0, [kta.u[u8]] + qa.u, Sb.u)
                            if r >= 0:
                                mm(Sb.t[:, cs:cs + 128], mk[:, 0, :], mk[:, 1, :], False, True, mk.u, Sb.u)
                            act(pt[:, cs:512], Sb.t[:, cs:512], AF.Exp, Sb.u, pt.u)
                            cur.append(pt)
                        if pend is not None:
                            emit_pv(*pend)
                        pend = (kt, cur)
                    emit_pv(*pend)
                    for rr in range(4):
                        epiA(h, rr, 128, oblk[:, rr, h * 128:(h + 1) * 128])
                lo_t = max(0, 4 * j - 4)
                hi_t = 4 * j + 4
                kb_ = ktb[bi % 2]
                vb_ = vbt[bi % 2]
                nkl = (hi_t - lo_t) * 128
                dma("sp", "ktb%d" % (bi % 2), kb_[:, :, 0:nkl],
                    KTB[:, :, lo_t * 128:hi_t * 128].rearrange("c p t -> p c t"), (), kb_.u)
                dma("sp", "vbt%d" % (bi % 2), vb_[:, 0:hi_t - lo_t, :],
                    VB[lo_t * 128:hi_t * 128, :].rearrange("(kt p) e -> p kt e", p=128), (), vb_.u)
                for rr in range(4 if P2F & 2 else 0):
                    i_ = 4 * j + rr
                    jjs = [jj for jj in range(5) if i_ - 4 + jj >= 0]

                    def emit_pvb(jj, hf, pt, ktl):
                        for hh in range(4):
                            hb = hf * 4 + hh
                            bk = banks[4 + hf]
                            mmg(bk.t[:, hh * 65:hh * 65 + 65], pt[:, hh * 128:(hh + 1) * 128],
                                vb_[:, ktl, hb * 72:hb * 72 + 65], (jj == jjs[0] and hh == 0), jj == 4,
                                pt.u + vb_.u, bk.u)

                    pendb = None
                    for jj in jjs:
                        ktl = i_ - 4 + jj - lo_t
                        for hf in range(2):
                            Sb = banks[pcnt % 4]
                            pt = pT[pcnt % 6]
                            pcnt += 1
                            for hh in range(4):
                                hb = hf * 4 + hh
                                cc = hb // 2
                                prt = (hb % 2) * 64
                                cols = Sb.t[:, hh * 128:(hh + 1) * 128]
                                mm(cols, kb_[prt:prt + 64, cc, ktl * 128:(ktl + 1) * 128],
                                   qb[prt:prt + 64, cc, rr * 128:(rr + 1) * 128], True, False, kb_.u + qb.u, Sb.u)
                                if jj <= 2:
                                    mm(cols, mk[:, 4, :], cbrow[:, hb, :], False, jj != 0, mk.u + cbrow.u, Sb.u)
                                    if jj == 0:
                                        mm(cols, mk[:, 2, :], mk[:, 3, :], False, True, mk.u, Sb.u)
                                elif jj == 3:
                                    mm(cols, ident_b[:], bias_b[:, hb, 0, :], False, True, ident_b.u + bias_b.u, Sb.u)
                                else:
                                    mm(cols, ident_b[:], bias_b[:, hb, 1, :], False, False, ident_b.u + bias_b.u, Sb.u)
                                    mm(cols, mk[:, 0, :], mk[:, 1, :], False, True, mk.u, Sb.u)
                            act(pt[:, :], Sb.t[:, :], AF.Exp, Sb.u, pt.u)
                            if pendb is not None:
                                emit_pvb(*pendb)
                            pendb = (jj, hf, pt, ktl)
                    emit_pvb(*pendb)
                    epiB(128, oblk[:, rr, 512:1024], 65)
                if not P2F & 4:
                    continue
                for rr in range(4):
                    tb = banks[rr % 2]
                    tbv = tb.t[:, :].bitcast(BF16)
                    for kc in range(8):
                        tr(tbv[:, kc * 128:(kc + 1) * 128], oblk[:, rr, kc * 128:(kc + 1) * 128], ident_b[:],
                           oblk.u + ident_b.u, tb.u)
                    cp("act" if rr % 2 == 0 else "dve", oT[:, :, rr * 128:(rr + 1) * 128],
                       tbv.rearrange("p (k t) -> p k t", t=128), tb.u, oT.u)
                outproj(x, c0, 512)

            x = load_xb(SEQ, NS)
            qas = sb("qas", [128, 4, NS], BF16, st=ph)
            qbs = sb("qbs", [128, 4, NS], BF16, st=ph)
            kan = sb("kan", [128, 4, NS], BF16, st=ph)
            kbn = sb("kbn", [128, 4, NS], BF16, st=ph)
            van = [sb("van%d" % s_, [16, 4, 136], BF16, st=ph) for s_ in range(4)]
            vbn = [sb("vbn%d" % s_, [16, 576], BF16, st=ph) for s_ in range(4)]
            dma("sp", "smp", qas[:], QTA[:, :, SEQ:T].rearrange("h p t -> p h t"), (), qas.u)
            dma("sp", "smp", qbs[:], QTB[:, :, SEQ:T].rearrange("h p t -> p h t"), (), qbs.u)
            dma("sp", "smp", kan[:], KTA[:, :, SEQ:T].rearrange("h p t -> p h t"), (), kan.u)
            dma("sp", "smp", kbn[:], KTB[:, :, SEQ:T].rearrange("h p t -> p h t"), (), kbn.u)
            for s_ in range(4):
                dma("sp", "smp", van[s_][:], VA[:, SEQ + 16 * s_:SEQ + 16 * s_ + 16, :].rearrange("h t e -> t h e"),
                    (), van[s_].u)
                dma("sp", "smp", vbn[s_][:], VB[SEQ + 16 * s_:SEQ + 16 * s_ + 16, :], (), vbn[s_].u)
            kcb = [sb("kcb%d" % i, [128, 512], BF16, st=ph) for i in range(2)]
            vca = [sb("vca%d" % i, [128, 4, 136], BF16, st=ph) for i in range(2)]
            vcb = [sb("vcb%d" % i, [128, 8, 72], BF16, st=ph) for i in range(2)]
            for i in range(2):
                memset("pool", vca[i][:, :, 128:136], 1.0, vca[i].u)
                memset("pool", vcb[i][:, :, 64:72], 1.0, vcb[i].u)
            kTs = [sb("kTs%d" % i, [128, 4, 128], BF16, st=ph) for i in range(2)]
            pts = [sb("pts%d" % i, [128, 128], BF16, st=ph) for i in range(2)]
            os_ = sb("os_", [16, 1024], BF16, st=ph)
            scnt = 0
            for s_ in range(4 if P2F & 8 else 0):
                q0 = 16 * s_
                SF = int(os.environ.get('MK_SF', '31'))
                for kt in ([int(v) for v in os.environ['MK_SK'].split(',')] if os.environ.get('MK_SK') else range(33 if SF & 1 else 0)):
                    kk = 128 if kt < 32 else 16
                    if kt < 32:
                        kc = kcb[scnt % 2]
                        vc = vca[scnt % 2]
                        kT = kTs[scnt % 2]
                        dma("pool", "kcb%d" % (scnt % 2), kc[:], I.cak[l, s_, kt * 128:(kt + 1) * 128, :], (), kc.u)
                        dma("pool", "vca%d" % (scnt % 2), vc[:, :, 0:128],
                            I.cav[l, s_, kt * 128:(kt + 1) * 128, :].rearrange("t (h e) -> t h e", e=128), (), vc.u)
                        tb = banks[0]
                        tbv = tb.t[:, :].bitcast(BF16)
                        for h in range(4):
                            tr(tbv[:, h * 128:(h + 1) * 128], kc[:, h * 128:(h + 1) * 128], ident_b[:],
                               kc.u + ident_b.u, tb.u)
                        cp("dve", kT[:], tbv[:, 0:512].rearrange("p (h t) -> p h t", t=128), tb.u, kT.u)
                        kTv = lambda h, m_, kT=kT: kT[64 * m_:64 * m_ + 64, h, :]
                        kTu = kT.u
                        vv = lambda h, vc=vc: vc[:, h, 0:129]
                        vu = vc.u
                    else:
                        if os.environ.get('MK_SWAP'):
                            kTv = lambda h, m_: kbn[64 * m_:64 * m_ + 64, h, q0:q0 + 16]
                        else:
                            kTv = lambda h, m_: kan[64 * m_:64 * m_ + 64, h, q0:q0 + 16]
                        kTu = kan.u + kbn.u
                        vv = lambda h: van[s_][0:16, h, 0:129]
                        vu = van[s_].u
                    Sbs = [(banks[1], banks[2]), (banks[3], banks[7])][scnt % 2]
                    pt = pts[scnt % 2]
                    scnt += 1
                    SX = int(os.environ.get('MK_SX', '7'))
                    VV_ = os.environ.get('MK_V', '')
                    for m_ in range((1 if VV_ == 'm0' else 2) if SX & 1 else 0):
                        for h in range(4):
                            g_ = h * 2 + m_
                            qq = qbs if os.environ.get('MK_SWAP') else qas
                            mm(Sbs[m_].t[0:kk, h * 16:h * 16 + 16], kTv(h, m_), qq[64 * m_:64 * m_ + 64, h, q0:q0 + 16],
                               True, True, kTu + qas.u + qbs.u, Sbs[m_].u)
                    for m_ in range(2):
                        act(pt[0:kk, m_ * 64:(m_ + 1) * 64], Sbs[m_].t[0:kk, 0:64], AF.Exp, Sbs[m_].u, pt.u)
                    for m_ in range(2 if SX & 4 else 0):
                        for h in range(4):
                            g_ = m_ * 4 + h
                            bk, ap_, first = accA(m_, h)
                            mmg(ap_[0:16, :], pt[0:kk, g_ * 16:g_ * 16 + 16], vv(h), (kt == 0 and first), kt == 32,
                                pt.u + vu, bk.u)
                for h in range(4 if SF & 2 else 0):
                    epiA(h, h, 16, os_[0:16, h * 128:(h + 1) * 128])
                for kt in range(5 if SF & 4 else 0):
                    kk = 128 if kt < 4 else 16
                    if kt < 4:
                        kc = kcb[scnt % 2]
                        vc = vcb[scnt % 2]
                        kT = kTs[scnt % 2]
                        dma("pool", "kcb%d" % (scnt % 2), kc[:], I.cbk[l, s_, kt * 128:(kt + 1) * 128, :], (), kc.u)
                        dma("pool", "vcb%d" % (scnt % 2), vc[:, :, 0:64],
                            I.cbv[l, s_, kt * 128:(kt + 1) * 128, :].rearrange("t (h e) -> t h e", e=64), (), vc.u)
                        tb = banks[scnt % 2]
                        tbv = tb.t[:, :].bitcast(BF16)
                        for cc in range(4):
                            tr(tbv[:, cc * 128:(cc + 1) * 128], kc[:, cc * 128:(cc + 1) * 128], ident_b[:],
                               kc.u + ident_b.u, tb.u)
                        cp("dve", kT[:], tbv[:, 0:512].rearrange("p (h t) -> p h t", t=128), tb.u, kT.u)
                        kTv = lambda cc, prt, kT=kT: kT[prt:prt + 64, cc, :]
                        kTu = kT.u
                        vv = lambda hb, vc=vc: vc[:, hb, 0:65]
                        vu = vc.u
                    else:
                        kTv = lambda cc, prt: kbn[prt:prt + 64, cc, q0:q0 + 16]
                        kTu = kbn.u
                        vv = lambda hb: vbn[s_][0:16, hb * 72:hb * 72 + 65]
                        vu = vbn[s_].u
                    Sb = banks[2 + scnt % 2]
                    pt = pts[scnt % 2]
                    scnt += 1
                    for hb in range(8):
                        cc = hb // 2
                        prt = (hb % 2) * 64
                        cols = Sb.t[0:kk, hb * 16:hb * 16 + 16]
                        mm(cols, kTv(cc, prt), qbs[prt:prt + 64, cc, q0:q0 + 16], True, False, kTu + qbs.u, Sb.u)
                        if kt <= 2:
                            mm(cols, mk[:, 4, 0:kk], cbrow[:, hb, 0:16], False, True, mk.u + cbrow.u, Sb.u)
                        elif kt == 3:
                            mm(cols, ident_b[:], bias_b[:, hb, 0, 0:16], False, True, ident_b.u + bias_b.u, Sb.u)
                        else:
                            mm(cols, ident_b[:, 0:16], bias_b[:, hb, 1, 0:16], False, True,
                               ident_b.u + bias_b.u, Sb.u)
                    act(pt[0:kk, :], Sb.t[0:kk, 0:128], AF.Exp, Sb.u, pt.u)
                    for hb in range(8):
                        bk = banks[4 + hb // 4]
                        hh = hb % 4
                        mmg(bk.t[0:16, hh * 65:hh * 65 + 65], pt[0:kk, hb * 16:hb * 16 + 16], vv(hb),
                            (kt == 0 and hh == 0), kt == 4, pt.u + vu, bk.u)
                if SF & 8:
                    epiB(16, os_[0:16, 512:1024], 65)
                if not SF & 16:
                    continue
                tb = banks[scnt % 2]
                tbv = tb.t[:, :].bitcast(BF16)
                for kc_ in range(8):
                    tr(tbv[:, kc_ * 16:kc_ * 16 + 16], os_[0:16, kc_ * 128:(kc_ + 1) * 128], ident_b[0:16, 0:16],
                       oblk.u + ident_b.u, tb.u)
                cp("dve", oT[:, :, q0:q0 + 16], tbv[:, 0:128].rearrange("p (k t) -> p k t", t=16), tb.u, oT.u)
            if P2F & 8:
                outproj(x, SEQ, NS)
            pg.barrier()

    def pu(l):
        with ExitStack() as ph:
            ubuf = [sb("ubuf%d" % i, [128, D], BF16, st=ph) for i in range(2)]
            vbuf = [sb("vbuf%d" % i, [128, D], BF16, st=ph) for i in range(2)]
            utb = [sb("utb%d" % i, [128, 8, 128], BF16, st=ph) for i in range(2)]
            for ec in range(128):
                ub = ubuf[ec % 2]
                vb_ = vbuf[ec % 2]
                ut = utb[ec % 2]
                dma("pool", "ubuf%d" % (ec % 2), ub[:], I.pk_u[l, ec * 128:(ec + 1) * 128, :], (), ub.u)
                dma("pool", "vbuf%d" % (ec % 2), vb_[:], I.pk_v[l, ec * 128:(ec + 1) * 128, :], (), vb_.u)
                tb = banks[ec % 2]
                tbv = tb.t[:, :].bitcast(BF16)
                for k in range(8):
                    tr(tbv[:, k * 128:(k + 1) * 128], ub[:, k * 128:(k + 1) * 128], ident_b[:], ub.u + ident_b.u, tb.u)
                cp("act" if ec % 2 == 0 else "dve", ut[:], tbv.rearrange("p (k e) -> p k e", e=128), tb.u, ut.u)
                dma("sp", "utb%d" % (ec % 2), UT[:, :, ec * 128:(ec + 1) * 128].rearrange("k p e -> p k e"), ut[:], ut.u, ())
                dma("sp", "vbufo%d" % (ec % 2), VBF[ec * 128:(ec + 1) * 128, :], vb_[:], vb_.u, ())
            pg.barrier()

    def p3(l):
        with ExitStack() as ph:
            nb = NB(ph, "p3")
            h2 = nb.h
            wq = sb("wq", [128, 8, 2048], BF16, nunits=16, st=ph)
            for k in range(8):
                for c2 in range(2):
                    dma("pool", "wq", wq[:, k, c2 * 1024:(c2 + 1) * 1024],
                        I.pk_wq[l][k * 128:(k + 1) * 128, c2 * 1024:(c2 + 1) * 1024], (), [wq.u[k * 2 + c2]])
            kraw = sb("kraw", [128, 16, 128], BF16, st=ph)
            keysT = sb("keysT", [128, 16, 128], BF16, st=ph)
            for g in range(16):
                dma("pool", "kraw", kraw[:, g, :], I.pk_keys[l, g], (), kraw.u)
            for g in range(16):
                tb = banks[g % 2]
                tbv = tb.t[:, :].bitcast(BF16)
                tr(tbv[:, 0:128], kraw[:, g, :], ident_b[:], kraw.u + ident_b.u, tb.u)
                cp("dve", keysT[:, g, :], tbv[:, 0:128], tb.u, keysT.u)
            qT = sb("qT", [128, 16, 512], BF16, st=ph)
            sc = sb("sc", [128, 16, 128], st=ph)
            sc2 = sb("sc2", [128, 16, 128], st=ph)
            sv = sb("sv", [128, 16, 16], st=ph)
            si = sb("si", [128, 16, 16], U32, st=ph)
            sif = sb("sif", [128, 16, 16], st=ph)
            cand = sb("cand", [128, 8, 256], st=ph)
            cand2 = sb("cand2", [128, 8, 256], st=ph)
            cv = sb("cv", [128, 8, 16], st=ph)
            ci = sb("ci", [128, 8, 16], U32, st=ph)
            cab = sb("cab", [128, 2, 8, 16], U32, st=ph)
            cabf = sb("cabf", [128, 2, 8, 16], st=ph)
            eq = sb("eq", [128, 8, 16, 16], st=ph)
            res = sb("res", [128, 3, 128], st=ph)
            gs = sb("gs", [128, 8], st=ph)
            rts = [sb("rts%d" % i, [128, 3, 128], st=ph) for i in range(2)]
            bcnt = 0
            for (c0, n) in blocks:
                x = load_xb(c0, n)
                norm_mod(nb, x, c0, n, A2[l], modT[l], 24, banks[7])
                dma("sp", "h2st", H2T[:, :, c0:c0 + n].rearrange("k p t -> p k t"), h2[:, :, 0:n], h2.u, ())
                for qc in range(16):
                    b = banks[qc % 2]
                    for k in range(8):
                        mm(b.t[:, 0:n], wq[:, k, qc * 128:(qc + 1) * 128], h2[:, k, 0:n], k == 0, k == 7,
                           wq.u + h2.u, b.u)
                    cp("act" if qc % 2 == 0 else "dve", qT[:, qc, 0:n], b.t[:, 0:n], b.u, qT.u)
                for st in range((n + 127) // 128):
                    m = min(128, n - st * 128)
                    r0 = c0 + st * 128
                    for qc in range(16):
                        b = banks[2 + qc // 4]
                        mm(b.t[0:m, (qc % 4) * 128:(qc % 4 + 1) * 128], qT[:, qc, st * 128:st * 128 + m],
                           keysT[:, qc, :], True, True, qT.u + keysT.u, b.u)
                    for q4 in range(4):
                        b = banks[2 + q4]
                        cp("act", sc[0:m, q4 * 4:(q4 + 1) * 4, :], b.t[0:m, :].rearrange("p (g n) -> p g n", n=128),
                           b.u, sc.u)
                    for g in range(16):
                        vmax(sv[0:m, g, 0:8], sc[0:m, g, :], sc.u, sv.u)
                    for g in range(16):
                        vmaxidx(si[0:m, g, 0:8], sv[0:m, g, 0:8], sc[0:m, g, :], sc.u + sv.u, si.u)
                    for g in range(16):
                        vmatch(sc2[0:m, g, :], sv[0:m, g, 0:8], sc[0:m, g, :], sc.u + sv.u, sc2.u)
                    for g in range(16):
                        vmax(sv[0:m, g, 8:16], sc2[0:m, g, :], sc2.u, sv.u)
                    for g in range(16):
                        vmaxidx(si[0:m, g, 8:16], sv[0:m, g, 8:16], sc2[0:m, g, :], sc2.u + sv.u, si.u)
                    svv = sv[0:m].rearrange("p (h two) a -> p h two a", two=2)
                    tt("dve", cand[0:m].rearrange("p h (a b) -> p h a b", b=16),
                       svv[:, :, 0, :].unsqueeze(3).to_broadcast([m, 8, 16, 16]),
                       svv[:, :, 1, :].unsqueeze(2).to_broadcast([m, 8, 16, 16]), ALU.add, sv.u, cand.u)
                    for h in range(8):
                        vmax(cv[0:m, h, 0:8], cand[0:m, h, :], cand.u, cv.u)
                    for h in range(8):
                        vmaxidx(ci[0:m, h, 0:8], cv[0:m, h, 0:8], cand[0:m, h, :], cand.u + cv.u, ci.u)
                    for h in range(8):
                        vmatch(cand2[0:m, h, :], cv[0:m, h, 0:8], cand[0:m, h, :], cand.u + cv.u, cand2.u)
                    for h in range(8):
                        vmax(cv[0:m, h, 8:16], cand2[0:m, h, :], cand2.u, cv.u)
                    for h in range(8):
                        vmaxidx(ci[0:m, h, 8:16], cv[0:m, h, 8:16], cand2[0:m, h, :], cand2.u + cv.u, ci.u)
                    ts("dve", cab[0:m, 0], ci[0:m], 4, ALU.logical_shift_right, ci.u, cab.u)
                    ts("dve", cab[0:m, 1], ci[0:m], 15, ALU.bitwise_and, ci.u, cab.u)
                    cp("dve", cabf[0:m], cab[0:m], cab.u, cabf.u)
                    cp("dve", sif[0:m], si[0:m], si.u, sif.u)
                    sifv = sif[0:m].rearrange("p (h two) a -> p h two a", two=2)
                    for w2 in range(2):
                        tt("dve", eq[0:m], cabf[0:m, w2].unsqueeze(3).to_broadcast([m, 8, 16, 16]),
                           iota16[0:m, :].unsqueeze(1).unsqueeze(1).to_broadcast([m, 8, 16, 16]), ALU.is_equal,
                           cabf.u + iota16.u, eq.u)
                        tt("dve", eq[0:m], eq[0:m], sifv[:, :, w2, :].unsqueeze(2).to_broadcast([m, 8, 16, 16]),
                           ALU.mult, eq.u + sif.u, eq.u)
                        vredsum(res[0:m, w2, :].rearrange("p (h k) -> p h k", k=16), eq[0:m], eq.u, res.u)
                    resg = res[0:m, 2, :].rearrange("p (h k) -> p h k", k=16)
                    tt("dve", resg, cv[0:m], cv[0:m, :, 0:1].to_broadcast([m, 8, 16]), ALU.subtract, cv.u, res.u)
                    act(resg, resg, AF.Exp, res.u, res.u)
                    vredsum(gs[0:m, :], res[0:m, 2, :].rearrange("p (h k) -> p h k", k=16), res.u, gs.u)
                    vrecip(gs[0:m, :], gs[0:m, :], gs.u, gs.u)
                    tt("dve", resg, resg, gs[0:m, :].unsqueeze(2).to_broadcast([m, 8, 16]), ALU.mult, res.u + gs.u, res.u)
                    tb = banks[6]
                    for q3 in range(3):
                        tr(tb.t[:, q3 * 128:q3 * 128 + m], res[0:m, q3, :], ident_f[0:m, 0:m], res.u + ident_f.u, tb.u)
                    ro = rts[bcnt % 2]
                    cp("act", ro[:, :, 0:m], tb.t[:, 0:384].rearrange("p (q t) -> p q t", t=128)[:, :, 0:m], tb.u, ro.u)
                    dma("sp", "rts%d" % (bcnt % 2), RT[:, :, r0:r0 + m].rearrange("q p t -> p q t"), ro[:, :, 0:m], ro.u, ())
                    bcnt += 1
            pg.barrier()

    def p4(l):
        TG = 256
        with ExitStack() as ph:
            Wall = sb("Wall", [128, TG, 128], BF16, st=ph)
            ohj = [sb("ohj%d" % i, [128, 32, 128], BF16, st=ph) for i in range(2)]
            ohi = [sb("ohi%d" % i, [128, 32, 128], BF16, st=ph) for i in range(2)]
            ust = [sb("ust%d" % i, [128, 8, 512], BF16, st=ph) for i in range(2)]
            vst = [sb("vst%d" % i, [128, 4, D], BF16, st=ph) for i in range(2)]
            h2g = [sb("h2g%d" % i, [128, 8, TG], BF16, st=ph) for i in range(2)]
            rtg = [sb("rtg%d" % i, [128, 3, TG], st=ph) for i in range(2)]
            gl = [sb("gl%d" % i, [128, TG], BF16, st=ph) for i in range(3)]
            pTt = [sb("pTt%d" % i, [128, TG], BF16, st=ph) for i in range(3)]
            pgroups = [(c, TG) for c in range(0, SEQ, TG)]
            if NBLK:
                pgroups = pgroups[:NBLK]
            groups = pgroups + [(SEQ, NS)]
            pc = 0
            wc = 0
            sc_ = 0
            def load_grp(gi_):
                c0, n = groups[gi_]
                h2 = h2g[gi_ % 2]
                rt = rtg[gi_ % 2]
                dma("sp", "h2g%d" % (gi_ % 2), h2[:, :, 0:n], H2T[:, :, c0:c0 + n].rearrange("k p t -> p k t"), (), h2.u)
                dma("sp", "rtg%d" % (gi_ % 2), rt[:, :, 0:n], RT[:, :, c0:c0 + n].rearrange("q p t -> p q t"), (), rt.u)

            def gen_piece(gi_, t0):
                rt = rtg[gi_ % 2]
                pi_ = (t0 // 32) % 2
                oj = ohj[pi_]
                oi = ohi[pi_]
                for t in range(32):
                    tk = t0 + t
                    ts("dve", oj[:, t, :], iota_b[:], rt[:, 1, tk:tk + 1], ALU.is_equal, iota_b.u + rt.u, oj.u)
                    ts("dve", oi[:, t, :], iota_b[:], rt[:, 0, tk:tk + 1], ALU.is_equal, iota_b.u + rt.u, oi.u,
                       s2=rt[:, 2, tk:tk + 1], op1=ALU.mult)
                return oj, oi

            load_grp(0)
            pregen = {}
            for gi_, (c0, n) in enumerate(groups):
                h2 = h2g[gi_ % 2]
                rt = rtg[gi_ % 2]
                x = load_xb(c0, n)
                for t0 in range(0, n, 32):
                    if (gi_, t0) in pregen:
                        oj, oi = pregen[(gi_, t0)]
                    else:
                        oj, oi = gen_piece(gi_, t0)
                    for t4 in range(0, 32, 4):
                        wb = banks[6 + wc % 2]
                        wc += 1
                        for t in range(4):
                            mm(wb.t[:, t * 128:(t + 1) * 128], oj[:, t4 + t, :], oi[:, t4 + t, :], True, True,
                               oj.u + oi.u, wb.u)
                        cp("act", Wall[:, t0 + t4:t0 + t4 + 4, :],
                           wb.t[:, :].rearrange("p (t i) -> p t i", i=128), wb.u, Wall.u)
                    if t0 + 32 >= n and gi_ + 1 < len(groups):
                        load_grp(gi_ + 1)
                        nn = groups[gi_ + 1][1]
                        for tq in range(0, min(64, nn), 32):
                            pregen[(gi_ + 1, tq)] = gen_piece(gi_ + 1, tq)
                for eb in range(32):
                    us = ust[sc_ % 2]
                    vs = vst[sc_ % 2]
                    dma("sp", "ust%d" % (sc_ % 2), us[:], UT[:, :, eb * 512:(eb + 1) * 512].rearrange("k p e -> p k e"),
                        (), us.u)
                    dma("sp", "vst%d" % (sc_ % 2), vs[:], VBF[eb * 512:(eb + 1) * 512, :].rearrange("(c p) d -> p c d", p=128),
                        (), vs.u)
                    sc_ += 1
                    for c4 in range(4):
                        ec = eb * 4 + c4
                        ab = banks[4 + ec % 2]
                        for k in range(8):
                            mm(ab.t[:, 0:n], us[:, k, c4 * 128:(c4 + 1) * 128], h2[:, k, 0:n], k == 0, k == 7,
                               us.u + h2.u, ab.u)
                        g_ = gl[ec % 3]
                        p_ = pTt[ec % 3]
                        act(g_[:, 0:n], ab.t[:, 0:n], AF.Gelu_apprx_tanh, ab.u, g_.u)
                        tt("pool", p_[:, 0:n], g_[:, 0:n], Wall[:, 0:n, ec], ALU.mult, g_.u + Wall.u, p_.u)
                        for dc in range(8):
                            bk = banks[dc // 2]
                            mmg(bk.t[:, (dc % 2) * 256:(dc % 2) * 256 + n], vs[:, c4, dc * 128:(dc + 1) * 128],
                                p_[:, 0:n], (ec == 0 and dc % 2 == 0), ec == 127, vs.u + p_.u, bk.u)
                for dc in range(8):
                    bk = banks[dc // 2]
                    for (lo, hi, r) in segs(c0, n):
                        o_ = (dc % 2) * 256
                        stt(x[:, dc, lo:hi], bk.t[:, o_ + lo:o_ + hi], modT[l][:, 40 + dc, r:r + 1], x[:, dc, lo:hi],
                            ALU.mult, ALU.add, bk.u + modT[l].u + x.u, x.u)
                store_xb(x, c0, n)
            pg.barrier()

    def pfinal():
        with ExitStack() as ph:
            gfin = sb("gfin", [128, D], st=ph)
            dma("sp", "c0", gfin[:], I.g_final[0:1, :].partition_broadcast(128), (), gfin.u)
            xo = [sb("xo%d" % i, [128, D], st=ph) for i in range(2)]
            yo = [sb("yo%d" % i, [128, D], st=ph) for i in range(2)]
            xs_ = [sb("xs%d" % i, [128, 8, 128], st=ph) for i in range(2)]
            junk = sb("fjunk", [128, D], st=ph)
            ss = sb("fss", [128, 1], st=ph)
            tiles = list(range((T + 127) // 128))
            if NBLK:
                tiles = tiles[:4 * NBLK] + tiles[-5:]
            for ti in tiles:
                r0 = ti * 128
                m = min(128, T - r0)
                xs = xs_[ti % 2]
                dma("sp", "xs%d" % (ti % 2), xs[:, :, 0:m], xT[:, :, r0:r0 + m].rearrange("k p t -> p k t"), (), xs.u)
                o = xo[ti % 2]
                for hf in range(2):
                    b = banks[(ti * 2 + hf) % 4]
                    for kk in range(4):
                        k = hf * 4 + kk
                        tr(b.t[0:m, kk * 128:(kk + 1) * 128], xs[:, k, 0:m], ident_f[:], xs.u + ident_f.u, b.u)
                    cp("act" if hf == 0 else "dve", o[0:m, hf * 512:(hf + 1) * 512], b.t[0:m, :], b.u, o.u)
                act(junk[0:m, :], o[0:m, :], AF.Square, o.u, junk.u + ss.u, accum=ss[0:m, 0:1])
                act(ss[0:m, :], ss[0:m, :], AF.Sqrt, ss.u + epsc.u, ss.u, bias=epsc[0:m, 0:1], scale=1.0 / D)
                V("dve", lambda e, m=m: e.reciprocal(ss[0:m, :], ss[0:m, :]), ss.u, ss.u)
                y = yo[ti % 2]
                stt(y[0:m, :], o[0:m, :], ss[0:m, 0:1], gfin[0:m, :], ALU.mult, ALU.mult, o.u + ss.u + gfin.u, y.u)
                dma("sp", "yo%d" % (ti % 2), y_out[r0:r0 + m, :], y[0:m, :], y.u, ())

    for l in range(DEPTH):
        p1(l)
        if stage("p1_%d" % l):
            return finish()
        p2(l)
        if stage("p2_%d" % l):
            return finish()
        pu(l)
        p3(l)
        if stage("p3_%d" % l):
            return finish()
        p4(l)
        if stage("p4_%d" % l):
            return finish()
    pfinal()
    return finish()


_CACHE = {}


def _rope_tables():
    half = 8
    inv = (np.float32(500000.0) ** (-(np.arange(0, 16, 2, dtype=np.float32)) / np.float32(16))).astype(np.float32)
    pos = np.concatenate([np.arange(SEQ), np.tile(PAST + np.arange(16), 4)]).astype(np.float32)
    ang = (pos[:, None] * inv[None, :]).astype(np.float32)
    return np.cos(ang).astype(np.float32), np.sin(ang).astype(np.float32)


def make_in_maps(inp):
    f = lambda a: np.ascontiguousarray(np.asarray(a, dtype=np.float32))
    rel = f(inp["rel_bias"])
    q = np.arange(128)[None, :]
    k = np.arange(128)[:, None]
    d3 = np.clip(128 + q - k, -128, 128) + 128
    d4 = np.clip(q - k, -128, 128) + 128
    biasT = np.stack([rel[:, :, d3], rel[:, :, d4]], axis=2)
    bconst = np.ascontiguousarray(np.broadcast_to(rel[:, :, 256][:, :, None], (DEPTH, 8, 128)))
    rc, rs = _rope_tables()
    shared = {
        "w_ada": f(inp["w_ada"]),
        "b_adaT": f(np.asarray(inp["b_ada"]).reshape(DEPTH, 48, 128).transpose(0, 2, 1)),
        "g_attnT": f(np.asarray(inp["g_attn"]).reshape(DEPTH, 8, 128).transpose(0, 2, 1)),
        "g_ffnT": f(np.asarray(inp["g_ffn"]).reshape(DEPTH, 8, 128).transpose(0, 2, 1)),
        "w_in": f(inp["w_in"]),
        "lamv": f(np.concatenate([np.asarray(inp["lam_q1"]), np.asarray(inp["lam_k1"]),
                                  np.asarray(inp["lam_q2"]), np.asarray(inp["lam_k2"])], axis=1)),
        "a_gain": f(inp["a_gain"]),
        "biasT": f(biasT),
        "bconst": f(bconst),
        "b_gain": f(inp["b_gain"]),
        "w_out": f(inp["w_out"]),
        "pk_wq": f(inp["pk_wq"]),
        "pk_keys": f(np.asarray(inp["pk_keys"]).reshape(DEPTH, 16, 128, 128)),
        "pk_u": f(inp["pk_u"]),
        "pk_v": f(inp["pk_v"]),
        "g_final": f(np.asarray(inp["g_final"]).reshape(1, D)),
        "ident": np.eye(128, dtype=np.float32),
        "ropec": rc,
        "ropes": rs,
        "iota_in": np.ascontiguousarray(np.broadcast_to(np.arange(128, dtype=np.float32)[None, :], (128, 128))),
    }
    xp = np.asarray(inp["x_prompt"])
    xs = np.asarray(inp["x_sample"])
    cp_ = np.asarray(inp["c_prompt"])
    cs = np.asarray(inp["c_sample"])
    maps = []
    for c in range(8):
        b = c % 4
        sl = slice(4 * c, 4 * c + 4)
        c5 = np.concatenate([cp_[b:b + 1], cs[sl]], axis=0)
        m = dict(shared)
        m["xin"] = f(np.concatenate([xp[b], xs[sl].reshape(NS, D)], axis=0))
        m["cT"] = f(c5.reshape(5, 8, 128).transpose(2, 1, 0))
        m["cak"] = f(np.asarray(inp["cache_a_k"])[:, sl].reshape(DEPTH, 4, PAST, 512))
        m["cav"] = f(np.asarray(inp["cache_a_v"])[:, sl].reshape(DEPTH, 4, PAST, 512))
        m["cbk"] = f(np.asarray(inp["cache_b_k"])[:, sl].reshape(DEPTH, 4, 512, 512))
        m["cbv"] = f(np.asarray(inp["cache_b_v"])[:, sl].reshape(DEPTH, 4, 512, 512))
        maps.append(m)
    return maps


def assemble(results):
    y_p = np.stack([results[b]["y"][:SEQ] for b in range(4)])
    y_s = np.concatenate([results[c]["y"][SEQ:].reshape(4, 16, D) for c in range(8)])
    nakp = np.stack([results[b]["nak"][:, :SEQ] for b in range(4)], axis=1).reshape(DEPTH, 4, SEQ, 4, 128)
    navp = np.stack([results[b]["nav"][:, :SEQ] for b in range(4)], axis=1).reshape(DEPTH, 4, SEQ, 4, 128)
    nbkp = np.stack([results[b]["nbk_p"] for b in range(4)], axis=1).reshape(DEPTH, 4, 512, 8, 64)
    nbvp = np.stack([results[b]["nbv_p"] for b in range(4)], axis=1).reshape(DEPTH, 4, 512, 8, 64)
    naks = np.concatenate([results[c]["nak"][:, SEQ:].reshape(DEPTH, 4, 16, 4, 128) for c in range(8)], axis=1)
    navs = np.concatenate([results[c]["nav"][:, SEQ:].reshape(DEPTH, 4, 16, 4, 128) for c in range(8)], axis=1)
    nbks = np.concatenate([results[c]["nbk_s"].reshape(DEPTH, 4, 512, 8, 64) for c in range(8)], axis=1)
    nbvs = np.concatenate([results[c]["nbv_s"].reshape(DEPTH, 4, 512, 8, 64) for c in range(8)], axis=1)
    return tuple(np.ascontiguousarray(a, dtype=np.float32) for a in
                 (y_p, y_s, nakp, navp, nbkp, nbvp, naks, navs, nbks, nbvs))


def kernel(**inputs):
    nc, es = build()
    maps = make_in_maps(inputs)
    used = set(build.used_inputs)
    maps = [{k: v for k, v in m.items() if k in used} for m in maps]
    res = run_bass_kernel_spmd(nc, maps, core_ids=list(range(8)))
    return assemble(res.results)
```

```python
import os
import math
import numpy as np
from contextlib import ExitStack
import concourse.bass as bass
import concourse.mybir as mybir
from concourse.bass_utils import run_bass_kernel_spmd

F32 = mybir.dt.float32
BF16 = mybir.dt.bfloat16
U32 = mybir.dt.uint32
AF = mybir.ActivationFunctionType
ALU = mybir.AluOpType
AX = mybir.AxisListType

D = 1024
SEQ = 8192
NS = 64
T = SEQ + NS
DEPTH = 2
PAST = 4096
EPS = 1e-6
NEXP = 16384
NEG = -30000.0


class Unit:
    __slots__ = ("name", "w", "r", "excl")

    def __init__(self, name):
        self.name = name
        self.w = None
        self.r = {}
        self.excl = False


class Op:
    __slots__ = ("eng", "fn", "deps", "inc", "cnt", "dma")


ENGS = ("pe", "act", "dve", "pool", "sp")


class Prog:
    def __init__(self, nc, es):
        self.nc = nc
        self.es = es
        self.ops = {e: [] for e in ENGS}
        self.eng_sem = {e: es.enter_context(nc.semaphore("s_" + e)) for e in ENGS}
        self.dma_sem = {}
        self.dma_cnt = {}
        self.nunits = 0

    def unit(self, name=None):
        self.nunits += 1
        return Unit(name or "u%d" % self.nunits)

    def _dep(self, op, d, kind):
        if d is op:
            return
        if d.dma is None and op.dma is None and d.eng == op.eng and kind != "raw":
            return
        op.deps.append(d)
        if d.dma is None:
            d.inc = True

    def add(self, eng, fn, reads=(), writes=(), dma=None):
        op = Op()
        op.eng = eng
        op.fn = fn
        op.deps = []
        op.inc = False
        op.cnt = 0
        op.dma = dma
        ex = [u for u in reads if u.excl]
        if ex:
            writes = list(writes) + [u for u in ex if u not in writes]
        for u in reads:
            if u.w is not None:
                self._dep(op, u.w, "raw")
        for u in writes:
            if u.w is not None:
                self._dep(op, u.w, "waw")
            for r in u.r.values():
                self._dep(op, r, "war")
        key = dma if dma is not None else eng
        for u in reads:
            u.r[key] = op
        for u in writes:
            u.w = op
            u.r = {}
        if dma is not None:
            if dma not in self.dma_sem:
                self.dma_sem[dma] = self.es.enter_context(self.nc.semaphore("d_" + dma))
                self.dma_cnt[dma] = 0
            self.dma_cnt[dma] += 16
            op.cnt = self.dma_cnt[dma]
        self.ops[eng].append(op)
        return op

    def _sem(self, d):
        return self.dma_sem[d.dma] if d.dma is not None else self.eng_sem[d.eng]

    def barrier(self):
        if os.environ.get("MK_NOBAR"):
            return
        deps = []
        for e in ENGS:
            for op in reversed(self.ops[e]):
                if op.dma is None and op.fn is not None:
                    deps.append(op)
                    break
        lastd = {}
        for e in ENGS:
            for op in self.ops[e]:
                if op.dma is not None:
                    lastd[op.dma] = op
        deps += list(lastd.values())
        for e in ENGS:
            op = Op()
            op.eng = e
            op.fn = None
            op.deps = []
            op.inc = False
            op.cnt = 0
            op.dma = None
            for d in deps:
                if d.dma is None and d.eng == e:
                    continue
                op.deps.append(d)
                if d.dma is None:
                    d.inc = True
            self.ops[e].append(op)

    def emit(self):
        nc = self.nc
        for e in ENGS:
            c = 0
            for op in self.ops[e]:
                if op.dma is None and op.inc:
                    c += 1
                    op.cnt = c
        final = [(self.dma_sem[k], v) for k, v in self.dma_cnt.items()]

        def run(ename, eng):
            waited = {}
            for op in self.ops[ename]:
                need = {}
                for d in op.deps:
                    s = self._sem(d)
                    k = id(s)
                    if k not in need or need[k][1] < d.cnt:
                        need[k] = (s, d.cnt)
                for k, (s, c) in need.items():
                    if waited.get(k, 0) < c:
                        eng.wait_ge(s, c)
                        waited[k] = c
                if op.fn is None:
                    continue
                inst = op.fn(eng)
                if op.dma is not None:
                    inst.then_inc(self.dma_sem[op.dma], 16)
                elif op.inc:
                    inst.then_inc(self.eng_sem[ename], 1)
            if ename == "sp":
                for s, c in final:
                    eng.wait_ge(s, c)

        with nc.Block() as block:
            @block.tensor
            def _(e):
                run("pe", e)

            @block.scalar
            def _(e):
                run("act", e)

            @block.vector
            def _(e):
                run("dve", e)

            @block.gpsimd
            def _(e):
                run("pool", e)

            @block.sync
            def _(e):
                run("sp", e)


class Tl:
    def __init__(self, pg, t, nunits=1):
        self.t = t
        self.u = [pg.unit() for _ in range(nunits)]

    def __getitem__(self, k):
        return self.t[k]


def build(stop=None):
    nc = bass.Bass("TRN2", target_bir_lowering=False)
    es = ExitStack()
    pg = Prog(nc, es)
    stop = stop or os.environ.get("MK_STOP", "")

    def din(name, shape, dt=F32):
        return nc.dram_tensor(name, list(shape), dt, kind="ExternalInput").ap()

    def dout(name, shape, dt=F32):
        return nc.dram_tensor(name, list(shape), dt, kind="ExternalOutput").ap()

    def dscr(name, shape, dt):
        return nc.dram_tensor(name, list(shape), dt).ap()

    IN_SHAPES = {
        "xin": [T, D], "cT": [128, 8, 5], "cak": [DEPTH, 4, PAST, 512], "cav": [DEPTH, 4, PAST, 512],
        "cbk": [DEPTH, 4, 512, 512], "cbv": [DEPTH, 4, 512, 512], "w_ada": [DEPTH, D, 6 * D],
        "b_adaT": [DEPTH, 128, 48], "g_attnT": [DEPTH, 128, 8], "g_ffnT": [DEPTH, 128, 8],
        "w_in": [DEPTH, D, 3072], "lamv": [DEPTH, 256], "a_gain": [DEPTH, 128],
        "biasT": [DEPTH, 8, 2, 128, 128], "bconst": [DEPTH, 8, 128], "b_gain": [DEPTH, 512],
        "w_out": [DEPTH, D, D], "pk_wq": [DEPTH, D, 2048], "pk_keys": [DEPTH, 16, 128, 128],
        "pk_u": [DEPTH, NEXP, D], "pk_v": [DEPTH, NEXP, D], "g_final": [1, D], "ident": [128, 128],
        "ropec": [T, 8], "ropes": [T, 8], "iota_in": [128, 128],
    }
    _ins = {}

    class _IN:
        def __getattr__(self, name):
            if name not in _ins:
                _ins[name] = din(name, IN_SHAPES[name])
            return _ins[name]

    I = _IN()
    build.used_inputs = _ins

    y_out = dout("y", [T, D])
    nak = dout("nak", [DEPTH, T, 512])
    nav = dout("nav", [DEPTH, T, 512])
    nbk_p = dout("nbk_p", [DEPTH, 512, 512])
    nbv_p = dout("nbv_p", [DEPTH, 512, 512])
    nbk_s = dout("nbk_s", [DEPTH, 4, 512, 512])
    nbv_s = dout("nbv_s", [DEPTH, 4, 512, 512])

    xT = dscr("xT", [8, 128, T], F32)
    QTA = dscr("QTA", [4, 128, T], BF16)
    KTA = dscr("KTA", [4, 128, T], BF16)
    VA = dscr("VA", [4, T, 136], BF16)
    QTB = dscr("QTB", [4, 128, T], BF16)
    KTB = dscr("KTB", [4, 128, T], BF16)
    VB = dscr("VB", [T, 8 * 72], BF16)
    UT = dscr("UT", [8, 128, NEXP], BF16)
    VBF = dscr("VBF", [NEXP, D], BF16)
    H2T = dscr("H2T", [8, 128, T], BF16)
    RT = dscr("RT", [3, 128, T], F32)
    u_xT = pg.unit("xT")
    u_qk = pg.unit("qkscr")
    u_uv = pg.unit("uvscr")
    u_h2 = pg.unit("h2scr")
    u_rt = pg.unit("rtscr")
    u_out = pg.unit("outs")

    sbn = [0]

    def sb(name, shape, dt=F32, nunits=1, st=None):
        sbn[0] += 1
        return Tl(pg, (st or es).enter_context(nc.sbuf_tensor("%s_%d" % (name, sbn[0]), list(shape), dt)), nunits)

    banks = [Tl(pg, es.enter_context(nc.psum_tensor("bank%d" % i, [128, 512], F32))) for i in range(8)]

    for b_ in banks:
        b_.u[0].excl = True

    def bk_bf(i):
        return banks[i].t[:].bitcast(BF16)

    PAR_KEYS = {"wbig", "wq", "wout", "biasb"}
    keyunit = {}

    def dma(eng, key, out, in_, reads, writes):
        if key not in PAR_KEYS:
            if key not in keyunit:
                keyunit[key] = pg.unit("k_" + key)
            writes = list(writes) + [keyunit[key]]
        pg.add(eng, lambda e: e.dma_start(out=out, in_=in_), reads, writes, dma=key)

    def mm(out, lhsT, rhs, start, stop, reads, writes):
        pg.add("pe", lambda e: e.matmul(out, lhsT, rhs, start=start, stop=stop), reads, writes)

    def tr(out, in_, idn, reads, writes):
        pg.add("pe", lambda e: e.transpose(out, in_, idn), reads, writes)

    def act(out, in_, func, reads, writes, bias=None, scale=None, accum=None):
        def f(e):
            kw = {}
            if bias is not None:
                kw["bias"] = bias
            if scale is not None:
                kw["scale"] = scale
            if accum is not None:
                kw["accum_out"] = accum
            return e.activation(out=out, in_=in_, func=func, **kw)
        pg.add("act", f, reads, writes)

    def V(eng, fn, reads, writes):
        pg.add(eng, fn, reads, writes)

    def tt(eng, out, a, b, op, reads, writes):
        pg.add(eng, lambda e: e.tensor_tensor(out, a, b, op), reads, writes)

    def ts(eng, out, a, s1, op0, reads, writes, s2=None, op1=None):
        if op1 is None:
            pg.add(eng, lambda e: e.tensor_scalar(out, a, s1, None, op0), reads, writes)
        else:
            pg.add(eng, lambda e: e.tensor_scalar(out, a, s1, s2, op0, op1), reads, writes)

    def stt(out, a, s, b, op0, op1, reads, writes):
        pg.add("dve", lambda e: e.scalar_tensor_tensor(out, a, s, b, op0, op1), reads, writes)

    def vmax(out, in_, reads, writes):
        pg.add("dve", lambda e: e.max(out=out, in_=in_), reads, writes)

    def vmaxidx(out, in_max, in_values, reads, writes):
        pg.add("dve", lambda e: e.max_index(out=out, in_max=in_max, in_values=in_values), reads, writes)

    def vmatch(out, rep, vals, reads, writes):
        pg.add("dve", lambda e: e.match_replace(out=out, in_to_replace=rep, in_values=vals, imm_value=-1e30),
               reads, writes)

    def vredsum(out, in_, reads, writes):
        pg.add("dve", lambda e: e.reduce_sum(out, in_, AX.X), reads, writes)

    def vrecip(out, in_, reads, writes):
        pg.add("dve", lambda e: e.reciprocal(out, in_), reads, writes)

    def cp(eng, out, in_, reads, writes):
        if eng == "act":
            pg.add("act", lambda e: e.copy(out, in_), reads, writes)
        else:
            pg.add(eng, lambda e: e.tensor_copy(out, in_), reads, writes)

    def memset(eng, ap, val, writes):
        pg.add(eng, lambda e: e.memset(ap, val), (), writes)

    ident_f = sb("ident_f", [128, 128])
    ident_b = sb("ident_b", [128, 128], BF16)
    ones_b = sb("ones_b", [128, 128], BF16)
    iota_b = sb("iota_b", [128, 128], BF16)
    iota16 = sb("iota16", [128, 16])
    mk = sb("mk", [128, 5, 128], BF16)
    epsc = sb("epsc", [128, 1])
    dma("sp", "c0", ident_f[:], I.ident[:, :], (), ident_f.u)
    dma("pool", "c1", ident_b[:], I.ident[:, :], (), ident_b.u)
    dma("pool", "c1", iota_b[:], I.iota_in[:, :], (), iota_b.u)
    dma("sp", "c0", iota16[:], I.iota_in[:, 0:16], (), iota16.u)
    memset("dve", ones_b[:], 1.0, ones_b.u)
    memset("dve", epsc[:], EPS, epsc.u)
    memset("dve", mk[:], 0.0, mk.u)
    memset("dve", mk[0:1, 0, 0:64], 0.0, mk.u)
    memset("dve", mk[0:1, 0, 64:128], 1.0, mk.u)
    memset("dve", mk[0:1, 1, 0:64], NEG, mk.u)
    memset("dve", mk[0:1, 1, 64:128], 0.0, mk.u)
    memset("dve", mk[0:1, 2, 0:64], 1.0, mk.u)
    memset("dve", mk[0:1, 2, 64:128], 0.0, mk.u)
    memset("dve", mk[0:1, 3, 0:64], 0.0, mk.u)
    memset("dve", mk[0:1, 3, 64:128], NEG, mk.u)
    memset("dve", mk[0:1, 4, :], 1.0, mk.u)

    NBLK = int(os.environ.get("MK_NB", "0"))
    DBG = bool(os.environ.get("MK_DBG"))
    dbg_xT = dout("dbg_xT", [8, 128, T]) if DBG else None
    dbg_RT = dout("dbg_RT", [3, 128, T]) if DBG else None

    def stage(name):
        return stop == name

    def finish():
        if DBG:
            pg.barrier()
            dma("sp", "dbg", dbg_xT[:, :, :], xT[:, :, :], (), ())
            dma("sp", "dbg", dbg_RT[:, :, :], RT[:, :, :], (), ())
        pg.emit()
        return nc, es

    modT = [sb("modT%d" % l, [128, 48, 5]) for l in range(DEPTH)]
    A1 = [sb("A1_%d" % l, [128, 8, 5]) for l in range(DEPTH)]
    A2 = [sb("A2_%d" % l, [128, 8, 5]) for l in range(DEPTH)]
    lam = sb("lam", [128, DEPTH, 4])
    gainA = sb("gainA", [128, DEPTH, 128])
    gainB = sb("gainB", [128, DEPTH, 512])
    with ExitStack() as ph:
        scT = sb("scT", [128, 8, 5], st=ph)
        dma("sp", "c0", scT[:], I.cT[:, :, :], (), scT.u)
        act(scT[:], scT[:], AF.Silu, scT.u, scT.u)
        badaT = sb("badaT", [128, DEPTH, 48], st=ph)
        gaT = sb("gaT", [128, DEPTH, 8], st=ph)
        gfT = sb("gfT", [128, DEPTH, 8], st=ph)
        for l in range(DEPTH):
            dma("sp", "c0", badaT[:, l, :], I.b_adaT[l], (), badaT.u)
            dma("sp", "c0", gaT[:, l, :], I.g_attnT[l], (), gaT.u)
            dma("sp", "c0", gfT[:, l, :], I.g_ffnT[l], (), gfT.u)
        wada = [sb("wada%d" % i, [128, 8, 512], st=ph) for i in range(2)]
        gi = 0
        for l in range(DEPTH):
            for g in range(12):
                wt = wada[gi % 2]
                gi += 1
                dma("sp", "wada%d" % (gi % 2), wt[:],
                    I.w_ada[l][:, g * 512:(g + 1) * 512].rearrange("(k p) f -> p k f", p=128), (), wt.u)
                for j in range(4):
                    ch = g * 4 + j
                    for k in range(8):
                        mm(banks[7].t[:, ch * 5:ch * 5 + 5], wt[:, k, j * 128:(j + 1) * 128], scT[:, k, :],
                           k == 0, k == 7, wt.u + scT.u, banks[7].u)
            tt("dve", modT[l][:], banks[7].t[:, 0:240].rearrange("p (c r) -> p c r", r=5),
               badaT[:, l, :].unsqueeze(2).to_broadcast([128, 48, 5]), ALU.add, banks[7].u + badaT.u, modT[l].u)
            stt(A1[l][:], modT[l][:, 8:16, :], 1.0, gaT[:, l, :].unsqueeze(2).to_broadcast([128, 8, 5]), ALU.add,
                ALU.mult, modT[l].u + gaT.u, A1[l].u)
            stt(A2[l][:], modT[l][:, 32:40, :], 1.0, gfT[:, l, :].unsqueeze(2).to_broadcast([128, 8, 5]), ALU.add,
                ALU.mult, modT[l].u + gfT.u, A2[l].u)
        lamsrc = sb("lamsrc", [128, DEPTH, 256], st=ph)
        lamt = sb("lamt", [128, 64], st=ph)
        for l in range(DEPTH):
            dma("sp", "c0", lamsrc[:, l, :], I.lamv[l:l + 1, :].partition_broadcast(128), (), lamsrc.u)
            dma("sp", "c0", gainA[:, l, :], I.a_gain[l:l + 1, :].partition_broadcast(128), (), gainA.u)
            dma("sp", "c0", gainB[:, l, :], I.b_gain[l:l + 1, :].partition_broadcast(128), (), gainB.u)
        for l in range(DEPTH):
            lam_init = 0.8 - 0.6 * math.exp(-0.3 * l)
            for i in range(2):
                tt("dve", lamt[:], lamsrc[:, l, i * 128:i * 128 + 64], lamsrc[:, l, i * 128 + 64:i * 128 + 128],
                   ALU.mult, lamsrc.u, lamt.u)
                V("dve", lambda e, l=l, i=i: e.reduce_sum(lam[:, l, i:i + 1], lamt[:], AX.X), lamt.u, lam.u)
            act(lam[:, l, 0:2], lam[:, l, 0:2], AF.Exp, lam.u, lam.u)
            tt("dve", lam[:, l, 2:3], lam[:, l, 1:2], lam[:, l, 0:1], ALU.subtract, lam.u, lam.u)
            ts("dve", lam[:, l, 2:3], lam[:, l, 2:3], -lam_init, ALU.add, lam.u, lam.u)
            ts("dve", gainA[:, l, :], gainA[:, l, :], 1.0 - lam_init, ALU.mult, gainA.u, gainA.u)
        pg.barrier()

    def segs(c0, n):
        out = []
        if c0 < SEQ:
            out.append((0, min(n, SEQ - c0), 0))
        for s in range(4):
            lo, hi = SEQ + 16 * s, SEQ + 16 * s + 16
            a, b = max(lo, c0), min(hi, c0 + n)
            if a < b:
                out.append((a - c0, b - c0, 1 + s))
        return out

    with ExitStack() as ph:
        xld = [sb("xld%d" % i, [128, D], st=ph) for i in range(2)]
        xtt = [sb("xtt%d" % i, [128, 8, 128], st=ph) for i in range(2)]
        ntile = (T + 127) // 128
        for ti in range(ntile):
            r0 = ti * 128
            m = min(128, T - r0)
            xl = xld[ti % 2]
            xo = xtt[ti % 2]
            dma("sp", "xld%d" % (ti % 2), xl[0:m, :], I.xin[r0:r0 + m, :], (), xl.u)
            for hf in range(2):
                b = banks[(ti * 2 + hf) % 4]
                for kk in range(4):
                    k = hf * 4 + kk
                    tr(b.t[:, kk * 128:kk * 128 + m], xl[0:m, k * 128:(k + 1) * 128], ident_f[0:m, 0:m],
                       xl.u + ident_f.u, b.u)
                cp("act" if hf == 0 else "dve", xo[:, hf * 4:(hf + 1) * 4, 0:m],
                   b.t[:, :].rearrange("p (k t) -> p k t", t=128)[:, :, 0:m], b.u, xo.u)
            dma("sp", "xtt%d" % (ti % 2), xT[:, :, r0:r0 + m].rearrange("k p t -> p k t"), xo[:, :, 0:m], xo.u, ())
        pg.barrier()
    if stage("p0"):
        return finish()

    xb = [sb("xb%d" % i, [128, 8, 512]) for i in range(2)]
    xbi = [0]

    def load_xb(c0, n):
        x = xb[xbi[0] % 2]
        xbi[0] += 1
        x.key = "xb%d" % (xbi[0] % 2)
        dma("sp", x.key, x[:, :, 0:n], xT[:, :, c0:c0 + n].rearrange("k p t -> p k t"), (), x.u)
        return x

    def store_xb(x, c0, n):
        dma("sp", x.key, xT[:, :, c0:c0 + n].rearrange("k p t -> p k t"), x[:, :, 0:n], x.u, ())

    class NB:
        def __init__(self, ph, name):
            self.sqb = sb(name + "sqb", [128, 8, 512], BF16, st=ph)
            self.rstd = sb(name + "rstd", [128, 512], st=ph)
            self.xn = sb(name + "xn", [128, 8, 512], st=ph)
            self.h = sb(name + "h", [128, 8, 512], BF16, st=ph)

    def norm_mod(nb, x, c0, n, Aq, Bq, boff, bank):
        sqb, rstd, xn, dst = nb.sqb, nb.rstd, nb.xn, nb.h
        act(sqb[:, :, 0:n], x[:, :, 0:n], AF.Square, x.u, sqb.u)
        for k in range(8):
            mm(bank.t[:, 0:n], ones_b[:], sqb[:, k, 0:n], k == 0, k == 7, ones_b.u + sqb.u, bank.u)
        act(rstd[:, 0:n], bank.t[:, 0:n], AF.Sqrt, bank.u + epsc.u, rstd.u, bias=epsc[:, 0:1], scale=1.0 / D)
        V("dve", lambda e: e.reciprocal(rstd[:, 0:n], rstd[:, 0:n]), rstd.u, rstd.u)
        tt("dve", xn[:, :, 0:n], x[:, :, 0:n], rstd[:, 0:n].unsqueeze(1).to_broadcast([128, 8, n]), ALU.mult,
           x.u + rstd.u, xn.u)
        for (lo, hi, r) in segs(c0, n):
            for k in range(8):
                act(dst[:, k, lo:hi], xn[:, k, lo:hi], AF.Identity, xn.u + Aq.u + Bq.u, dst.u,
                    bias=Bq[:, boff + k, r:r + 1], scale=Aq[:, k, r:r + 1])

    pblocks = [(c, 512) for c in range(0, SEQ, 512)]
    if NBLK:
        pblocks = pblocks[:NBLK] + pblocks[-1:]
    blocks = pblocks + [(SEQ, NS)]

    def p1(l):
        with ExitStack() as ph:
            nb = NB(ph, "p1")
            hT = nb.h
            wbig = sb("wbig", [128, 8, 3072], BF16, nunits=24, st=ph)
            qkf = [sb("qkf%d" % i, [128, 512], st=ph) for i in range(2)]
            qkb = [sb("qkb%d" % i, [128, 512], BF16, st=ph) for i in range(2)]
            vf = [sb("vf%d" % i, [128, 512], st=ph) for i in range(3)]
            vab = [sb("vab%d" % i, [128, 4, 136], BF16, st=ph) for i in range(2)]
            vbb = [sb("vbb%d" % i, [128, 8, 72], BF16, st=ph) for i in range(2)]
            for i in range(2):
                memset("pool", vab[i][:, :, 128:136], 1.0, vab[i].u)
                memset("pool", vbb[i][:, :, 64:72], 1.0, vbb[i].u)
            qkT = [sb("qkT%d" % i, [128, 4, 128], BF16, st=ph) for i in range(2)]
            qkbT = [sb("qkbT%d" % i, [128, 512], BF16, st=ph) for i in range(2)]
            rc = [sb("rc%d" % i, [128, 2, 8], st=ph) for i in range(2)]
            rtmp = sb("rtmp", [128, 4, 8, 8], st=ph)
            for k in range(8):
                for c3 in range(3):
                    dma("pool", "wbig", wbig[:, k, c3 * 1024:(c3 + 1) * 1024],
                        I.w_in[l][k * 128:(k + 1) * 128, c3 * 1024:(c3 + 1) * 1024], (), [wbig.u[k * 3 + c3]])
            cnt = 0
            for (c0, n) in blocks:
                x = load_xb(c0, n)
                norm_mod(nb, x, c0, n, A1[l], modT[l], 0, banks[7])
                outblk = (c0 == SEQ - 512) or (c0 == SEQ)
                for which, col0, scr, scl in ((0, 1536, QTB, 0.125), (1, 2048, KTB, 1.0)):
                    for cc in range(4):
                        b = banks[cnt % 2]
                        o = qkbT[cnt % 2]
                        cnt += 1
                        for k in range(8):
                            mm(b.t[:, 0:n], wbig[:, k, col0 + cc * 128:col0 + (cc + 1) * 128], hT[:, k, 0:n],
                               k == 0, k == 7, wbig.u + hT.u, b.u)
                        act(o[:, 0:n], b.t[:, 0:n], AF.Copy, b.u, o.u, scale=scl)
                        dma("sp", "qkbT%d" % ((cnt - 1) % 2), scr[cc, :, c0:c0 + n], o[:, 0:n], o.u, ())
                for st in range((n + 127) // 128):
                    m = min(128, n - st * 128)
                    r0 = c0 + st * 128
                    rcs = rc[st % 2]
                    dma("sp", "rc%d" % (st % 2), rcs[0:m, 0, :], I.ropec[r0:r0 + m, :], (), rcs.u)
                    dma("sp", "rc%d" % (st % 2), rcs[0:m, 1, :], I.ropes[r0:r0 + m, :], (), rcs.u)
                    for which, col0, scr, scl in ((0, 0, QTA, 0.125), (1, 512, KTA, 1.0)):
                        b = banks[2 + which]
                        f = qkf[which]
                        bb = qkb[which]
                        for k in range(8):
                            mm(b.t[0:m, :], hT[:, k, st * 128:st * 128 + m], wbig[:, k, col0:col0 + 512],
                               k == 0, k == 7, wbig.u + hT.u, b.u)
                        act(f[0:m, :], b.t[0:m, :], AF.Copy, b.u, f.u, scale=scl)
                        fv = f[0:m, :].rearrange("p (g e) -> p g e", e=64)
                        cosb = rcs[0:m, 0, :].unsqueeze(1).to_broadcast([m, 8, 8])
                        sinb = rcs[0:m, 1, :].unsqueeze(1).to_broadcast([m, 8, 8])
                        x1 = fv[:, :, 0:8]
                        x2 = fv[:, :, 8:16]
                        ru = rtmp.u + f.u + rcs.u
                        tt("pool", rtmp[0:m, 0], x1, cosb, ALU.mult, f.u + rcs.u, rtmp.u)
                        tt("pool", rtmp[0:m, 1], x2, sinb, ALU.mult, f.u + rcs.u, rtmp.u)
                        tt("pool", rtmp[0:m, 2], x2, cosb, ALU.mult, f.u + rcs.u, rtmp.u)
                        tt("pool", rtmp[0:m, 3], x1, sinb, ALU.mult, f.u + rcs.u, rtmp.u)
                        tt("pool", x1, rtmp[0:m, 0], rtmp[0:m, 1], ALU.subtract, ru, f.u)
                        tt("pool", x2, rtmp[0:m, 2], rtmp[0:m, 3], ALU.add, ru, f.u)
                        if which == 1:
                            dma("sp", "qkf1", nak[l, r0:r0 + m, :], f[0:m, :], f.u, ())
                        cp("dve", bb[0:m, :], f[0:m, :], f.u, bb.u)
                        tb = banks[4 + which]
                        o = qkT[which]
                        for h in range(4):
                            tr(tb.t[:, :].bitcast(BF16)[:, h * 128:h * 128 + m], bb[0:m, h * 128:(h + 1) * 128],
                               ident_b[0:m, 0:m], bb.u + ident_b.u, tb.u)
                        cp("dve", o[:, :, 0:m],
                           tb.t[:, :].bitcast(BF16)[:, 0:512].rearrange("p (h t) -> p h t", t=128)[:, :, 0:m], tb.u, o.u)
                        dma("sp", "qkT%d" % which, scr[:, :, r0:r0 + m].rearrange("h p t -> p h t"), o[:, :, 0:m], o.u, ())
                    b = banks[6]
                    for k in range(8):
                        mm(b.t[0:m, :], hT[:, k, st * 128:st * 128 + m], wbig[:, k, 1024:1536], k == 0, k == 7,
                           wbig.u + hT.u, b.u)
                    f = vf[0]
                    act(f[0:m, :], b.t[0:m, :], AF.Copy, b.u, f.u)
                    dma("sp", "vf0", nav[l, r0:r0 + m, :], f[0:m, :], f.u, ())
                    vb_ = vab[st % 2]
                    cp("dve", vb_[0:m, :, 0:128], b.t[0:m, :].rearrange("p (h e) -> p h e", e=128), b.u, vb_.u)
                    dma("sp", "vab%d" % (st % 2), VA[:, r0:r0 + m, :].rearrange("h t e -> t h e"), vb_[0:m, :, :], vb_.u, ())
                    b = banks[7]
                    for k in range(8):
                        mm(b.t[0:m, :], hT[:, k, st * 128:st * 128 + m], wbig[:, k, 2560:3072], k == 0, k == 7,
                           wbig.u + hT.u, b.u)
                    vb_ = vbb[st % 2]
                    cp("dve", vb_[0:m, :, 0:64], b.t[0:m, :].rearrange("p (h e) -> p h e", e=64), b.u, vb_.u)
                    dma("sp", "vbb%d" % (st % 2), VB[r0:r0 + m, :], vb_[0:m, :, :].rearrange("p h e -> p (h e)"), vb_.u, ())
                    if outblk:
                        f = vf[1]
                        act(f[0:m, :], b.t[0:m, :], AF.Copy, b.u, f.u)
                        if c0 == SEQ:
                            for s in range(4):
                                dma("sp", "vf1", nbv_s[l, s, 496:512, :], f[16 * s:16 * s + 16, :], f.u, ())
                        else:
                            dma("sp", "vf1", nbv_p[l, st * 128:st * 128 + m, :], f[0:m, :], f.u, ())
                        b = banks[6]
                        for k in range(8):
                            mm(b.t[0:m, :], hT[:, k, st * 128:st * 128 + m], wbig[:, k, 2048:2560], k == 0, k == 7,
                               wbig.u + hT.u, b.u)
                        f = vf[2]
                        act(f[0:m, :], b.t[0:m, :], AF.Copy, b.u, f.u)
                        if c0 == SEQ:
                            for s in range(4):
                                dma("sp", "vf2", nbk_s[l, s, 496:512, :], f[16 * s:16 * s + 16, :], f.u, ())
                        else:
                            dma("sp", "vf2", nbk_p[l, st * 128:st * 128 + m, :], f[0:m, :], f.u, ())
            for s in range(4):
                dma("sp", "roll", nbk_s[l, s, 0:496, :], I.cbk[l, s, 16:512, :], (), ())
                dma("sp", "roll", nbv_s[l, s, 0:496, :], I.cbv[l, s, 16:512, :], (), ())
            pg.barrier()

    def mmg(out, lhsT, rhs, start, stop, reads, writes):
        pg.add("pe", lambda e: e.matmul(out, lhsT, rhs, start=start, stop=stop, skip_group_check=True), reads, writes)

    def p2(l):
        P2F = int(os.environ.get('MK_P2F', '15'))
        with ExitStack() as ph:
            wout = sb("wout", [128, 8, 1024], BF16, nunits=8, st=ph)
            for k in range(8):
                dma("pool", "wout", wout[:, k, :], I.w_out[l][k * 128:(k + 1) * 128, :], (), [wout.u[k]])
            bias_b = sb("bias_b", [128, 8, 2, 128], BF16, nunits=16, st=ph)
            for hb in range(8):
                for j2 in range(2):
                    dma("pool", "biasb", bias_b[:, hb, j2, :], I.biasT[l, hb, j2], (), [bias_b.u[hb * 2 + j2]])
            cbrow = sb("cbrow", [128, 8, 128], BF16, st=ph)
            memset("pool", cbrow[:], 0.0, cbrow.u)
            dma("pool", "biasb", cbrow[0:1, :, :], I.bconst[l:l + 1, :, :], (), cbrow.u)
            qta = [sb("qta%d" % i, [128, 4, 512], BF16, st=ph) for i in range(2)]
            qtb = [sb("qtb%d" % i, [128, 4, 512], BF16, st=ph) for i in range(2)]
            kta = sb("kta", [128, SEQ], BF16, nunits=8, st=ph)
            vat = sb("vat", [128, 64, 136], BF16, nunits=8, st=ph)
            ktb = [sb("ktb%d" % i, [128, 4, 1024], BF16, st=ph) for i in range(2)]
            vbt = [sb("vbt%d" % i, [128, 8, 576], BF16, st=ph) for i in range(2)]
            pT = [sb("pT%d" % i, [128, 512], BF16, st=ph) for i in range(6)]
            oblk = sb("oblk", [128, 4, 1024], BF16, st=ph)
            oT = sb("oT", [128, 8, 512], BF16, st=ph)
            rz = sb("rz", [128, 8], st=ph)
            t1 = sb("t1", [128, 128], st=ph)
            oa = sb("oa", [128, 512], st=ph)
            junk = sb("junk", [128, 512], st=ph)
            ss = sb("ss", [128, 1], st=ph)

            def accA(m_, rr):
                idx = m_ * 4 + rr
                bk = banks[4 + idx // 3]
                c = (idx % 3) * 129
                return bk, bk.t[:, c:c + 129], (idx % 3 == 0)

            def epiA(h, rr, np_, dst):
                b0, a0, _ = accA(0, rr)
                b1, a1, _ = accA(1, rr)
                V("dve", lambda e: e.reciprocal(rz[0:np_, 0:1], a0[0:np_, 128:129]), b0.u, rz.u)
                V("dve", lambda e: e.reciprocal(rz[0:np_, 1:2], a1[0:np_, 128:129]), b1.u, rz.u)
                ts("dve", rz[0:np_, 1:2], rz[0:np_, 1:2], lam[0:np_, l, 2:3], ALU.mult, rz.u + lam.u, rz.u)
                ts("dve", t1[0:np_, :], a1[0:np_, 0:128], rz[0:np_, 1:2], ALU.mult, b1.u + rz.u, t1.u)
                stt(oa[0:np_, 0:128], a0[0:np_, 0:128], rz[0:np_, 0:1], t1[0:np_, :], ALU.mult, ALU.add,
                    b0.u + rz.u + t1.u, oa.u)
                act(junk[0:np_, 0:128], oa[0:np_, 0:128], AF.Square, oa.u, junk.u + ss.u, accum=ss[0:np_, 0:1])
                act(ss[0:np_, :], ss[0:np_, :], AF.Sqrt, ss.u + epsc.u, ss.u, bias=epsc[0:np_, 0:1], scale=1.0 / 128)
                V("dve", lambda e: e.reciprocal(ss[0:np_, :], ss[0:np_, :]), ss.u, ss.u)
                stt(dst, oa[0:np_, 0:128], ss[0:np_, 0:1], gainA[0:np_, l, :], ALU.mult, ALU.mult,
                    oa.u + ss.u + gainA.u, oblk.u)

            def epiB(np_, dst, w):
                for hf in range(2):
                    bk = banks[4 + hf]
                    v = bk.t[0:np_, 0:4 * w].rearrange("p (h e) -> p h e", e=w)
                    V("dve", lambda e, v=v, hf=hf: e.reciprocal(rz[0:np_, hf * 4:hf * 4 + 4], v[:, :, 64]), bk.u, rz.u)
                    tt("dve", oa[0:np_, hf * 256:(hf + 1) * 256].rearrange("p (h e) -> p h e", e=64), v[:, :, 0:64],
                       rz[0:np_, hf * 4:hf * 4 + 4].unsqueeze(2).to_broadcast([np_, 4, 64]), ALU.mult,
                       bk.u + rz.u, oa.u)
                act(junk[0:np_, :], oa[0:np_, :], AF.Square, oa.u, junk.u + ss.u, accum=ss[0:np_, 0:1])
                act(ss[0:np_, :], ss[0:np_, :], AF.Sqrt, ss.u + epsc.u, ss.u, bias=epsc[0:np_, 0:1], scale=1.0 / 512)
                V("dve", lambda e: e.reciprocal(ss[0:np_, :], ss[0:np_, :]), ss.u, ss.u)
                stt(dst, oa[0:np_, :], ss[0:np_, 0:1], gainB[0:np_, l, :], ALU.mult, ALU.mult,
                    oa.u + ss.u + gainB.u, oblk.u)

            def outproj(x, c0, n):
                for dc in range(8):
                    yb = banks[2 + dc % 2]
                    for kc in range(8):
                        mm(yb.t[:, 0:n], wout[:, kc, dc * 128:(dc + 1) * 128], oT[:, kc, 0:n], kc == 0, kc == 7,
                           wout.u + oT.u, yb.u)
                    for (lo, hi, r) in segs(c0, n):
                        stt(x[:, dc, lo:hi], yb.t[:, lo:hi], modT[l][:, 16 + dc, r:r + 1], x[:, dc, lo:hi],
                            ALU.mult, ALU.add, yb.u + modT[l].u + x.u, x.u)
                store_xb(x, c0, n)

            pcnt = 0
            for bi, (c0, n) in enumerate(pblocks):
                j = c0 // 512
                x = load_xb(c0, n)
                qa = qta[bi % 2]
                qb = qtb[bi % 2]
                dma("sp", "qta%d" % (bi % 2), qa[:], QTA[:, :, c0:c0 + 512].rearrange("h p t -> p h t"), (), qa.u)
                dma("sp", "qtb%d" % (bi % 2), qb[:], QTB[:, :, c0:c0 + 512].rearrange("h p t -> p h t"), (), qb.u)
                nkt = 4 * (j + 1)
                for h in range(4 if P2F & 1 else 0):
                    for u8 in range((nkt * 128 + 1023) // 1024):
                        lo = u8 * 1024
                        hi = min(lo + 1024, nkt * 128)
                        dma("sp", "kta%d" % u8, kta[:, lo:hi], KTA[h, :, lo:hi], (), [kta.u[u8]])
                        dma("sp", "vat%d" % u8, vat[:, lo // 128:hi // 128, :],
                            VA[h, lo:hi, :].rearrange("(kt p) e -> p kt e", p=128), (), [vat.u[u8]])
                    def emit_pv(kt, pts_):
                        r = kt - 4 * j
                        u8 = kt // 8
                        for m_ in range(2):
                            pt = pts_[m_]
                            for rr in range(max(r, 0), 4):
                                bk, ap_, first = accA(m_, rr)
                                mmg(ap_, pt[:, rr * 128:(rr + 1) * 128], vat[:, kt, 0:129],
                                    (kt == 0 and first), kt == 4 * j + rr, pt.u + [vat.u[u8]], bk.u)

                    pend = None
                    for kt in range(nkt):
                        r = kt - 4 * j
                        cs = max(r, 0) * 128
                        u8 = kt // 8
                        cur = []
                        for m_ in range(2):
                            Sb = banks[pcnt % 4]
                            pt = pT[pcnt % 6]
                            pcnt += 1
                            mm(Sb.t[:, cs:512], kta[64 * m_:64 * m_ + 64, kt * 128:(kt + 1) * 128],
                               qa[64 * m_:64 * m_ + 64, h, cs:512], True, r < 0, [kta.u[u8]] + qa.u, Sb.u)
                            if r >= 0:
                                mm(Sb.t[:, cs:cs + 128], mk[:, 0, :], mk[:, 1, :], False, True, mk.u, Sb.u)
                            act(pt[:, cs:512], Sb.t[:, cs:512], AF.Exp, Sb.u, pt.u)
                            cur.append(pt)
                        if pend is not None:
                            emit_pv(*pend)
                        pend = (kt, cur)
                    emit_pv(*pend)
                    for rr in range(4):
                        epiA(h, rr, 128, oblk[:, rr, h * 128:(h + 1) * 128])
                lo_t = max(0, 4 * j - 4)
                hi_t = 4 * j + 4
                kb_ = ktb[bi % 2]
                vb_ = vbt[bi % 2]
                nkl = (hi_t - lo_t) * 128
                dma("sp", "ktb%d" % (bi % 2), kb_[:, :, 0:nkl],
                    KTB[:, :, lo_t * 128:hi_t * 128].rearrange("c p t -> p c t"), (), kb_.u)
                dma("sp", "vbt%d" % (bi % 2), vb_[:, 0:hi_t - lo_t, :],
                    VB[lo_t * 128:hi_t * 128, :].rearrange("(kt p) e -> p kt e", p=128), (), vb_.u)
                for rr in range(4 if P2F & 2 else 0):
                    i_ = 4 * j + rr
                    jjs = [jj for jj in range(5) if i_ - 4 + jj >= 0]

                    def emit_pvb(jj, hf, pt, ktl):
                        for hh in range(4):
                            hb = hf * 4 + hh
                            bk = banks[4 + hf]
                            mmg(bk.t[:, hh * 65:hh * 65 + 65], pt[:, hh * 128:(hh + 1) * 128],
                                vb_[:, ktl, hb * 72:hb * 72 + 65], (jj == jjs[0] and hh == 0), jj == 4,
                                pt.u + vb_.u, bk.u)

                    pendb = None
                    for jj in jjs:
                        ktl = i_ - 4 + jj - lo_t
                        for hf in range(2):
                            Sb = banks[pcnt % 4]
                            pt = pT[pcnt % 6]
                            pcnt += 1
                            for hh in range(4):
                                hb = hf * 4 + hh
                                cc = hb // 2
                                prt = (hb % 2) * 64
                                cols = Sb.t[:, hh * 128:(hh + 1) * 128]
                                mm(cols, kb_[prt:prt + 64, cc, ktl * 128:(ktl + 1) * 128],
                                   qb[prt:prt + 64, cc, rr * 128:(rr + 1) * 128], True, False, kb_.u + qb.u, Sb.u)
                                if jj <= 2:
                                    mm(cols, mk[:, 4, :], cbrow[:, hb, :], False, jj != 0, mk.u + cbrow.u, Sb.u)
                                    if jj == 0:
                                        mm(cols, mk[:, 2, :], mk[:, 3, :], False, True, mk.u, Sb.u)
                                elif jj == 3:
                                    mm(cols, ident_b[:], bias_b[:, hb, 0, :], False, True, ident_b.u + bias_b.u, Sb.u)
                                else:
                                    mm(cols, ident_b[:], bias_b[:, hb, 1, :], False, False, ident_b.u + bias_b.u, Sb.u)
                                    mm(cols, mk[:, 0, :], mk[:, 1, :], False, True, mk.u, Sb.u)
                            act(pt[:, :], Sb.t[:, :], AF.Exp, Sb.u, pt.u)
                            if pendb is not None:
                                emit_pvb(*pendb)
                            pendb = (jj, hf, pt, ktl)
                    emit_pvb(*pendb)
                    epiB(128, oblk[:, rr, 512:1024], 65)
                if not P2F & 4:
                    continue
                for rr in range(4):
                    tb = banks[rr % 2]
                    tbv = tb.t[:, :].bitcast(BF16)
                    for kc in range(8):
                        tr(tbv[:, kc * 128:(kc + 1) * 128], oblk[:, rr, kc * 128:(kc + 1) * 128], ident_b[:],
                           oblk.u + ident_b.u, tb.u)
                    cp("act" if rr % 2 == 0 else "dve", oT[:, :, rr * 128:(rr + 1) * 128],
                       tbv.rearrange("p (k t) -> p k t", t=128), tb.u, oT.u)
                outproj(x, c0, 512)

            x = load_xb(SEQ, NS)
            qas = sb("qas", [128, 4, NS], BF16, st=ph)
            qbs = sb("qbs", [128, 4, NS], BF16, st=ph)
            kan = sb("kan", [128, 4, NS], BF16, st=ph)
            kbn = sb("kbn", [128, 4, NS], BF16, st=ph)
            van = [sb("van%d" % s_, [16, 4, 136], BF16, st=ph) for s_ in range(4)]
            vbn = [sb("vbn%d" % s_, [16, 576], BF16, st=ph) for s_ in range(4)]
            dma("sp", "smp", qas[:], QTA[:, :, SEQ:T].rearrange("h p t -> p h t"), (), qas.u)
            dma("sp", "smp", qbs[:], QTB[:, :, SEQ:T].rearrange("h p t -> p h t"), (), qbs.u)
            dma("sp", "smp", kan[:], KTA[:, :, SEQ:T].rearrange("h p t -> p h t"), (), kan.u)
            dma("sp", "smp", kbn[:], KTB[:, :, SEQ:T].rearrange("h p t -> p h t"), (), kbn.u)
            for s_ in range(4):
                dma("sp", "smp", van[s_][:], VA[:, SEQ + 16 * s_:SEQ + 16 * s_ + 16, :].rearrange("h t e -> t h e"),
                    (), van[s_].u)
                dma("sp", "smp", vbn[s_][:], VB[SEQ + 16 * s_:SEQ + 16 * s_ + 16, :], (), vbn[s_].u)
            kcb = [sb("kcb%d" % i, [128, 512], BF16, st=ph) for i in range(2)]
            vca = [sb("vca%d" % i, [128, 4, 136], BF16, st=ph) for i in range(2)]
            vcb = [sb("vcb%d" % i, [128, 8, 72], BF16, st=ph) for i in range(2)]
            for i in range(2):
                memset("pool", vca[i][:, :, 128:136], 1.0, vca[i].u)
                memset("pool", vcb[i][:, :, 64:72], 1.0, vcb[i].u)
            kTs = [sb("kTs%d" % i, [128, 4, 128], BF16, st=ph) for i in range(2)]
            pts = [sb("pts%d" % i, [128, 128], BF16, st=ph) for i in range(2)]
            os_ = sb("os_", [16, 1024], BF16, st=ph)
            scnt = 0
            for s_ in range(4 if P2F & 8 else 0):
                q0 = 16 * s_
                SF = int(os.environ.get('MK_SF', '31'))
                for kt in ([int(v) for v in os.environ['MK_SK'].split(',')] if os.environ.get('MK_SK') else range(33 if SF & 1 else 0)):
                    kk = 128 if kt < 32 else 16
                    if kt < 32:
                        kc = kcb[scnt % 2]
                        vc = vca[scnt % 2]
                        kT = kTs[scnt % 2]
                        dma("pool", "kcb%d" % (scnt % 2), kc[:], I.cak[l, s_, kt * 128:(kt + 1) * 128, :], (), kc.u)
                        dma("pool", "vca%d" % (scnt % 2), vc[:, :, 0:128],
                            I.cav[l, s_, kt * 128:(kt + 1) * 128, :].rearrange("t (h e) -> t h e", e=128), (), vc.u)
                        tb = banks[0]
                        tbv = tb.t[:, :].bitcast(BF16)
                        for h in range(4):
                            tr(tbv[:, h * 128:(h + 1) * 128], kc[:, h * 128:(h + 1) * 128], ident_b[:],
                               kc.u + ident_b.u, tb.u)
                        cp("dve", kT[:], tbv[:, 0:512].rearrange("p (h t) -> p h t", t=128), tb.u, kT.u)
                        kTv = lambda h, m_, kT=kT: kT[64 * m_:64 * m_ + 64, h, :]
                        kTu = kT.u
                        vv = lambda h, vc=vc: vc[:, h, 0:129]
                        vu = vc.u
                    else:
                        if os.environ.get('MK_SWAP'):
                            kTv = lambda h, m_: kbn[64 * m_:64 * m_ + 64, h, q0:q0 + 16]
                        else:
                            kTv = lambda h, m_: kan[64 * m_:64 * m_ + 64, h, q0:q0 + 16]
                        kTu = kan.u + kbn.u
                        vv = lambda h: van[s_][0:16, h, 0:129]
                        vu = van[s_].u
                    Sbs = [(banks[1], banks[2]), (banks[3], banks[7])][scnt % 2]
                    pt = pts[scnt % 2]
                    scnt += 1
                    SX = int(os.environ.get('MK_SX', '7'))
                    VV_ = os.environ.get('MK_V', '')
                    for m_ in range((1 if VV_ == 'm0' else 2) if SX & 1 else 0):
                        for h in range(4):
                            g_ = h * 2 + m_
                            qq = qbs if os.environ.get('MK_SWAP') else qas
                            mm(Sbs[m_].t[0:kk, h * 16:h * 16 + 16], kTv(h, m_), qq[64 * m_:64 * m_ + 64, h, q0:q0 + 16],
                               True, True, kTu + qas.u + qbs.u, Sbs[m_].u)
                    for m_ in range(2):
                        act(pt[0:kk, m_ * 64:(m_ + 1) * 64], Sbs[m_].t[0:kk, 0:64], AF.Exp, Sbs[m_].u, pt.u)
                    for m_ in range(2 if SX & 4 else 0):
                        for h in range(4):
                            g_ = m_ * 4 + h
                            bk, ap_, first = accA(m_, h)
                            mmg(ap_[0:16, :], pt[0:kk, g_ * 16:g_ * 16 + 16], vv(h), (kt == 0 and first), kt == 32,
                                pt.u + vu, bk.u)
                for h in range(4 if SF & 2 else 0):
                    epiA(h, h, 16, os_[0:16, h * 128:(h + 1) * 128])
                for kt in range(5 if SF & 4 else 0):
                    kk = 128 if kt < 4 else 16
                    if kt < 4:
                        kc = kcb[scnt % 2]
                        vc = vcb[scnt % 2]
                        kT = kTs[scnt % 2]
                        dma("pool", "kcb%d" % (scnt % 2), kc[:], I.cbk[l, s_, kt * 128:(kt + 1) * 128, :], (), kc.u)
                        dma("pool", "vcb%d" % (scnt % 2), vc[:, :, 0:64],
                            I.cbv[l, s_, kt * 128:(kt + 1) * 128, :].rearrange("t (h e) -> t h e", e=64), (), vc.u)
                        tb = banks[scnt % 2]
                        tbv = tb.t[:, :].bitcast(BF16)
                        for cc in range(4):
                            tr(tbv[:, cc * 128:(cc + 1) * 128], kc[:, cc * 128:(cc + 1) * 128], ident_b[:],
                               kc.u + ident_b.u, tb.u)
                        cp("dve", kT[:], tbv[:, 0:512].rearrange("p (h t) -> p h t", t=128), tb.u, kT.u)
                        kTv = lambda cc, prt, kT=kT: kT[prt:prt + 64, cc, :]
                        kTu = kT.u
                        vv = lambda hb, vc=vc: vc[:, hb, 0:65]
                        vu = vc.u
                    else:
                        kTv = lambda cc, prt: kbn[prt:prt + 64, cc, q0:q0 + 16]
                        kTu = kbn.u
                        vv = lambda hb: vbn[s_][0:16, hb * 72:hb * 72 + 65]
                        vu = vbn[s_].u
                    Sb = banks[2 + scnt % 2]
                    pt = pts[scnt % 2]
                    scnt += 1
                    for hb in range(8):
                        cc = hb // 2
                        prt = (hb % 2) * 64
                        cols = Sb.t[0:kk, hb * 16:hb * 16 + 16]
                        mm(cols, kTv(cc, prt), qbs[prt:prt + 64, cc, q0:q0 + 16], True, False, kTu + qbs.u, Sb.u)
                        if kt <= 2:
                            mm(cols, mk[:, 4, 0:kk], cbrow[:, hb, 0:16], False, True, mk.u + cbrow.u, Sb.u)
                        elif kt == 3:
                            mm(cols, ident_b[:], bias_b[:, hb, 0, 0:16], False, True, ident_b.u + bias_b.u, Sb.u)
                        else:
                            mm(cols, ident_b[:, 0:16], bias_b[:, hb, 1, 0:16], False, True,
                               ident_b.u + bias_b.u, Sb.u)
                    act(pt[0:kk, :], Sb.t[0:kk, 0:128], AF.Exp, Sb.u, pt.u)
                    for hb in range(8):
                        bk = banks[4 + hb // 4]
                        hh = hb % 4
                        mmg(bk.t[0:16, hh * 65:hh * 65 + 65], pt[0:kk, hb * 16:hb * 16 + 16], vv(hb),
                            (kt == 0 and hh == 0), kt == 4, pt.u + vu, bk.u)
                if SF & 8:
                    epiB(16, os_[0:16, 512:1024], 65)
                if not SF & 16:
                    continue
                tb = banks[scnt % 2]
                tbv = tb.t[:, :].bitcast(BF16)
                for kc_ in range(8):
                    tr(tbv[:, kc_ * 16:kc_ * 16 + 16], os_[0:16, kc_ * 128:(kc_ + 1) * 128], ident_b[0:16, 0:16],
                       oblk.u + ident_b.u, tb.u)
                cp("dve", oT[:, :, q0:q0 + 16], tbv[:, 0:128].rearrange("p (k t) -> p k t", t=16), tb.u, oT.u)
            if P2F & 8:
                outproj(x, SEQ, NS)
            pg.barrier()

    def pu(l):
        with ExitStack() as ph:
            ubuf = [sb("ubuf%d" % i, [128, D], BF16, st=ph) for i in range(2)]
            vbuf = [sb("vbuf%d" % i, [128, D], BF16, st=ph) for i in range(2)]
            utb = [sb("utb%d" % i, [128, 8, 128], BF16, st=ph) for i in range(2)]
            for ec in range(128):
                ub = ubuf[ec % 2]
                vb_ = vbuf[ec % 2]
                ut = utb[ec % 2]
                dma("pool", "ubuf%d" % (ec % 2), ub[:], I.pk_u[l, ec * 128:(ec + 1) * 128, :], (), ub.u)
                dma("pool", "vbuf%d" % (ec % 2), vb_[:], I.pk_v[l, ec * 128:(ec + 1) * 128, :], (), vb_.u)
                tb = banks[ec % 2]
                tbv = tb.t[:, :].bitcast(BF16)
                for k in range(8):
                    tr(tbv[:, k * 128:(k + 1) * 128], ub[:, k * 128:(k + 1) * 128], ident_b[:], ub.u + ident_b.u, tb.u)
                cp("act" if ec % 2 == 0 else "dve", ut[:], tbv.rearrange("p (k e) -> p k e", e=128), tb.u, ut.u)
                dma("sp", "utb%d" % (ec % 2), UT[:, :, ec * 128:(ec + 1) * 128].rearrange("k p e -> p k e"), ut[:], ut.u, ())
                dma("sp", "vbufo%d" % (ec % 2), VBF[ec * 128:(ec + 1) * 128, :], vb_[:], vb_.u, ())
            pg.barrier()

    def p3(l):
        with ExitStack() as ph:
            nb = NB(ph, "p3")
            h2 = nb.h
            wq = sb("wq", [128, 8, 2048], BF16, nunits=16, st=ph)
            for k in range(8):
                for c2 in range(2):
                    dma("pool", "wq", wq[:, k, c2 * 1024:(c2 + 1) * 1024],
                        I.pk_wq[l][k * 128:(k + 1) * 128, c2 * 1024:(c2 + 1) * 1024], (), [wq.u[k * 2 + c2]])
            kraw = sb("kraw", [128, 16, 128], BF16, st=ph)
            keysT = sb("keysT", [128, 16, 128], BF16, st=ph)
            for g in range(16):
                dma("pool", "kraw", kraw[:, g, :], I.pk_keys[l, g], (), kraw.u)
            for g in range(16):
                tb = banks[g % 2]
                tbv = tb.t[:, :].bitcast(BF16)
                tr(tbv[:, 0:128], kraw[:, g, :], ident_b[:], kraw.u + ident_b.u, tb.u)
                cp("dve", keysT[:, g, :], tbv[:, 0:128], tb.u, keysT.u)
            qT = sb("qT", [128, 16, 512], BF16, st=ph)
            sc = sb("sc", [128, 16, 128], st=ph)
            sc2 = sb("sc2", [128, 16, 128], st=ph)
            sv = sb("sv", [128, 16, 16], st=ph)
            si = sb("si", [128, 16, 16], U32, st=ph)
            sif = sb("sif", [128, 16, 16], st=ph)
            cand = sb("cand", [128, 8, 256], st=ph)
            cand2 = sb("cand2", [128, 8, 256], st=ph)
            cv = sb("cv", [128, 8, 16], st=ph)
            ci = sb("ci", [128, 8, 16], U32, st=ph)
            cab = sb("cab", [128, 2, 8, 16], U32, st=ph)
            cabf = sb("cabf", [128, 2, 8, 16], st=ph)
            eq = sb("eq", [128, 8, 16, 16], st=ph)
            res = sb("res", [128, 3, 128], st=ph)
            gs = sb("gs", [128, 8], st=ph)
            rts = [sb("rts%d" % i, [128, 3, 128], st=ph) for i in range(2)]
            bcnt = 0
            for (c0, n) in blocks:
                x = load_xb(c0, n)
                norm_mod(nb, x, c0, n, A2[l], modT[l], 24, banks[7])
                dma("sp", "h2st", H2T[:, :, c0:c0 + n].rearrange("k p t -> p k t"), h2[:, :, 0:n], h2.u, ())
                for qc in range(16):
                    b = banks[qc % 2]
                    for k in range(8):
                        mm(b.t[:, 0:n], wq[:, k, qc * 128:(qc + 1) * 128], h2[:, k, 0:n], k == 0, k == 7,
                           wq.u + h2.u, b.u)
                    cp("act" if qc % 2 == 0 else "dve", qT[:, qc, 0:n], b.t[:, 0:n], b.u, qT.u)
                for st in range((n + 127) // 128):
                    m = min(128, n - st * 128)
                    r0 = c0 + st * 128
                    for qc in range(16):
                        b = banks[2 + qc // 4]
                        mm(b.t[0:m, (qc % 4) * 128:(qc % 4 + 1) * 128], qT[:, qc, st * 128:st * 128 + m],
                           keysT[:, qc, :], True, True, qT.u + keysT.u, b.u)
                    for q4 in range(4):
                        b = banks[2 + q4]
                        cp("act", sc[0:m, q4 * 4:(q4 + 1) * 4, :], b.t[0:m, :].rearrange("p (g n) -> p g n", n=128),
                           b.u, sc.u)
                    for g in range(16):
                        vmax(sv[0:m, g, 0:8], sc[0:m, g, :], sc.u, sv.u)
                    for g in range(16):
                        vmaxidx(si[0:m, g, 0:8], sv[0:m, g, 0:8], sc[0:m, g, :], sc.u + sv.u, si.u)
                    for g in range(16):
                        vmatch(sc2[0:m, g, :], sv[0:m, g, 0:8], sc[0:m, g, :], sc.u + sv.u, sc2.u)
                    for g in range(16):
                        vmax(sv[0:m, g, 8:16], sc2[0:m, g, :], sc2.u, sv.u)
                    for g in range(16):
                        vmaxidx(si[0:m, g, 8:16], sv[0:m, g, 8:16], sc2[0:m, g, :], sc2.u + sv.u, si.u)
                    svv = sv[0:m].rearrange("p (h two) a -> p h two a", two=2)
                    tt("dve", cand[0:m].rearrange("p h (a b) -> p h a b", b=16),
                       svv[:, :, 0, :].unsqueeze(3).to_broadcast([m, 8, 16, 16]),
                       svv[:, :, 1, :].unsqueeze(2).to_broadcast([m, 8, 16, 16]), ALU.add, sv.u, cand.u)
                    for h in range(8):
                        vmax(cv[0:m, h, 0:8], cand[0:m, h, :], cand.u, cv.u)
                    for h in range(8):
                        vmaxidx(ci[0:m, h, 0:8], cv[0:m, h, 0:8], cand[0:m, h, :], cand.u + cv.u, ci.u)
                    for h in range(8):
                        vmatch(cand2[0:m, h, :], cv[0:m, h, 0:8], cand[0:m, h, :], cand.u + cv.u, cand2.u)
                    for h in range(8):
                        vmax(cv[0:m, h, 8:16], cand2[0:m, h, :], cand2.u, cv.u)
                    for h in range(8):
                        vmaxidx(ci[0:m, h, 8:16], cv[0:m, h, 8:16], cand2[0:m, h, :], cand2.u + cv.u, ci.u)
                    ts("dve", cab[0:m, 0], ci[0:m], 4, ALU.logical_shift_right, ci.u, cab.u)
                    ts("dve", cab[0:m, 1], ci[0:m], 15, ALU.bitwise_and, ci.u, cab.u)
                    cp("dve", cabf[0:m], cab[0:m], cab.u, cabf.u)
                    cp("dve", sif[0:m], si[0:m], si.u, sif.u)
                    sifv = sif[0:m].rearrange("p (h two) a -> p h two a", two=2)
                    for w2 in range(2):
                        tt("dve", eq[0:m], cabf[0:m, w2].unsqueeze(3).to_broadcast([m, 8, 16, 16]),
                           iota16[0:m, :].unsqueeze(1).unsqueeze(1).to_broadcast([m, 8, 16, 16]), ALU.is_equal,
                           cabf.u + iota16.u, eq.u)
                        tt("dve", eq[0:m], eq[0:m], sifv[:, :, w2, :].unsqueeze(2).to_broadcast([m, 8, 16, 16]),
                           ALU.mult, eq.u + sif.u, eq.u)
                        vredsum(res[0:m, w2, :].rearrange("p (h k) -> p h k", k=16), eq[0:m], eq.u, res.u)
                    resg = res[0:m, 2, :].rearrange("p (h k) -> p h k", k=16)
                    tt("dve", resg, cv[0:m], cv[0:m, :, 0:1].to_broadcast([m, 8, 16]), ALU.subtract, cv.u, res.u)
                    act(resg, resg, AF.Exp, res.u, res.u)
                    vredsum(gs[0:m, :], res[0:m, 2, :].rearrange("p (h k) -> p h k", k=16), res.u, gs.u)
                    vrecip(gs[0:m, :], gs[0:m, :], gs.u, gs.u)
                    tt("dve", resg, resg, gs[0:m, :].unsqueeze(2).to_broadcast([m, 8, 16]), ALU.mult, res.u + gs.u, res.u)
                    tb = banks[6]
                    for q3 in range(3):
                        tr(tb.t[:, q3 * 128:q3 * 128 + m], res[0:m, q3, :], ident_f[0:m, 0:m], res.u + ident_f.u, tb.u)
                    ro = rts[bcnt % 2]
                    cp("act", ro[:, :, 0:m], tb.t[:, 0:384].rearrange("p (q t) -> p q t", t=128)[:, :, 0:m], tb.u, ro.u)
                    dma("sp", "rts%d" % (bcnt % 2), RT[:, :, r0:r0 + m].rearrange("q p t -> p q t"), ro[:, :, 0:m], ro.u, ())
                    bcnt += 1
            pg.barrier()

    def p4(l):
        TG = 256
        with ExitStack() as ph:
            Wall = sb("Wall", [128, TG, 128], BF16, st=ph)
            ohj = [sb("ohj%d" % i, [128, 32, 128], BF16, st=ph) for i in range(2)]
            ohi = [sb("ohi%d" % i, [128, 32, 128], BF16, st=ph) for i in range(2)]
            ust = [sb("ust%d" % i, [128, 8, 512], BF16, st=ph) for i in range(2)]
            vst = [sb("vst%d" % i, [128, 4, D], BF16, st=ph) for i in range(2)]
            h2g = [sb("h2g%d" % i, [128, 8, TG], BF16, st=ph) for i in range(2)]
            rtg = [sb("rtg%d" % i, [128, 3, TG], st=ph) for i in range(2)]
            gl = [sb("gl%d" % i, [128, TG], BF16, st=ph) for i in range(3)]
            pTt = [sb("pTt%d" % i, [128, TG], BF16, st=ph) for i in range(3)]
            pgroups = [(c, TG) for c in range(0, SEQ, TG)]
            if NBLK:
                pgroups = pgroups[:NBLK]
            groups = pgroups + [(SEQ, NS)]
            pc = 0
            wc = 0
            sc_ = 0
            def load_grp(gi_):
                c0, n = groups[gi_]
                h2 = h2g[gi_ % 2]
                rt = rtg[gi_ % 2]
                dma("sp", "h2g%d" % (gi_ % 2), h2[:, :, 0:n], H2T[:, :, c0:c0 + n].rearrange("k p t -> p k t"), (), h2.u)
                dma("sp", "rtg%d" % (gi_ % 2), rt[:, :, 0:n], RT[:, :, c0:c0 + n].rearrange("q p t -> p q t"), (), rt.u)

            def gen_piece(gi_, t0):
                rt = rtg[gi_ % 2]
                pi_ = (t0 // 32) % 2
                oj = ohj[pi_]
                oi = ohi[pi_]
                for t in range(32):
                    tk = t0 + t
                    ts("dve", oj[:, t, :], iota_b[:], rt[:, 1, tk:tk + 1], ALU.is_equal, iota_b.u + rt.u, oj.u)
                    ts("dve", oi[:, t, :], iota_b[:], rt[:, 0, tk:tk + 1], ALU.is_equal, iota_b.u + rt.u, oi.u,
                       s2=rt[:, 2, tk:tk + 1], op1=ALU.mult)
                return oj, oi

            load_grp(0)
            pregen = {}
            for gi_, (c0, n) in enumerate(groups):
                h2 = h2g[gi_ % 2]
                rt = rtg[gi_ % 2]
                x = load_xb(c0, n)
                for t0 in range(0, n, 32):
                    if (gi_, t0) in pregen:
                        oj, oi = pregen[(gi_, t0)]
                    else:
                        oj, oi = gen_piece(gi_, t0)
                    for t4 in range(0, 32, 4):
                        wb = banks[6 + wc % 2]
                        wc += 1
                        for t in range(4):
                            mm(wb.t[:, t * 128:(t + 1) * 128], oj[:, t4 + t, :], oi[:, t4 + t, :], True, True,
                               oj.u + oi.u, wb.u)
                        cp("act", Wall[:, t0 + t4:t0 + t4 + 4, :],
                           wb.t[:, :].rearrange("p (t i) -> p t i", i=128), wb.u, Wall.u)
                    if t0 + 32 >= n and gi_ + 1 < len(groups):
                        load_grp(gi_ + 1)
                        nn = groups[gi_ + 1][1]
                        for tq in range(0, min(64, nn), 32):
                            pregen[(gi_ + 1, tq)] = gen_piece(gi_ + 1, tq)
                def emit_out(ec, vs, c4, p_):
                    for dc in range(8):
                        bk = banks[dc // 2]
                        mmg(bk.t[:, (dc % 2) * 256:(dc % 2) * 256 + n], vs[:, c4, dc * 128:(dc + 1) * 128],
                            p_[:, 0:n], (ec == 0 and dc % 2 == 0), ec == 127, vs.u + p_.u, bk.u)

                pend4 = None
                for eb in range(32):
                    us = ust[sc_ % 2]
                    vs = vst[sc_ % 2]
                    dma("sp", "ust%d" % (sc_ % 2), us[:], UT[:, :, eb * 512:(eb + 1) * 512].rearrange("k p e -> p k e"),
                        (), us.u)
                    dma("sp", "vst%d" % (sc_ % 2), vs[:], VBF[eb * 512:(eb + 1) * 512, :].rearrange("(c p) d -> p c d", p=128),
                        (), vs.u)
                    sc_ += 1
                    for c4 in range(4):
                        ec = eb * 4 + c4
                        ab = banks[4 + ec % 2]
                        for k in range(8):
                            mm(ab.t[:, 0:n], us[:, k, c4 * 128:(c4 + 1) * 128], h2[:, k, 0:n], k == 0, k == 7,
                               us.u + h2.u, ab.u)
                        g_ = gl[ec % 3]
                        p_ = pTt[ec % 3]
                        act(g_[:, 0:n], ab.t[:, 0:n], AF.Gelu_apprx_tanh, ab.u, g_.u)
                        tt("pool", p_[:, 0:n], g_[:, 0:n], Wall[:, 0:n, ec], ALU.mult, g_.u + Wall.u, p_.u)
                        if pend4 is not None:
                            emit_out(*pend4)
                        pend4 = (ec, vs, c4, p_)
                emit_out(*pend4)
                for dc in range(8):
                    bk = banks[dc // 2]
                    for (lo, hi, r) in segs(c0, n):
                        o_ = (dc % 2) * 256
                        stt(x[:, dc, lo:hi], bk.t[:, o_ + lo:o_ + hi], modT[l][:, 40 + dc, r:r + 1], x[:, dc, lo:hi],
                            ALU.mult, ALU.add, bk.u + modT[l].u + x.u, x.u)
                store_xb(x, c0, n)
            pg.barrier()

    def pfinal():
        with ExitStack() as ph:
            gfin = sb("gfin", [128, D], st=ph)
            dma("sp", "c0", gfin[:], I.g_final[0:1, :].partition_broadcast(128), (), gfin.u)
            xo = [sb("xo%d" % i, [128, D], st=ph) for i in range(2)]
            yo = [sb("yo%d" % i, [128, D], st=ph) for i in range(2)]
            xs_ = [sb("xs%d" % i, [128, 8, 128], st=ph) for i in range(2)]
            junk = sb("fjunk", [128, D], st=ph)
            ss = sb("fss", [128, 1], st=ph)
            tiles = list(range((T + 127) // 128))
            if NBLK:
                tiles = tiles[:4 * NBLK] + tiles[-5:]
            for ti in tiles:
                r0 = ti * 128
                m = min(128, T - r0)
                xs = xs_[ti % 2]
                dma("sp", "xs%d" % (ti % 2), xs[:, :, 0:m], xT[:, :, r0:r0 + m].rearrange("k p t -> p k t"), (), xs.u)
                o = xo[ti % 2]
                for hf in range(2):
                    b = banks[(ti * 2 + hf) % 4]
                    for kk in range(4):
                        k = hf * 4 + kk
                        tr(b.t[0:m, kk * 128:(kk + 1) * 128], xs[:, k, 0:m], ident_f[:], xs.u + ident_f.u, b.u)
                    cp("act" if hf == 0 else "dve", o[0:m, hf * 512:(hf + 1) * 512], b.t[0:m, :], b.u, o.u)
                act(junk[0:m, :], o[0:m, :], AF.Square, o.u, junk.u + ss.u, accum=ss[0:m, 0:1])
                act(ss[0:m, :], ss[0:m, :], AF.Sqrt, ss.u + epsc.u, ss.u, bias=epsc[0:m, 0:1], scale=1.0 / D)
                V("dve", lambda e, m=m: e.reciprocal(ss[0:m, :], ss[0:m, :]), ss.u, ss.u)
                y = yo[ti % 2]
                stt(y[0:m, :], o[0:m, :], ss[0:m, 0:1], gfin[0:m, :], ALU.mult, ALU.mult, o.u + ss.u + gfin.u, y.u)
                dma("sp", "yo%d" % (ti % 2), y_out[r0:r0 + m, :], y[0:m, :], y.u, ())

    for l in range(DEPTH):
        p1(l)
        if stage("p1_%d" % l):
            return finish()
        p2(l)
        if stage("p2_%d" % l):
            return finish()
        pu(l)
        p3(l)
        if stage("p3_%d" % l):
            return finish()
        p4(l)
        if stage("p4_%d" % l):
            return finish()
    pfinal()
    return finish()


_CACHE = {}


def _rope_tables():
    half = 8
    inv = (np.float32(500000.0) ** (-(np.arange(0, 16, 2, dtype=np.float32)) / np.float32(16))).astype(np.float32)
    pos = np.concatenate([np.arange(SEQ), np.tile(PAST + np.arange(16), 4)]).astype(np.float32)
    ang = (pos[:, None] * inv[None, :]).astype(np.float32)
    return np.cos(ang).astype(np.float32), np.sin(ang).astype(np.float32)


def make_in_maps(inp):
    f = lambda a: np.ascontiguousarray(np.asarray(a, dtype=np.float32))
    rel = f(inp["rel_bias"])
    q = np.arange(128)[None, :]
    k = np.arange(128)[:, None]
    d3 = np.clip(128 + q - k, -128, 128) + 128
    d4 = np.clip(q - k, -128, 128) + 128
    biasT = np.stack([rel[:, :, d3], rel[:, :, d4]], axis=2)
    bconst = np.ascontiguousarray(np.broadcast_to(rel[:, :, 256][:, :, None], (DEPTH, 8, 128)))
    rc, rs = _rope_tables()
    shared = {
        "w_ada": f(inp["w_ada"]),
        "b_adaT": f(np.asarray(inp["b_ada"]).reshape(DEPTH, 48, 128).transpose(0, 2, 1)),
        "g_attnT": f(np.asarray(inp["g_attn"]).reshape(DEPTH, 8, 128).transpose(0, 2, 1)),
        "g_ffnT": f(np.asarray(inp["g_ffn"]).reshape(DEPTH, 8, 128).transpose(0, 2, 1)),
        "w_in": f(inp["w_in"]),
        "lamv": f(np.concatenate([np.asarray(inp["lam_q1"]), np.asarray(inp["lam_k1"]),
                                  np.asarray(inp["lam_q2"]), np.asarray(inp["lam_k2"])], axis=1)),
        "a_gain": f(inp["a_gain"]),
        "biasT": f(biasT),
        "bconst": f(bconst),
        "b_gain": f(inp["b_gain"]),
        "w_out": f(inp["w_out"]),
        "pk_wq": f(inp["pk_wq"]),
        "pk_keys": f(np.asarray(inp["pk_keys"]).reshape(DEPTH, 16, 128, 128)),
        "pk_u": f(inp["pk_u"]),
        "pk_v": f(inp["pk_v"]),
        "g_final": f(np.asarray(inp["g_final"]).reshape(1, D)),
        "ident": np.eye(128, dtype=np.float32),
        "ropec": rc,
        "ropes": rs,
        "iota_in": np.ascontiguousarray(np.broadcast_to(np.arange(128, dtype=np.float32)[None, :], (128, 128))),
    }
    xp = np.asarray(inp["x_prompt"])
    xs = np.asarray(inp["x_sample"])
    cp_ = np.asarray(inp["c_prompt"])
    cs = np.asarray(inp["c_sample"])
    maps = []
    for c in range(8):
        b = c % 4
        sl = slice(4 * c, 4 * c + 4)
        c5 = np.concatenate([cp_[b:b + 1], cs[sl]], axis=0)
        m = dict(shared)
        m["xin"] = f(np.concatenate([xp[b], xs[sl].reshape(NS, D)], axis=0))
        m["cT"] = f(c5.reshape(5, 8, 128).transpose(2, 1, 0))
        m["cak"] = f(np.asarray(inp["cache_a_k"])[:, sl].reshape(DEPTH, 4, PAST, 512))
        m["cav"] = f(np.asarray(inp["cache_a_v"])[:, sl].reshape(DEPTH, 4, PAST, 512))
        m["cbk"] = f(np.asarray(inp["cache_b_k"])[:, sl].reshape(DEPTH, 4, 512, 512))
        m["cbv"] = f(np.asarray(inp["cache_b_v"])[:, sl].reshape(DEPTH, 4, 512, 512))
        maps.append(m)
    return maps


def assemble(results):
    y_p = np.stack([results[b]["y"][:SEQ] for b in range(4)])
    y_s = np.concatenate([results[c]["y"][SEQ:].reshape(4, 16, D) for c in range(8)])
    nakp = np.stack([results[b]["nak"][:, :SEQ] for b in range(4)], axis=1).reshape(DEPTH, 4, SEQ, 4, 128)
    navp = np.stack([results[b]["nav"][:, :SEQ] for b in range(4)], axis=1).reshape(DEPTH, 4, SEQ, 4, 128)
    nbkp = np.stack([results[b]["nbk_p"] for b in range(4)], axis=1).reshape(DEPTH, 4, 512, 8, 64)
    nbvp = np.stack([results[b]["nbv_p"] for b in range(4)], axis=1).reshape(DEPTH, 4, 512, 8, 64)
    naks = np.concatenate([results[c]["nak"][:, SEQ:].reshape(DEPTH, 4, 16, 4, 128) for c in range(8)], axis=1)
    navs = np.concatenate([results[c]["nav"][:, SEQ:].reshape(DEPTH, 4, 16, 4, 128) for c in range(8)], axis=1)
    nbks = np.concatenate([results[c]["nbk_s"].reshape(DEPTH, 4, 512, 8, 64) for c in range(8)], axis=1)
    nbvs = np.concatenate([results[c]["nbv_s"].reshape(DEPTH, 4, 512, 8, 64) for c in range(8)], axis=1)
    return tuple(np.ascontiguousarray(a, dtype=np.float32) for a in
                 (y_p, y_s, nakp, navp, nbkp, nbvp, naks, navs, nbks, nbvs))


def kernel(**inputs):
    nc, es = build()
    maps = make_in_maps(inputs)
    used = set(build.used_inputs)
    maps = [{k: v for k, v in m.items() if k in used} for m in maps]
    res = run_bass_kernel_spmd(nc, maps, core_ids=list(range(8)))
    return assemble(res.results)
```
